# Optimizing a Trainium2 kernel written in Bass

```python
import math
import jax
import jax.numpy as jnp
from jax import lax
import numpy as np

D_MODEL = 1024
BATCH = 8
SEQ = 4096
DEPTH = 2

GRID_W = 64
CTX_LEN = 256
EPS = 1e-6
ATT_HEADS = 8
ATT_KV_HEADS = 2
ATT_GROUP = ATT_HEADS // ATT_KV_HEADS
HEAD_DIM = 64
ROPE_PAIRS = HEAD_DIM // 4
ROPE_THETA = 10000.0
Q_BLOCK = 128
A_Q = ATT_HEADS * HEAD_DIM
A_KV = ATT_KV_HEADS * HEAD_DIM
GM_GROUPS = 4
GM_CHUNK = 128
GM_GROUP_CH = 128
GM_CH = GM_GROUPS * GM_GROUP_CH
CV_CH = 512
CV_WIDTH = 31
DN_HEADS = 4
DN_DK = 128
DN_DV = 128
DN_CONV = 5
DN_CHUNK = 64
N_DIR = 2
DN_QK = DN_HEADS * DN_DK
DN_V = DN_HEADS * DN_DV
DN_QKV = 2 * DN_QK + DN_V
DN_AB = 2 * N_DIR * DN_HEADS
AB_IN = A_Q + 2 * A_KV + 2 * GM_CH
AB_OUT = A_Q + GM_CH
CD_IN = 2 * CV_CH + DN_QKV + DN_AB + DN_V
CD_OUT = CV_CH + DN_V
N_AB = (DEPTH + 1) // 2
N_CD = DEPTH // 2
N_EXPERTS = 16
N_GROUPS = 4
EXPERTS_PER_GROUP = N_EXPERTS // N_GROUPS
TOP_K = 2
EXPERT_FF = 512

kernel_name = 'hybrid_dit_gqa_gmlp_conformer_deltanet_moe'


def rms_norm(x, g):
    xf = x.astype(jnp.float32)
    y = xf * lax.rsqrt(jnp.mean(xf * xf, axis=-1, keepdims=True) + EPS)
    return (y * g.astype(jnp.float32)).astype(x.dtype)


def layer_norm(x, g, b):
    xf = x.astype(jnp.float32)
    mu = jnp.mean(xf, axis=-1, keepdims=True)
    var = jnp.mean(jnp.square(xf - mu), axis=-1, keepdims=True)
    y = (xf - mu) * lax.rsqrt(var + EPS)
    return (y * g.astype(jnp.float32) + b.astype(jnp.float32)).astype(x.dtype)


def l2_normalize(x):
    return x * lax.rsqrt(jnp.sum(x * x, axis=-1, keepdims=True) + EPS)


def modulate(h, shift, scale):
    return h * (1.0 + scale) + shift


def depthwise_conv(x, w):
    k = w.shape[0]
    return lax.conv_general_dilated(
        x, w[:, None, :].astype(x.dtype), (1,), [(k // 2, k // 2)],
        dimension_numbers=('NWC', 'WIO', 'NWC'), feature_group_count=w.shape[1])


def apply_axial_rope(x, cos, sin):
    b, s, h, d = x.shape
    xr = x.astype(jnp.float32).reshape(b, s, h, 2, 2, d // 4)
    x1, x2 = xr[..., 0, :], xr[..., 1, :]
    out = jnp.stack([x1 * cos - x2 * sin, x2 * cos + x1 * sin], axis=-2)
    return out.reshape(b, s, h, d).astype(x.dtype)


def softmax_attend(q, k, v):
    s = jnp.einsum('bqhgd,bshd->bhgqs', q, k, preferred_element_type=jnp.float32) * (HEAD_DIM ** -0.5)
    p = jax.nn.softmax(s, axis=-1).astype(v.dtype)
    return jnp.einsum('bhgqs,bshd->bqhgd', p, v)


def blocked_latent_attention(q, k_all, v_all):
    b, s, h, d = q.shape
    qb = q.reshape(b, s // Q_BLOCK, Q_BLOCK, ATT_KV_HEADS, ATT_GROUP, d).transpose(1, 0, 2, 3, 4, 5)
    o = lax.map(lambda qi: softmax_attend(qi, k_all, v_all), qb)
    return o.transpose(1, 0, 2, 3, 4, 5).reshape(b, s, h * d)


def spatial_gating(u, v, ln_g, ln_b, w_s, b_s):
    b, t, _ = v.shape
    v = layer_norm(v, ln_g, ln_b).reshape(b, t // GM_CHUNK, GM_CHUNK, GM_GROUPS, GM_GROUP_CH)
    mixed = jnp.einsum('gpq,bnqgc->bnpgc', w_s, v) + b_s.T[:, :, None]
    return u * mixed.reshape(b, t, GM_CH)


def attn_gmlp_mixer(h_lat, h_ctx, w_in, q_norm, k_norm, ln_g, ln_b, w_s, b_s, w_out, cos, sin, need_ctx):
    cuts = [A_Q, A_Q + A_KV, A_Q + 2 * A_KV, A_Q + 2 * A_KV + GM_CH]

    def project(h, with_gate):
        b, t, _ = h.shape
        q, k, v, u, vg = jnp.split(h @ w_in, cuts, axis=-1)
        q = rms_norm(q.reshape(b, t, ATT_HEADS, HEAD_DIM), q_norm)
        k = rms_norm(k.reshape(b, t, ATT_KV_HEADS, HEAD_DIM), k_norm)
        v = v.reshape(b, t, ATT_KV_HEADS, HEAD_DIM)
        sg = spatial_gating(jax.nn.gelu(u), jax.nn.gelu(vg), ln_g, ln_b, w_s, b_s) if with_gate else None
        return q, k, v, sg

    ql, kl, vl, sl = project(h_lat, True)
    qc, kc, vc, sc = project(h_ctx, need_ctx)
    ql = apply_axial_rope(ql, cos, sin)
    kl = apply_axial_rope(kl, cos, sin)
    k_all = jnp.concatenate([kc, kl], axis=1)
    v_all = jnp.concatenate([vc, vl], axis=1)
    o_lat = blocked_latent_attention(ql, k_all, v_all)
    out_lat = jnp.concatenate([o_lat, sl], axis=-1) @ w_out
    if not need_ctx:
        return out_lat, None
    b, cl = qc.shape[0], qc.shape[1]
    o_ctx = softmax_attend(qc.reshape(b, cl, ATT_KV_HEADS, ATT_GROUP, HEAD_DIM), kc, vc).reshape(b, cl, A_Q)
    out_ctx = jnp.concatenate([o_ctx, sc], axis=-1) @ w_out
    return out_lat, out_ctx


def conformer_branch(a, gate, dw_w, dw_b, ln_g, ln_b):
    y = a * jax.nn.sigmoid(gate)
    y = depthwise_conv(y, dw_w) + dw_b
    return jax.nn.silu(layer_norm(y, ln_g, ln_b))


def delta_features(qkv, ab, conv_w, a_log, dt_bias):
    b, t, _ = qkv.shape
    y = jax.nn.silu(depthwise_conv(qkv, conv_w)).astype(jnp.float32)
    q, k, v = jnp.split(y, [DN_QK, 2 * DN_QK], axis=-1)
    q = l2_normalize(q.reshape(b, t, DN_HEADS, DN_DK)) * (DN_DK ** -0.5)
    k = l2_normalize(k.reshape(b, t, DN_HEADS, DN_DK))
    v = v.reshape(b, t, DN_HEADS, DN_DV)
    abr = ab.astype(jnp.float32).reshape(b, t, 2, N_DIR, DN_HEADS)
    g = -jnp.exp(a_log.astype(jnp.float32)) * jax.nn.softplus(abr[:, :, 0] + dt_bias.astype(jnp.float32))
    beta = jax.nn.sigmoid(abr[:, :, 1])
    return q, k, v, g, beta


def gated_delta_rule(q, k, v, g, beta, state0):
    b, t, h, dk = q.shape
    dv = v.shape[-1]
    L = DN_CHUNK
    n = t // L
    qc = q.reshape(b, n, L, h, dk).transpose(1, 0, 3, 2, 4)
    kc = k.reshape(b, n, L, h, dk).transpose(1, 0, 3, 2, 4)
    vc = v.reshape(b, n, L, h, dv).transpose(1, 0, 3, 2, 4)
    gc = jnp.cumsum(g.reshape(b, n, L, h).transpose(1, 0, 3, 2), axis=-1)
    bc = beta.reshape(b, n, L, h).transpose(1, 0, 3, 2)
    incl = jnp.tril(jnp.ones((L, L), dtype=bool))
    strict = jnp.tril(jnp.ones((L, L), dtype=bool), -1)
    decay = jnp.exp(jnp.where(incl, gc[..., :, None] - gc[..., None, :], -jnp.inf))
    kk = jnp.einsum('nbhid,nbhjd->nbhij', kc, kc)
    lower = jnp.where(strict, bc[..., :, None] * kk * decay, 0.0)
    rhs = jnp.concatenate([vc * bc[..., None], kc * (bc * jnp.exp(gc))[..., None]], axis=-1)
    sol = lax.linalg.triangular_solve(lower, rhs, left_side=True, lower=True, unit_diagonal=True)
    u_c, w_c = sol[..., :dv], sol[..., dv:]
    qk = jnp.einsum('nbhid,nbhjd->nbhij', qc, kc) * decay

    def step(s, inp):
        q_i, k_i, u_i, w_i, g_i, a_i = inp
        v_new = u_i - jnp.einsum('bhlk,bhkv->bhlv', w_i, s)
        o_i = (jnp.einsum('bhlk,bhkv->bhlv', q_i * jnp.exp(g_i)[..., None], s)
               + jnp.einsum('bhij,bhjv->bhiv', a_i, v_new))
        g_last = g_i[..., -1]
        k_dec = k_i * jnp.exp(g_last[..., None] - g_i)[..., None]
        s = s * jnp.exp(g_last)[..., None, None] + jnp.einsum('bhlk,bhlv->bhkv', k_dec, v_new)
        return s, o_i

    s_final, o = lax.scan(step, state0, (qc, kc, u_c, w_c, gc, qk))
    return s_final, o.transpose(1, 0, 3, 2, 4).reshape(b, t, h, dv)


def maybe_flip(t, rev):
    return jnp.flip(t, axis=1) if rev else t


def delta_output(o, gate, o_norm):
    b, t = gate.shape[0], gate.shape[1]
    y = rms_norm(o, o_norm).astype(gate.dtype) * jax.nn.silu(gate.reshape(b, t, DN_HEADS, DN_DV))
    return y.reshape(b, t, DN_V)


def conv_delta_mixer(h_lat, h_ctx, w_in, dw_w, dw_b, ln_g, ln_b, sconv_w, a_log, dt_bias, o_norm, w_out, need_ctx):
    cuts = [CV_CH, 2 * CV_CH, 2 * CV_CH + DN_QKV, 2 * CV_CH + DN_QKV + DN_AB]
    pl = jnp.split(h_lat @ w_in, cuts, axis=-1)
    pc = jnp.split(h_ctx @ w_in, cuts, axis=-1)
    ql, kl, vl, gl, bl = delta_features(pl[2], pl[3], sconv_w, a_log, dt_bias)
    qc, kc, vc, gcx, bcx = delta_features(pc[2], pc[3], sconv_w, a_log, dt_bias)
    b = h_lat.shape[0]
    zero_state = jnp.zeros((b, DN_HEADS, DN_DK, DN_DV), jnp.float32)
    o_lat = jnp.zeros(ql.shape[:3] + (DN_DV,), jnp.float32)
    o_ctx = jnp.zeros(qc.shape[:3] + (DN_DV,), jnp.float32)
    for d in range(N_DIR):
        rev = d == 1
        s_ctx, oc_d = gated_delta_rule(maybe_flip(qc, rev), maybe_flip(kc, rev), maybe_flip(vc, rev),
                                       maybe_flip(gcx[:, :, d], rev), maybe_flip(bcx[:, :, d], rev), zero_state)
        _, ol_d = gated_delta_rule(maybe_flip(ql, rev), maybe_flip(kl, rev), maybe_flip(vl, rev),
                                   maybe_flip(gl[:, :, d], rev), maybe_flip(bl[:, :, d], rev), s_ctx)
        o_lat = o_lat + maybe_flip(ol_d, rev)
        if need_ctx:
            o_ctx = o_ctx + maybe_flip(oc_d, rev)
    conv_lat = conformer_branch(pl[0], pl[1], dw_w, dw_b, ln_g, ln_b)
    out_lat = jnp.concatenate([conv_lat, delta_output(o_lat, pl[4], o_norm)], axis=-1) @ w_out
    if not need_ctx:
        return out_lat, None
    conv_ctx = conformer_branch(pc[0], pc[1], dw_w, dw_b, ln_g, ln_b)
    out_ctx = jnp.concatenate([conv_ctx, delta_output(o_ctx, pc[4], o_norm)], axis=-1) @ w_out
    return out_lat, out_ctx


def moe_ffn(h, router_w, router_b, w_gate, w_up, w_down):
    t = h.shape[0]
    scores = jax.nn.sigmoid(jnp.matmul(h, router_w, preferred_element_type=jnp.float32))
    sel = scores + router_b.astype(jnp.float32)
    group_score = lax.top_k(sel.reshape(t, N_GROUPS, EXPERTS_PER_GROUP), TOP_K)[0].sum(-1)
    best = jnp.argmax(group_score, axis=-1)
    in_group = (jnp.arange(N_EXPERTS) // EXPERTS_PER_GROUP)[None, :] == best[:, None]
    _, idx = lax.top_k(jnp.where(in_group, sel, -jnp.inf), TOP_K)
    w = jnp.take_along_axis(scores, idx, axis=-1)
    w = w / jnp.sum(w, axis=-1, keepdims=True)
    combine = jnp.einsum('tk,tke->te', w, jax.nn.one_hot(idx, N_EXPERTS, dtype=jnp.float32)).astype(h.dtype)
    out = jnp.zeros_like(h)
    for e in range(N_EXPERTS):
        a = jax.nn.silu(h @ w_gate[e]) * (h @ w_up[e])
        out = out + combine[:, e:e + 1] * (a @ w_down[e])
    return out


def setup_inputs(seed: int = 0) -> dict:
    key = jax.random.key(seed)
    ks = iter(jax.random.split(key, 40))
    f32 = jnp.float32

    def nrm(shape, scale):
        return jax.random.normal(next(ks), shape, f32) * scale

    def gain(shape):
        return 1.0 + nrm(shape, 0.02)

    dt = jnp.exp(jax.random.uniform(next(ks), (N_CD, N_DIR, DN_HEADS), f32,
                                    minval=math.log(1e-3), maxval=math.log(1e-1)))
    return {
        'x': nrm((BATCH, SEQ, D_MODEL), 1.0),
        'c': nrm((BATCH, D_MODEL), 1.0),
        'ctx': nrm((BATCH, CTX_LEN, D_MODEL), 1.0),
        'c_ctx': nrm((D_MODEL,), 1.0),
        'mod_w': nrm((DEPTH, D_MODEL, 6 * D_MODEL), 0.5 * D_MODEL ** -0.5),
        'mod_b': nrm((DEPTH, 6 * D_MODEL), 0.02),
        'norm1_g': gain((DEPTH, D_MODEL)),
        'norm2_g': gain((DEPTH, D_MODEL)),
        'ab_w_in': nrm((N_AB, D_MODEL, AB_IN), D_MODEL ** -0.5),
        'ab_q_norm': gain((N_AB, HEAD_DIM)),
        'ab_k_norm': gain((N_AB, HEAD_DIM)),
        'gm_ln_g': gain((N_AB, GM_CH)),
        'gm_ln_b': nrm((N_AB, GM_CH), 0.02),
        'gm_w_s': nrm((N_AB, GM_GROUPS, GM_CHUNK, GM_CHUNK), GM_CHUNK ** -0.5),
        'gm_b_s': gain((N_AB, GM_GROUPS, GM_CHUNK)),
        'ab_w_out': nrm((N_AB, AB_OUT, D_MODEL), AB_OUT ** -0.5),
        'cd_w_in': nrm((N_CD, D_MODEL, CD_IN), D_MODEL ** -0.5),
        'cv_dw_w': nrm((N_CD, CV_WIDTH, CV_CH), CV_WIDTH ** -0.5),
        'cv_dw_b': nrm((N_CD, CV_CH), 0.02),
        'cv_ln_g': gain((N_CD, CV_CH)),
        'cv_ln_b': nrm((N_CD, CV_CH), 0.02),
        'dn_conv_w': nrm((N_CD, DN_CONV, DN_QKV), DN_CONV ** -0.5),
        'dn_a_log': jnp.log(jax.random.uniform(next(ks), (N_CD, N_DIR, DN_HEADS), f32, minval=1.0, maxval=16.0)),
        'dn_dt_bias': dt + jnp.log(-jnp.expm1(-dt)),
        'dn_o_norm': gain((N_CD, DN_DV)),
        'cd_w_out': nrm((N_CD, CD_OUT, D_MODEL), CD_OUT ** -0.5),
        'router_w': nrm((D_MODEL, N_EXPERTS), D_MODEL ** -0.5),
        'router_b': nrm((N_EXPERTS,), 0.01),
        'moe_w_gate': nrm((DEPTH, N_EXPERTS, D_MODEL, EXPERT_FF), D_MODEL ** -0.5),
        'moe_w_up': nrm((DEPTH, N_EXPERTS, D_MODEL, EXPERT_FF), D_MODEL ** -0.5),
        'moe_w_down': nrm((DEPTH, N_EXPERTS, EXPERT_FF, D_MODEL), EXPERT_FF ** -0.5),
        'final_norm_g': gain((D_MODEL,)),
    }


def reference(x, c, ctx, c_ctx, mod_w, mod_b, norm1_g, norm2_g,
              ab_w_in, ab_q_norm, ab_k_norm, gm_ln_g, gm_ln_b, gm_w_s, gm_b_s, ab_w_out,
              cd_w_in, cv_dw_w, cv_dw_b, cv_ln_g, cv_ln_b, dn_conv_w, dn_a_log, dn_dt_bias, dn_o_norm, cd_w_out,
              router_w, router_b, moe_w_gate, moe_w_up, moe_w_down, final_norm_g):
    b, s, d = x.shape
    rows = s // GRID_W
    row = jnp.repeat(jnp.arange(rows, dtype=jnp.int32), GRID_W)
    col = jnp.tile(jnp.arange(GRID_W, dtype=jnp.int32), rows)
    freqs = ROPE_THETA ** (-jnp.arange(ROPE_PAIRS, dtype=jnp.float32) / ROPE_PAIRS)
    ang = jnp.stack([row, col], axis=-1).astype(jnp.float32)[..., None] * freqs
    cos = jnp.cos(ang)[:, None]
    sin = jnp.sin(ang)[:, None]

    x_lat, x_ctx = x, ctx
    silu_c = jax.nn.silu(c)
    silu_cc = jax.nn.silu(c_ctx)
    for layer in range(DEPTH):
        last = layer == DEPTH - 1
        i = layer // 2
        mod = silu_c @ mod_w[layer] + mod_b[layer]
        mod_c = silu_cc @ mod_w[layer] + mod_b[layer]
        sh1, sc1, ga1, sh2, sc2, ga2 = jnp.split(mod[:, None, :], 6, axis=-1)
        csh1, csc1, cga1, csh2, csc2, cga2 = jnp.split(mod_c, 6, axis=-1)
        h_l = modulate(rms_norm(x_lat, norm1_g[layer]), sh1, sc1)
        h_c = modulate(rms_norm(x_ctx, norm1_g[layer]), csh1, csc1)
        if layer % 2 == 0:
            m_l, m_c = attn_gmlp_mixer(h_l, h_c, ab_w_in[i], ab_q_norm[i], ab_k_norm[i], gm_ln_g[i], gm_ln_b[i],
                                       gm_w_s[i], gm_b_s[i], ab_w_out[i], cos, sin, not last)
        else:
            m_l, m_c = conv_delta_mixer(h_l, h_c, cd_w_in[i], cv_dw_w[i], cv_dw_b[i], cv_ln_g[i], cv_ln_b[i],
                                        dn_conv_w[i], dn_a_log[i], dn_dt_bias[i], dn_o_norm[i], cd_w_out[i], not last)
        x_lat = x_lat + ga1 * m_l
        h_l = modulate(rms_norm(x_lat, norm2_g[layer]), sh2, sc2).reshape(-1, d)
        if last:
            f_l = moe_ffn(h_l, router_w, router_b, moe_w_gate[layer], moe_w_up[layer], moe_w_down[layer])
        else:
            x_ctx = x_ctx + cga1 * m_c
            h_c = modulate(rms_norm(x_ctx, norm2_g[layer]), csh2, csc2).reshape(-1, d)
            f = moe_ffn(jnp.concatenate([h_l, h_c], axis=0), router_w, router_b,
                        moe_w_gate[layer], moe_w_up[layer], moe_w_down[layer])
            f_l = f[:b * s]
            x_ctx = x_ctx + cga2 * f[b * s:].reshape(x_ctx.shape)
        x_lat = x_lat + ga2 * f_l.reshape(b, s, d)
    return rms_norm(x_lat, final_norm_g)
```

```python
import os
import contextlib
import numpy as np
import ml_dtypes
import concourse.bass as bass
import concourse.mybir as mybir
from concourse.bass_utils import run_bass_kernel_spmd


F32 = mybir.dt.float32
BF16 = mybir.dt.bfloat16
AF = mybir.ActivationFunctionType
ALU = mybir.AluOpType
AX = mybir.AxisListType

ENGS = ["sync", "scalar", "vector", "gpsimd", "tensor"]


def _box(ap):
    t = ap.tensor
    name = ap.name
    dsz = mybir.dt.size(ap.dtype)
    pairs = list(ap.ap)
    off = int(ap.offset)
    space = str(ap.space)
    if space in ("SB", "PSUM"):
        pstep, pcnt = pairs[0]
        rest = pairs[1:]
        if pstep == 0:
            p0, f0 = 0, off
            pcnt_eff = 1
        else:
            p0, f0 = off // pstep, off % pstep
            pcnt_eff = pcnt
        ext = 0
        for st, cn in rest:
            ext += abs(st) * (cn - 1)
        if space == "PSUM":
            return (name, 0, 128, 0, 1 << 20)
        return (name, p0, p0 + pcnt_eff, f0 * dsz, (f0 + ext + 1) * dsz)
    else:
        ext = 0
        for st, cn in pairs:
            ext += abs(st) * (cn - 1)
        return (name, 0, 1, off * dsz, (off + ext + 1) * dsz)


def _ovl(a, b):
    return a[1] < b[2] and b[1] < a[2] and a[3] < b[4] and b[3] < a[4]


def _covers(a, b):
    return a[1] <= b[1] and a[2] >= b[2] and a[3] <= b[3] and a[4] >= b[4]


class _EngProxy:
    def __init__(self, prog, name):
        self._p = prog
        self._n = name

    def __getattr__(self, meth):
        def call(**kw):
            reads, writes = [], []
            for k, v in kw.items():
                if v is not None and k in ("out_offset", "in_offset") and hasattr(v, "ap"):
                    reads.append(v.ap)
                    continue
                if v is None or not hasattr(v, "ap") or not hasattr(v, "tensor"):
                    continue
                if k in ("out", "accum_out", "ap"):
                    writes.append(v)
                else:
                    reads.append(v)
            if meth == "matmul" and kw.get("start") is False:
                pass
            self._p.add(self._n, meth, kw, reads, writes)
        return call


class Prog:
    def __init__(self, nc, n_dma_sems=12, epoch=20000):
        self.nc = nc
        self.ops = []
        self.hist = {}
        self.epoch = epoch
        self.n_dma_sems = n_dma_sems
        self.sync = _EngProxy(self, "sync")
        self.scalar = _EngProxy(self, "scalar")
        self.vector = _EngProxy(self, "vector")
        self.gpsimd = _EngProxy(self, "gpsimd")
        self.tensor = _EngProxy(self, "tensor")
        self._psum_banks = None
        self._psum_i = 0
        self._uid = 0
        self._stacks = []
        self._bar_from = 0
        self._bar_tails = set()
        self._pending = {}

    def sb(self, name, shape, dtype):
        self._uid += 1
        nm = f"{name}_{self._uid}"
        if self._stacks:
            return self._stacks[-1].enter_context(self.nc.sbuf_tensor(nm, list(shape), dtype))
        return self.nc.alloc_sbuf_tensor(nm, list(shape), dtype)

    def pool(self, name, shape, dtype, bufs=2):
        ts = [self.sb(f"{name}{i}", shape, dtype) for i in range(bufs)]
        return _Pool(ts)

    def scope(self):
        return _Scope(self)

    def barrier(self):
        tails = set()
        last = {}
        for i in range(self._bar_from, len(self.ops)):
            op = self.ops[i]
            if op["dma"]:
                tails.add(i)
            else:
                last[op["eng"]] = i
        tails |= set(last.values())
        tails |= self._bar_tails
        self._bar_tails = set(last.values())
        self._bar_from = len(self.ops)
        self._pending = {e: set(tails) | self._pending.get(e, set()) for e in ENGS}

    def psum_init(self):
        self._psum_banks = [self.nc.alloc_psum_tensor(f"psb{i}", [128, 512], F32) for i in range(8)]

    def psum(self, dtype=F32):
        t = self._psum_banks[self._psum_i % 8]
        self._psum_i += 1
        a = t[:]
        if dtype != F32:
            a = a.bitcast(dtype)
        return a

    def dram(self, name, shape, dtype, kind="Internal"):
        return self.nc.dram_tensor(name, list(shape), dtype, kind=kind).ap()

    def add(self, eng, meth, kw, reads, writes):
        idx = len(self.ops)
        deps = set()
        if eng in self._pending:
            deps |= self._pending.pop(eng)
        rb = [_box(a) for a in reads]
        wb = [_box(a) for a in writes]
        wb = wb + [b for b in rb if b[4] == (1 << 20)]
        for b in rb:
            for (ob, oi, kind) in self.hist.get(b[0], ()):
                if kind == "w" and _ovl(b, ob):
                    deps.add(oi)
        for b in wb:
            for (ob, oi, kind) in self.hist.get(b[0], ()):
                if _ovl(b, ob):
                    deps.add(oi)
        for b in wb:
            lst = self.hist.setdefault(b[0], [])
            lst[:] = [e for e in lst if not _covers(b, e[0])]
            lst.append((b, idx, "w"))
        for b in rb:
            lst = self.hist.setdefault(b[0], [])
            lst[:] = [e for e in lst if not (e[2] == "r" and e[0] == b and (e[1] == idx or self.ops[e[1]]["eng"] == eng))]
            lst.append((b, idx, "r"))
        self.ops.append(dict(eng=eng, meth=meth, kw=kw, deps=deps, dma=(meth in ("dma_start", "indirect_dma_start"))))

    def emit(self):
        nc = self.nc
        ops = self.ops
        eng_count = {e: 0 for e in ENGS}
        eng_sems = {e: [] for e in ENGS}
        dma_sems = {e: [] for e in ENGS}
        dma_cnt = {}
        dma_rr = {e: 0 for e in ENGS}
        dma_last = {}
        for i, op in enumerate(ops):
            e = op["eng"]
            if op["dma"]:
                if not dma_sems[e]:
                    n = self.n_dma_sems if e == "sync" else 6
                    dma_sems[e] = [nc.alloc_semaphore(f"dq_{e}_{k}") for k in range(n)]
                k = dma_rr[e] % len(dma_sems[e])
                dma_rr[e] += 1
                sem = dma_sems[e][k]
                key = (e, k)
                prev = dma_last.get(key)
                if prev is not None:
                    op["deps"].add(prev)
                dma_last[key] = i
                c = dma_cnt.get(key, 0) + 1
                dma_cnt[key] = c
                op["tok"] = (sem, 16 * c, key)
                op["inc"] = 16
            else:
                c = eng_count[e]
                ep = c // self.epoch
                if ep >= len(eng_sems[e]):
                    eng_sems[e].append(nc.alloc_semaphore(f"es_{e}_{ep}"))
                sem = eng_sems[e][ep]
                eng_count[e] = c + 1
                op["tok"] = (sem, (c % self.epoch) + 1, (e, "c", ep))
                op["inc"] = 1
        self.final_dma = [(dma_sems[e][k], 16 * c) for (e, k), c in dma_cnt.items()]
        per_eng = {e: [i for i, op in enumerate(ops) if op["eng"] == e] for e in ENGS}
        self.n_waits = 0

        def run(engname, eobj):
            seen = {}
            for i in per_eng[engname]:
                op = ops[i]
                need = {}
                for d in op["deps"]:
                    dop = ops[d]
                    if dop["eng"] == engname and not dop["dma"] and engname == "tensor":
                        continue
                    sem, val, key = dop["tok"]
                    if seen.get(key, 0) >= val:
                        continue
                    if key not in need or need[key][1] < val:
                        need[key] = (sem, val)
                for key, (sem, val) in need.items():
                    eobj.wait_ge(sem, val)
                    seen[key] = val
                    self.n_waits += 1
                try:
                    ins = getattr(eobj, op["meth"])(**op["kw"])
                except Exception:
                    print("FAILED OP", engname, op["meth"], {k_: (getattr(v_, "shape", None), getattr(v_, "name", None)) for k_, v_ in op["kw"].items()})
                    raise
                sem, val, key = op["tok"]
                ins.then_inc(sem, op["inc"])
            if engname == "sync":
                for sem, val in self.final_dma:
                    eobj.wait_ge(sem, val)

        with nc.Block() as block:
            @block.sync
            def _(e):
                run("sync", e)

            @block.scalar
            def _(e):
                run("scalar", e)

            @block.vector
            def _(e):
                run("vector", e)

            @block.gpsimd
            def _(e):
                run("gpsimd", e)

            @block.tensor
            def _(e):
                run("tensor", e)


class _Pool:
    def __init__(self, ts):
        self.ts = ts
        self.i = 0

    def next(self):
        t = self.ts[self.i % len(self.ts)]
        self.i += 1
        return t


class _Scope:
    def __init__(self, p):
        self.p = p

    def __enter__(self):
        st = contextlib.ExitStack()
        st.__enter__()
        self.p._stacks.append(st)
        return self

    def __exit__(self, *a):
        st = self.p._stacks.pop()
        st.__exit__(None, None, None)
        self.p.barrier()
        return False


D = 1024
S = 4096
CL = 256
NT = 34
EPS = 1e-6
NE = 16
FF = 512


def fap(ap, dims):
    return bass.AP(ap.tensor, ap.offset, [list(ap.ap[0])] + [list(d) for d in dims])


class PsumPool:
    def __init__(self, p, banks):
        self.p = p
        self.banks = banks
        self.i = 0

    def next(self, dtype=F32):
        t = self.p._psum_banks[self.banks[self.i % len(self.banks)]]
        self.i += 1
        a = t[:]
        if dtype != F32:
            a = a.bitcast(dtype)
        return a


class K:
    pass


def rstd_from_ss(p, out, ss, n, tmp):
    p.vector.tensor_scalar(out=tmp, in0=ss, scalar1=1.0 / n, scalar2=EPS, op0=ALU.mult, op1=ALU.add)
    p.scalar.activation(out=tmp, in_=tmp, func=AF.Sqrt)
    p.vector.reciprocal(out=out, in_=tmp)


def build(debug=False, stop_after=None):
    nc = bass.Bass("TRN2", target_bir_lowering=False)
    p = Prog(nc)
    p.psum_init()
    k = K()
    IN = lambda name, shape, dt=F32: p.dram(name, shape, dt, kind="ExternalInput")
    x_in = IN("x", [S, D])
    ctx_in = IN("ctx", [CL, D])
    c_in = IN("c", [8, 128])
    cc_in = IN("c_ctx", [8, 128])
    mod_w = IN("mod_w", [2, D, 6 * D])
    mod_b = IN("mod_b", [2, 48, 128])
    n1g = IN("norm1_g", [2, 8, 128])
    n2g = IN("norm2_g", [2, 8, 128])
    fng = IN("final_norm_g", [1, D])
    ab_w_in = IN("ab_w_in", [D, 1792])
    ab_qn = IN("ab_q_norm", [1, 64])
    ab_kn = IN("ab_k_norm", [1, 64])
    gm_lng = IN("gm_ln_g", [1, 512])
    gm_lnb = IN("gm_ln_b", [1, 512])
    gm_ws = IN("gm_w_s", [4, 128, 128])
    gm_bs = IN("gm_b_s", [1, 512])
    ab_w_out = IN("ab_w_out", [D, D])
    router_w = IN("router_w", [D, NE])
    router_b = IN("router_b", [1, NE])
    w_gate = IN("moe_w_gate", [2, NE, D, FF])
    w_up = IN("moe_w_up", [2, NE, D, FF])
    w_down = IN("moe_w_down", [2, NE, FF, D])
    ident_in = IN("ident", [128, 128])
    cos_in = IN("rope_cos", [S, 32])
    sin_in = IN("rope_sin", [S, 32])
    out = p.dram("out", [S, D], F32, kind="ExternalOutput")

    dk = "ExternalOutput" if debug else "Internal"
    xl1 = p.dram("xl1", [S, D], F32, kind=dk)
    xc1 = p.dram("xc1", [CL, D], F32, kind=dk)
    xl2 = p.dram("xl2", [S, D], F32, kind=dk)
    xc2 = p.dram("xc2", [CL, D], F32, kind=dk)
    xl3 = p.dram("xl3", [S, D], F32, kind=dk)

    def tile_ap(lat, ctx, t):
        if t < 2:
            return ctx[t * 128:(t + 1) * 128, :]
        return lat[(t - 2) * 128:(t - 1) * 128, :]

    idt = p.sb("idt", [128, 128], F32)
    idb = p.sb("idb", [128, 128], BF16)
    ones_f = p.sb("ones_f", [128, 128], F32)
    p.sync.dma_start(out=idt[:], in_=ident_in)
    p.vector.tensor_copy(out=idb[:], in_=idt[:])
    p.vector.memset(ap=ones_f[:], constant=1.0)

    ps_all = PsumPool(p, list(range(8)))

    colT = p.sb("colT", [128, 96], F32)
    cs = p.sb("cs", [128, 8, 2], F32)
    modcol = p.sb("modcol", [128, 48, 2], F32)
    gmod1 = p.sb("gmod1", [128, 8, 2], F32)
    gmod2 = p.sb("gmod2", [128, 8, 2], F32)
    gabc = p.sb("gabc", [128, 4, D], F32)
    fng_bc = p.sb("fng_bc", [128, D], F32)
    p.sync.dma_start(out=fng_bc[:], in_=fng.partition_broadcast(128))
    small = p.pool("small", [128, 16], F32, bufs=8)
    junk = p.pool("junk", [128, D], BF16, bufs=2)

    def mod_stage(l):
      with p.scope():
        stg = p.sb(f"stg{l}", [96, 128], F32)
        p.sync.dma_start(out=stg[0:48, :], in_=mod_b[l])
        p.sync.dma_start(out=stg[48:56, :], in_=c_in)
        p.sync.dma_start(out=stg[56:64, :], in_=cc_in)
        p.sync.dma_start(out=stg[64:72, :], in_=n1g[l])
        p.sync.dma_start(out=stg[72:80, :], in_=n2g[l])
        pt = ps_all.next()
        p.tensor.transpose(out=pt[:, 0:80], in_=stg[0:80, :], identity=idt[0:80, 0:80])
        p.vector.tensor_copy(out=colT[:, 0:80], in_=pt[:, 0:80])
        p.scalar.activation(out=cs[:, :, 0], in_=colT[:, 48:56], func=AF.Silu)
        p.scalar.activation(out=cs[:, :, 1], in_=colT[:, 56:64], func=AF.Silu)
        wm = p.pool(f"wm{l}", [128, 8, 512], F32, bufs=2)
        pm = ps_all.next()
        for pn in range(12):
            w = wm.next()
            src = mod_w[l, :, pn * 512:(pn + 1) * 512].rearrange("(kc p) n -> p kc n", p=128)
            p.sync.dma_start(out=w[:, 0:4, :], in_=src[:, 0:4, :])
            p.sync.dma_start(out=w[:, 4:8, :], in_=src[:, 4:8, :])
            for jj in range(4):
                j = pn * 4 + jj
                for kc in range(8):
                    p.tensor.matmul(out=pm[:, 2 * j:2 * j + 2], lhsT=w[:, kc, jj * 128:(jj + 1) * 128], rhs=cs[:, kc, :],
                                    start=(kc == 0), stop=(kc == 7))
        p.vector.tensor_tensor(out=modcol[:], in0=fap(pm[:, 0:1], [[2, 48], [1, 2]]),
                               in1=fap(colT[:, 0:1], [[1, 48], [0, 2]]), op=ALU.add)
        p.vector.scalar_tensor_tensor(out=gmod1[:], in0=modcol[:, 8:16, :], scalar=1.0,
                                      in1=fap(colT[:, 64:65], [[1, 8], [0, 2]]), op0=ALU.add, op1=ALU.mult)
        p.vector.scalar_tensor_tensor(out=gmod2[:], in0=modcol[:, 32:40, :], scalar=1.0,
                                      in1=fap(colT[:, 72:73], [[1, 8], [0, 2]]), op0=ALU.add, op1=ALU.mult)
        dg = p.pool(f"dg{l}", [128, 128], F32, bufs=2)
        for vi, v in enumerate((2, 5)):
            for w_ in range(2):
                for half in range(2):
                    pb = ps_all.next()
                    for q in range(4):
                        kc = half * 4 + q
                        d_ = dg.next()
                        p.vector.tensor_scalar(out=d_[:], in0=idt[:], scalar1=modcol[:, v * 8 + kc, w_:w_ + 1],
                                               scalar2=None, op0=ALU.mult)
                        p.tensor.matmul(out=pb[:, q * 128:(q + 1) * 128], lhsT=ones_f[:], rhs=d_[:],
                                        start=True, stop=True)
                    p.scalar.copy(out=gabc[:, vi * 2 + w_, half * 512:(half + 1) * 512], in_=pb[:, :])

    xpool = p.pool("xpool", [128, D], F32, bufs=3)
    xspool = p.pool("xspool", [128, D], F32, bufs=2)

    def norm_tile(src_ap, gmod, shift_v, w_, out_fn):
        NSTOP = int(os.environ.get("NSTOP", "99"))
        xt = xpool.next()
        p.sync.dma_start(out=xt[:], in_=src_ap)
        jk = junk.next()
        sm = small.next()
        if NSTOP < 1:
            return xt
        p.scalar.activation(out=jk[:], in_=xt[:], func=AF.Square, accum_out=sm[:, 0:1])
        if NSTOP < 2:
            return xt
        rstd_from_ss(p, sm[:, 2:3], sm[:, 0:1], D, sm[:, 1:2])
        if NSTOP < 3:
            return xt
        xs = xspool.next()
        p.vector.tensor_scalar(out=xs[:], in0=xt[:], scalar1=sm[:, 2:3], scalar2=None, op0=ALU.mult)
        if NSTOP < 4:
            return xt
        for half in range(2):
            pt = ps_all.next()
            for q in range(4):
                kc = half * 4 + q
                p.tensor.transpose(out=pt[:, q * 128:(q + 1) * 128], in_=xs[:, kc * 128:(kc + 1) * 128], identity=idt[:])
            if NSTOP < 5:
                continue
            for q in range(4):
                kc = half * 4 + q
                AV = os.environ.get("AV", "0")
                if AV == "0":
                    p.scalar.activation(out=out_fn(kc), in_=pt[:, q * 128:(q + 1) * 128], func=AF.Identity,
                                        bias=modcol[:, shift_v * 8 + kc, w_:w_ + 1], scale=gmod[:, kc, w_:w_ + 1])
                elif AV == "1":
                    p.scalar.activation(out=out_fn(kc), in_=pt[:, q * 128:(q + 1) * 128], func=AF.Identity,
                                        scale=gmod[:, kc, w_:w_ + 1])
                elif AV == "2":
                    p.scalar.activation(out=out_fn(kc), in_=pt[:, q * 128:(q + 1) * 128], func=AF.Identity,
                                        bias=modcol[:, shift_v * 8 + kc, w_:w_ + 1])
                elif AV == "3":
                    p.scalar.activation(out=out_fn(kc), in_=pt[:, q * 128:(q + 1) * 128], func=AF.Copy)
                elif AV == "4":
                    p.vector.tensor_scalar(out=out_fn(kc), in0=pt[:, q * 128:(q + 1) * 128], scalar1=gmod[:, kc, w_:w_ + 1],
                                           scalar2=modcol[:, shift_v * 8 + kc, w_:w_ + 1], op0=ALU.mult, op1=ALU.add)
        k.last_xs = xs
        return xt

    blocks = [(0, 2)] + [(2 + 4 * i, 4) for i in range(8)]
    slT_d = p.dram("slT_d", [4, 128, NT * 128], BF16, kind=dk)
    oT_d = p.dram("oT_d", [4, 128, NT * 128], BF16, kind=dk)
    L0STOP = os.environ.get("L0STOP", "")

    def layer0_mixer():
      with p.scope():
        qT = p.sb("qT", [128, 4, NT * 128], BF16)
        kT = p.sb("kT", [128, NT * 128], BF16)
        va0 = p.sb("va0", [128, NT, 128], BF16)
        va1 = p.sb("va1", [128, NT, 128], BF16)
        p.gpsimd.memset(ap=va0[:, :, 64:128], constant=1.0)
        p.gpsimd.memset(ap=va1[:, :, 0:64], constant=1.0)
        with p.scope():
            win = p.sb("win", [128, 8, 1792], BF16)
            for kc in range(8):
                for w_ in range(2):
                    p.gpsimd.dma_start(out=fap(win[:, kc, w_ * 64:w_ * 64 + 1], [[128, 4], [1, 64]]),
                                       in_=ab_w_in[kc * 128:(kc + 1) * 128, w_ * 256:(w_ + 1) * 256].rearrange("p (j d) -> p j d", j=4))
                p.gpsimd.dma_start(out=win[:, kc, 512:1792], in_=ab_w_in[kc * 128:(kc + 1) * 128, 512:1792])
            gq = p.sb("gq", [128, 64], F32)
            gk = p.sb("gk", [128, 64], F32)
            lng = p.sb("lng", [128, 512], F32)
            lnb = p.sb("lnb", [128, 512], F32)
            bsb = p.sb("bsb", [128, 512], F32)
            p.sync.dma_start(out=gq[:], in_=ab_qn.partition_broadcast(128))
            p.sync.dma_start(out=gk[:], in_=ab_kn.partition_broadcast(128))
            p.sync.dma_start(out=lng[:], in_=gm_lng.partition_broadcast(128))
            p.sync.dma_start(out=lnb[:], in_=gm_lnb.partition_broadcast(128))
            p.sync.dma_start(out=bsb[:], in_=gm_bs.partition_broadcast(128))
            wsT = p.sb("wsT", [128, 4, 128], BF16)
            wsf = p.sb("wsf", [128, 4, 128], F32)
            p.sync.dma_start(out=wsf[:], in_=gm_ws.rearrange("g p q -> p g q"))
            pw = ps_all.next()
            for g in range(4):
                p.tensor.transpose(out=pw[:, g * 128:(g + 1) * 128], in_=wsf[:, g, :], identity=idt[:])
            p.vector.tensor_copy(out=wsT[:], in_=pw[:, :].rearrange("p (g q) -> p g q", g=4))

            hTp = p.pool("hTp", [128, 8, 512], BF16, bufs=2)
            guTp = p.pool("guTp", [128, 4, 512], BF16, bufs=2)
            sqq = p.pool("sqq", [128, 640], F32, bufs=1)
            qnp = p.pool("qnp", [128, 640], F32, bufs=2)
            qrp = p.pool("qrp", [128, 640], BF16, bufs=2)
            rt = p.pool("rt", [128, 4, 320], F32, bufs=1)
            csp = p.pool("csp", [128, 64], F32, bufs=2)
            gvp = p.pool("gvp", [128, 512], F32, bufs=2)
            vnp = p.pool("vnp", [128, 512], F32, bufs=1)
            vnbp = p.pool("vnbp", [128, 512], BF16, bufs=2)
            s1p = p.pool("s1p", [128, 512], F32, bufs=1)
            slp = p.pool("slp", [128, 4, 128], BF16, bufs=2)

            if L0STOP == "S":
                k.dbg("d_win", win[:], [128, 8, 1792], BF16)
                k.dbg("d_wsT", wsT[:], [128, 4, 128], BF16)
                return
            BLIM = int(os.environ.get("BLIM", "99"))
            SUB = int(os.environ.get("SUB", "99"))
            def sb_s1(t0, ntl):
                N = ntl * 128
                hT = hTp.next()
                for ti in range(ntl):
                    t = t0 + ti
                    w_ = 1 if t < 2 else 0
                    norm_tile(tile_ap(x_in, ctx_in, t), gmod1, 0, w_,
                              lambda kc, hT=hT, ti=ti: hT[:, kc, ti * 128:(ti + 1) * 128])
                guT = guTp.next()
                for g in range(4):
                    pu = ps_all.next()
                    for kc in range(8):
                        p.tensor.matmul(out=pu[:, 0:N], lhsT=win[:, kc, 768 + g * 128:768 + (g + 1) * 128],
                                        rhs=hT[:, kc, 0:N], start=(kc == 0), stop=(kc == 7))
                    p.scalar.activation(out=guT[:, g, 0:N], in_=pu[:, 0:N], func=AF.Gelu)
                return (t0, ntl, hT, guT)

            def sb_s2(t0, ntl, hT, guT):
                N = ntl * 128
                for ti in range(ntl):
                    t = t0 + ti
                    tok = t * 128
                    lat = t >= 2
                    pq = ps_all.next()
                    pkv = ps_all.next()
                    pvg = ps_all.next()
                    for (pp, c0, c1) in ((pq, 0, 512), (pkv, 512, 768), (pvg, 1280, 1792)):
                        for kc in range(8):
                            p.tensor.matmul(out=pp[:, 0:c1 - c0], lhsT=hT[:, kc, ti * 128:(ti + 1) * 128],
                                            rhs=win[:, kc, c0:c1], start=(kc == 0), stop=(kc == 7))
                    if SUB < 3:
                        continue
                    sq = sqq.next()
                    sm = small.next()
                    p.scalar.activation(out=sq[:, 0:512], in_=pq[:, 0:512], func=AF.Square)
                    p.scalar.activation(out=sq[:, 512:640], in_=pkv[:, 0:128], func=AF.Square)
                    p.vector.tensor_reduce(out=sm[:, 0:10], in_=sq[:].rearrange("p (h d) -> p h d", d=64),
                                           axis=AX.X, op=ALU.add)
                    sm2 = small.next()
                    rstd_from_ss(p, sm2[:, 0:10], sm[:, 0:10], 64, sm[:, 0:10])
                    qn = qnp.next()
                    p.vector.tensor_tensor(out=qn[:, 0:512].rearrange("p (h d) -> p h d", d=64),
                                           in0=pq[:, 0:512].rearrange("p (h d) -> p h d", d=64),
                                           in1=fap(sm2[:, 0:1], [[1, 8], [0, 64]]), op=ALU.mult)
                    p.vector.tensor_tensor(out=qn[:, 512:640].rearrange("p (h d) -> p h d", d=64),
                                           in0=pkv[:, 0:128].rearrange("p (h d) -> p h d", d=64),
                                           in1=fap(sm2[:, 8:9], [[1, 2], [0, 64]]), op=ALU.mult)
                    p.gpsimd.tensor_tensor(out=qn[:, 0:512].rearrange("p (h d) -> p h d", d=64),
                                           in0=qn[:, 0:512].rearrange("p (h d) -> p h d", d=64),
                                           in1=fap(gq[:, 0:1], [[0, 8], [1, 64]]), op=ALU.mult)
                    p.gpsimd.tensor_tensor(out=qn[:, 512:640].rearrange("p (h d) -> p h d", d=64),
                                           in0=qn[:, 512:640].rearrange("p (h d) -> p h d", d=64),
                                           in1=fap(gk[:, 0:1], [[0, 2], [1, 64]]), op=ALU.mult)
                    if SUB < 4:
                        continue
                    qr = qrp.next()
                    if lat:
                        cst = csp.next()
                        p.sync.dma_start(out=cst[:, 0:32], in_=cos_in[(t - 2) * 128:(t - 1) * 128, :])
                        p.sync.dma_start(out=cst[:, 32:64], in_=sin_in[(t - 2) * 128:(t - 1) * 128, :])
                        dims = [[64, 10], [32, 2], [1, 16]]
                        x1 = fap(qn[:, 0:1], dims)
                        x2 = fap(qn[:, 16:17], dims)
                        o1 = fap(qr[:, 0:1], dims)
                        o2 = fap(qr[:, 16:17], dims)
                        cb = fap(cst[:, 0:1], [[0, 10], [16, 2], [1, 16]])
                        sb_ = fap(cst[:, 32:33], [[0, 10], [16, 2], [1, 16]])
                        r = rt.next()
                        tv = lambda i: fap(r[:, i, 0:1], [[32, 10], [16, 2], [1, 16]])
                        p.vector.tensor_tensor(out=tv(0), in0=x1, in1=cb, op=ALU.mult)
                        p.gpsimd.tensor_tensor(out=tv(1), in0=x2, in1=sb_, op=ALU.mult)
                        p.vector.tensor_tensor(out=tv(2), in0=x1, in1=sb_, op=ALU.mult)
                        p.gpsimd.tensor_tensor(out=tv(3), in0=x2, in1=cb, op=ALU.mult)
                        p.vector.tensor_tensor(out=o1, in0=tv(0), in1=tv(1), op=ALU.subtract)
                        p.gpsimd.tensor_tensor(out=o2, in0=tv(3), in1=tv(2), op=ALU.add)
                    else:
                        p.vector.tensor_copy(out=qr[:], in_=qn[:])
                    if SUB < 5:
                        continue
                    ptq = ps_all.next(BF16)
                    for j in range(4):
                        p.tensor.transpose(out=ptq[:, j * 128:(j + 1) * 128],
                                           in_=qr[:, j * 128:(j + 1) * 128], identity=idb[:])
                    p.tensor.transpose(out=ptq[:, 512:640], in_=qr[:, 512:640], identity=idb[:])
                    p.vector.tensor_copy(out=qT[:, :, tok:tok + 128],
                                         in_=ptq[:, 0:512].rearrange("p (j q) -> p j q", j=4))
                    p.vector.tensor_copy(out=kT[:, tok:tok + 128], in_=ptq[:, 512:640])
                    k.precast_step(3)
                    p.scalar.copy(out=va0[:, t, 0:64], in_=pkv[:, 128:192])
                    p.scalar.copy(out=va1[:, t, 64:128], in_=pkv[:, 192:256])
                    if SUB < 6:
                        continue
                    gv = gvp.next()
                    sm3 = small.next()
                    p.scalar.activation(out=gv[:], in_=pvg[:, :], func=AF.Gelu, accum_out=sm3[:, 0:1])
                    p.vector.tensor_scalar(out=sm3[:, 1:2], in0=sm3[:, 0:1], scalar1=-1.0 / 512, scalar2=None, op0=ALU.mult)
                    jk = junk.next()
                    p.scalar.activation(out=jk[:, 0:512], in_=gv[:], func=AF.Square, bias=sm3[:, 1:2], scale=1.0,
                                        accum_out=sm3[:, 2:3])
                    rstd_from_ss(p, sm3[:, 4:5], sm3[:, 2:3], 512, sm3[:, 3:4])
                    vn = vnp.next()
                    p.vector.tensor_scalar(out=vn[:], in0=gv[:], scalar1=sm3[:, 1:2], scalar2=sm3[:, 4:5],
                                           op0=ALU.add, op1=ALU.mult)
                    p.gpsimd.tensor_tensor(out=vn[:], in0=vn[:], in1=lng[:], op=ALU.mult)
                    vnb = vnbp.next()
                    p.gpsimd.tensor_tensor(out=vnb[:], in0=vn[:], in1=lnb[:], op=ALU.add)
                    if SUB < 7:
                        continue
                    psp = ps_all.next()
                    for g in range(4):
                        p.tensor.matmul(out=psp[:, g * 128:(g + 1) * 128], lhsT=vnb[:, g * 128:(g + 1) * 128],
                                        rhs=wsT[:, g, :], start=True, stop=True)
                    s1 = s1p.next()
                    p.vector.tensor_tensor(out=s1[:], in0=psp[:, :], in1=bsb[:], op=ALU.add)
                    sl = slp.next()
                    p.gpsimd.tensor_tensor(out=sl[:], in0=s1[:].rearrange("p (g q) -> p g q", g=4),
                                           in1=guT[:, :, ti * 128:(ti + 1) * 128], op=ALU.mult)
                    p.sync.dma_start(out=slT_d[:, :, tok:tok + 128].rearrange("g c t -> c g t"), in_=sl[:])

            pend = None
            for (t0, ntl) in blocks:
                cur = sb_s1(t0, ntl)
                if pend is not None:
                    sb_s2(*pend)
                pend = cur
            sb_s2(*pend)

        if L0STOP == "B":
            k.dbg("d_qT", qT[:], [128, 4, NT * 128], BF16)
            k.dbg("d_kT", kT[:], [128, NT * 128], BF16)
            k.dbg("d_va0", va0[:], [128, NT, 128], BF16)
            k.dbg("d_va1", va1[:], [128, NT, 128], BF16)
            return
        with p.scope():
            ps_s = PsumPool(p, [0, 1, 2, 3, 4, 5])
            ps_acc = PsumPool(p, [6, 7])
            ppool = p.pool("ppool", [128, 512], BF16, bufs=6)
            rdp = p.pool("rdp", [128, 512], F32, bufs=2)
            otp = p.pool("otp", [128, 512], BF16, bufs=2)
            vas = (va0, va1)
            qblocks = [(0, 256, [0, 1])] + [(256 + 512 * i, 512, list(range(NT))) for i in range(8)]
            items = []
            for j in range(4):
                for (q0, N, kts) in qblocks:
                    for w_ in range(2):
                        for ki, kt in enumerate(kts):
                            items.append((j, q0, N, w_, kt, ki == 0, ki == len(kts) - 1))
            LA = 3
            pTs = {}
            state = {}
            qpads = []
            for w_ in range(2):
                qp = p.pool(f"qpad{w_}", [128, 512], BF16, bufs=2)
                for t_ in qp.ts:
                    p.gpsimd.memset(ap=t_[:], constant=0.0)
                qpads.append(qp)
            for idx in range(len(items) + LA):
                if idx < len(items):
                    (j, q0, N, w_, kt, first, last) = items[idx]
                    b0 = 64 * w_
                    if first:
                        state["qp"] = qpads[w_].next()
                        p.gpsimd.tensor_copy(out=state["qp"][b0:b0 + 64, 0:N], in_=qT[b0:b0 + 64, j, q0:q0 + N])
                    ps = ps_s.next()
                    p.tensor.matmul(out=ps[:, 0:N], lhsT=kT[:, kt * 128:(kt + 1) * 128],
                                    rhs=state["qp"][:, 0:N], start=True, stop=True)
                    pT = ppool.next()
                    p.scalar.activation(out=pT[:, 0:N], in_=ps[:, 0:N], func=AF.Exp, scale=0.125)
                    pTs[idx] = pT
                if idx >= LA:
                    (j, q0, N, w_, kt, first, last) = items[idx - LA]
                    pT = pTs.pop(idx - LA)
                    if first:
                        state["acc"] = ps_acc.next()
                        if w_ == 0:
                            state["ot"] = otp.next()
                    acc = state["acc"]
                    ot = state["ot"]
                    p.tensor.matmul(out=acc[:, 0:N], lhsT=vas[w_][:, kt, :], rhs=pT[:, 0:N], start=first, stop=last)
                    if last:
                        ob, db = (0, 64) if w_ == 0 else (64, 0)
                        rd = rdp.next()
                        p.vector.reciprocal(out=rd[ob:ob + 64, 0:N], in_=acc[db:db + 64, 0:N])
                        p.vector.tensor_tensor(out=ot[ob:ob + 64, 0:N], in0=acc[ob:ob + 64, 0:N],
                                               in1=rd[ob:ob + 64, 0:N], op=ALU.mult)
                        if w_ == 1:
                            p.sync.dma_start(out=oT_d[j, :, q0:q0 + N], in_=ot[:, 0:N])

      if L0STOP in ("B", "C"):
          return
      with p.scope():
        wo = p.sb("wo", [128, 8, D], BF16)
        for c_ in range(8):
            if c_ < 4:
                for w_ in range(2):
                    r0 = (c_ + 4 * w_) * 64
                    p.gpsimd.dma_start(out=wo[64 * w_:64 * w_ + 64, c_, :], in_=ab_w_out[r0:r0 + 64, :])
            else:
                r0 = 512 + (c_ - 4) * 128
                p.gpsimd.dma_start(out=wo[:, c_, :], in_=ab_w_out[r0:r0 + 128, :])
        tmpp = p.pool("tmpp", [128, D], F32, bufs=2)
        ylp = p.pool("ylp", [128, 8, 512], BF16, bufs=2)
        def sd_s1(t0, ntl):
            N = ntl * 128
            yl = ylp.next()
            p.sync.dma_start(out=yl[:, 0:4, 0:N], in_=oT_d[:, :, t0 * 128:t0 * 128 + N].rearrange("j c t -> c j t"))
            p.sync.dma_start(out=yl[:, 4:8, 0:N], in_=slT_d[:, :, t0 * 128:t0 * 128 + N].rearrange("j c t -> c j t"))
            xts = []
            for ti in range(ntl):
                xt = xp4.next()
                p.sync.dma_start(out=xt[:], in_=tile_ap(x_in, ctx_in, t0 + ti))
                xts.append(xt)
            return (t0, ntl, yl, xts)

        def sd_s2(t0, ntl, yl, xts):
            for ti in range(ntl):
                t = t0 + ti
                w_ = 1 if t < 2 else 0
                xt = xts[ti]
                tm = tmpp.next()
                for half in range(2):
                    pm = ps_all.next()
                    for c_ in range(8):
                        p.tensor.matmul(out=pm[:, :], lhsT=yl[:, c_, ti * 128:(ti + 1) * 128],
                                        rhs=wo[:, c_, half * 512:(half + 1) * 512], start=(c_ == 0), stop=(c_ == 7))
                    p.vector.tensor_tensor(out=tm[:, half * 512:(half + 1) * 512], in0=pm[:, :],
                                           in1=gabc[:, w_, half * 512:(half + 1) * 512], op=ALU.mult)
                p.gpsimd.tensor_tensor(out=tm[:], in0=tm[:], in1=xt[:], op=ALU.add)
                p.sync.dma_start(out=tile_ap(xl1, xc1, t), in_=tm[:])

        xp4 = p.pool("xp4", [128, D], F32, bufs=8)
        pend = None
        for (t0, ntl) in blocks:
            cur = sd_s1(t0, ntl)
            if pend is not None:
                sd_s2(*pend)
            pend = cur
        sd_s2(*pend)

    def moe(l, src, dst, tiles, final=False):
      with p.scope():
        wr = p.sb(f"wr{l}", [128, 8, NE], F32)
        p.sync.dma_start(out=wr[:], in_=router_w.rearrange("(kc p) e -> p kc e", p=128))
        rb = p.sb(f"rb{l}", [128, NE], F32)
        p.sync.dma_start(out=rb[:], in_=router_b.partition_broadcast(128))
        sbs = []
        cur = []
        for t in tiles:
            cur.append(t)
            if len(cur) == 12 or (len(cur) == 10 and cur[0] == 0):
                sbs.append(cur)
                cur = []
        if cur:
            sbs.append(cur)
        h2T = p.sb(f"h2T{l}", [128, 8, 12 * 128], BF16)
        acc = p.sb(f"macc{l}", [128, 12, D], F32)
        comb = p.sb(f"comb{l}", [128, 12, NE], F32)
        hf = p.pool(f"hf{l}", [128, 8, 128], F32, bufs=2)
        wgp = p.pool(f"wgp{l}", [128, 8, FF], BF16, bufs=2)
        wup = p.pool(f"wup{l}", [128, 8, FF], BF16, bufs=2)
        wdp = p.pool(f"wdp{l}", [128, 4, D], BF16, bufs=2)
        aTp = p.pool(f"aTp{l}", [128, 4, 512], BF16, bufs=2)
        sgp = p.pool(f"sgp{l}", [128, 512], BF16, bufs=3)
        rsm = p.pool(f"rsm{l}", [128, 8, 16], F32, bufs=2)
        for sbt in sbs:
            nloc = len(sbt)
            for tl, t in enumerate(sbt):
                w_ = 1 if t < 2 else 0
                h = hf.next()
                norm_tile(tile_ap(src[0], src[1], t), gmod2, 3, w_, lambda kc, h=h: h[:, kc, :])
                p.vector.tensor_copy(out=h2T[:, :, tl * 128:(tl + 1) * 128], in_=h[:])
                pl = ps_all.next()
                for kc in range(8):
                    p.tensor.matmul(out=pl[:, 0:NE], lhsT=h[:, kc, :], rhs=wr[:, kc, :], start=(kc == 0), stop=(kc == 7))
                r = rsm.next()
                sc = r[:, 0, :]
                sel = r[:, 1, :]
                sel4 = fap(r[:, 1, 0:1], [[4, 4], [1, 4]])
                p.scalar.activation(out=sc, in_=pl[:, 0:NE], func=AF.Sigmoid)
                p.vector.tensor_tensor(out=sel, in0=sc, in1=rb[:], op=ALU.add)
                m1 = r[:, 2, 0:4]
                m2 = r[:, 2, 4:8]
                gs = r[:, 2, 8:12]
                gmax = r[:, 2, 12:13]
                p.vector.tensor_reduce(out=m1, in_=sel4, axis=AX.X, op=ALU.max)
                eq = fap(r[:, 3, 0:1], [[4, 4], [1, 4]])
                p.vector.tensor_tensor(out=eq, in0=sel4, in1=fap(r[:, 2, 0:1], [[1, 4], [0, 4]]), op=ALU.is_equal)
                s2 = fap(r[:, 4, 0:1], [[4, 4], [1, 4]])
                p.vector.scalar_tensor_tensor(out=s2, in0=eq, scalar=-1e9, in1=sel4, op0=ALU.mult, op1=ALU.add)
                p.vector.tensor_reduce(out=m2, in_=s2, axis=AX.X, op=ALU.max)
                p.vector.tensor_tensor(out=gs, in0=m1, in1=m2, op=ALU.add)
                p.vector.tensor_reduce(out=gmax, in_=gs, axis=AX.X, op=ALU.max)
                ing = r[:, 5, 0:4]
                p.vector.tensor_scalar(out=ing, in0=gs, scalar1=gmax, scalar2=None, op0=ALU.is_equal)
                ge = fap(r[:, 6, 0:1], [[4, 4], [1, 4]])
                p.vector.tensor_tensor(out=ge, in0=sel4, in1=fap(r[:, 2, 4:5], [[1, 4], [0, 4]]), op=ALU.is_ge)
                p.vector.tensor_tensor(out=ge, in0=ge, in1=fap(r[:, 5, 0:1], [[1, 4], [0, 4]]), op=ALU.mult)
                wts = r[:, 7, :]
                p.vector.tensor_tensor(out=wts, in0=r[:, 6, :], in1=sc, op=ALU.mult)
                den = r[:, 5, 4:5]
                p.vector.tensor_reduce(out=den, in_=wts, axis=AX.X, op=ALU.add)
                rden = r[:, 5, 5:6]
                p.vector.reciprocal(out=rden, in_=den)
                p.vector.tensor_scalar(out=comb[:, tl, :], in0=wts, scalar1=rden, scalar2=None, op0=ALU.mult)
            lblocks = []
            i = 0
            while i < nloc:
                n = 2 if sbt[i] == 0 else min(4, nloc - i)
                lblocks.append((i, n))
                i += n
            for e in range(NE):
                wg = wgp.next()
                wu = wup.next()
                wd = wdp.next()
                p.gpsimd.dma_start(out=wg[:], in_=w_gate[l, e].rearrange("(kc p) f -> p kc f", p=128))
                p.gpsimd.dma_start(out=wu[:], in_=w_up[l, e].rearrange("(kc p) f -> p kc f", p=128))
                p.gpsimd.dma_start(out=wd[:], in_=w_down[l, e].rearrange("(f p) n -> p f n", p=128))
                for (i0, n) in lblocks:
                    N = n * 128
                    c0 = i0 * 128
                    aT = aTp.next()
                    for f in range(4):
                        pg = ps_all.next()
                        pu = ps_all.next()
                        for kc in range(8):
                            p.tensor.matmul(out=pg[:, 0:N], lhsT=wg[:, kc, f * 128:(f + 1) * 128],
                                            rhs=h2T[:, kc, c0:c0 + N], start=(kc == 0), stop=(kc == 7))
                        for kc in range(8):
                            p.tensor.matmul(out=pu[:, 0:N], lhsT=wu[:, kc, f * 128:(f + 1) * 128],
                                            rhs=h2T[:, kc, c0:c0 + N], start=(kc == 0), stop=(kc == 7))
                        sg = sgp.next()
                        p.scalar.activation(out=sg[:, 0:N], in_=pg[:, 0:N], func=AF.Silu)
                        p.vector.tensor_tensor(out=aT[:, f, 0:N], in0=sg[:, 0:N], in1=pu[:, 0:N], op=ALU.mult)
                    for ti in range(n):
                        tl = i0 + ti
                        for half in range(2):
                            pd = ps_all.next()
                            for f in range(4):
                                p.tensor.matmul(out=pd[:, :], lhsT=aT[:, f, ti * 128:(ti + 1) * 128],
                                                rhs=wd[:, f, half * 512:(half + 1) * 512], start=(f == 0), stop=(f == 3))
                            a_ = acc[:, tl, half * 512:(half + 1) * 512]
                            if e == 0:
                                p.vector.tensor_scalar(out=a_, in0=pd[:, :], scalar1=comb[:, tl, e:e + 1], scalar2=None,
                                                       op0=ALU.mult)
                            else:
                                p.vector.scalar_tensor_tensor(out=a_, in0=pd[:, :], scalar=comb[:, tl, e:e + 1], in1=a_,
                                                              op0=ALU.mult, op1=ALU.add)
            for tl, t in enumerate(sbt):
                w_ = 1 if t < 2 else 0
                xt = xpool.next()
                p.sync.dma_start(out=xt[:], in_=tile_ap(src[0], src[1], t))
                tm = xspool.next()
                p.vector.tensor_tensor(out=tm[:], in0=acc[:, tl, :], in1=gabc[:, 2 + w_, :], op=ALU.mult)
                p.vector.tensor_tensor(out=tm[:], in0=tm[:], in1=xt[:], op=ALU.add)
                if final:
                    jk = junk.next()
                    sm = small.next()
                    p.scalar.activation(out=jk[:], in_=tm[:], func=AF.Square, accum_out=sm[:, 0:1])
                    rstd_from_ss(p, sm[:, 2:3], sm[:, 0:1], D, sm[:, 1:2])
                    p.vector.scalar_tensor_tensor(out=tm[:], in0=tm[:], scalar=sm[:, 2:3], in1=fng_bc[:],
                                                  op0=ALU.mult, op1=ALU.mult)
                p.sync.dma_start(out=tile_ap(dst[0], dst[1], t), in_=tm[:])


    I32 = mybir.dt.int32
    su_in = IN("su_mat", [128, 128])
    thr_in = IN("thr512", [1, 40])
    base8_in = IN("base8", [128, 8])
    base4_in = IN("base4", [128, 4])
    zeros_in = IN("zeros_bf", [512, D], BF16)

    wgb = p.dram("wgb", [2 * NE * D, FF], BF16)
    wub = p.dram("wub", [2 * NE * D, FF], BF16)
    wdb = p.dram("wdb", [2 * NE * FF, D], BF16)

    def precast_weights():
        wg_f = w_gate.rearrange("l e k f -> (l e k) f")
        wu_f = w_up.rearrange("l e k f -> (l e k) f")
        wd_f = w_down.rearrange("l e k f -> (l e k) f")
        for l in range(2):
            for e in range(NE):
                r0 = (l * NE + e) * D
                p.gpsimd.dma_start(out=wgb[r0:r0 + D, :], in_=wg_f[r0:r0 + D, :])
                yield
                p.gpsimd.dma_start(out=wub[r0:r0 + D, :], in_=wu_f[r0:r0 + D, :])
                yield
                r1 = (l * NE + e) * FF
                p.gpsimd.dma_start(out=wdb[r1:r1 + FF, :], in_=wd_f[r1:r1 + FF, :])
                yield
    k.precast_weights = precast_weights
    k.precast_iter = None

    def precast_step(n):
        if k.precast_iter is None:
            return
        for _ in range(n):
            try:
                next(k.precast_iter)
            except StopIteration:
                k.precast_iter = None
                return
    k.precast_step = precast_step

    def moe_sparse(l, src, dst, tiles, final=False):
      NTL = len(tiles)
      ntok = NTL * 128
      NST = (2 * ntok) // 512 + 16
      NSLOT = NST * 512
      Xs = p.dram(f"Xs{l}", [NSLOT, D], BF16)
      Ys = p.dram(f"Ys{l}", [NSLOT, D], BF16)
      H2 = p.dram(f"H2{l}", [ntok, D], BF16)
      wd_tab = wdb
      IOA = bass.IndirectOffsetOnAxis
      with p.scope():
        comb_all = p.sb("comb_all", [128, NTL, NE], F32)
        mask_all = p.sb("mask_all", [128, NTL, NE], F32)
        sc_all = p.sb("sc_all", [128, NTL, NE], F32)
        sl_f = p.sb("sl_f", [128, NTL, 2], F32)
        sl_i = p.sb("sl_i", [128, NTL, 2], I32)
        wl = p.sb("wl", [128, NTL, 2], F32)
        widx_i = p.sb("widx_i", [128, NST, 8], I32)
        didx_i = p.sb("didx_i", [128, NST, 4], I32)
        for i in range(NST):
            p.sync.dma_start(out=Xs[i * 512:(i + 1) * 512, :], in_=zeros_in)
        with p.scope():
            wr = p.sb("wr", [128, 8, NE], F32)
            p.sync.dma_start(out=wr[:], in_=router_w.rearrange("(kc p) e -> p kc e", p=128))
            rb = p.sb("rb", [128, NE], F32)
            p.sync.dma_start(out=rb[:], in_=router_b.partition_broadcast(128))
            su = p.sb("su", [128, 128], F32)
            p.sync.dma_start(out=su[:], in_=su_in)
            thr = p.sb("thr", [128, 40], F32)
            p.sync.dma_start(out=thr[:], in_=thr_in.partition_broadcast(128))
            b8 = p.sb("b8", [128, 8], F32)
            b4 = p.sb("b4", [128, 4], F32)
            p.sync.dma_start(out=b8[:], in_=base8_in)
            p.sync.dma_start(out=b4[:], in_=base4_in)
            mbc = p.sb("mbc", [128, 4, D], F32)
            dg = p.pool("dgm", [128, 128], F32, bufs=2)
            for vi in range(2):
                for w_ in range(2):
                    for half in range(2):
                        pb = ps_all.next()
                        for q in range(4):
                            kc = half * 4 + q
                            d_ = dg.next()
                            col = gmod2[:, kc, w_:w_ + 1] if vi == 0 else modcol[:, 24 + kc, w_:w_ + 1]
                            p.vector.tensor_scalar(out=d_[:], in0=idt[:], scalar1=col, scalar2=None, op0=ALU.mult)
                            p.tensor.matmul(out=pb[:, q * 128:(q + 1) * 128], lhsT=ones_f[:], rhs=d_[:], start=True, stop=True)
                        p.scalar.copy(out=mbc[:, vi * 2 + w_, half * 512:(half + 1) * 512], in_=pb[:, :])
            hf = p.pool("hf", [128, 8, 128], F32, bufs=2)
            rsm = p.pool("rsm", [128, 8, 16], F32, bufs=2)
            h2p = p.pool("h2p", [128, D], BF16, bufs=2)
            h2f = p.pool("h2f", [128, D], F32, bufs=1)
            xs3 = p.pool("xs3", [128, D], F32, bufs=3)

            def rt_s1(tl, t):
                w_ = 1 if t < 2 else 0
                xt = xpool.next()
                p.sync.dma_start(out=xt[:], in_=tile_ap(src[0], src[1], t))
                jk = junk.next()
                sm = small.next()
                p.scalar.activation(out=jk[:], in_=xt[:], func=AF.Square, accum_out=sm[:, 0:1])
                rstd_from_ss(p, sm[:, 2:3], sm[:, 0:1], D, sm[:, 1:2])
                xs = xs3.next()
                p.vector.tensor_scalar(out=xs[:], in0=xt[:], scalar1=sm[:, 2:3], scalar2=None, op0=ALU.mult)
                hf32 = h2f.next()
                p.vector.tensor_tensor(out=hf32[:], in0=xs[:], in1=mbc[:, w_, :], op=ALU.mult)
                h2t = h2p.next()
                p.gpsimd.tensor_tensor(out=h2t[:], in0=hf32[:], in1=mbc[:, 2 + w_, :], op=ALU.add)
                p.sync.dma_start(out=H2[tl * 128:(tl + 1) * 128, :], in_=h2t[:])
                return (tl, xs, w_)

            def rt_s2(tl, xs, w_):
                h = hf.next()
                for half in range(2):
                    pt = ps_all.next()
                    for q in range(4):
                        kc = half * 4 + q
                        p.tensor.transpose(out=pt[:, q * 128:(q + 1) * 128], in_=xs[:, kc * 128:(kc + 1) * 128], identity=idt[:])
                    for q in range(4):
                        kc = half * 4 + q
                        p.scalar.activation(out=h[:, kc, :], in_=pt[:, q * 128:(q + 1) * 128], func=AF.Identity,
                                            bias=modcol[:, 24 + kc, w_:w_ + 1], scale=gmod2[:, kc, w_:w_ + 1])
                pl = ps_all.next()
                for kc in range(8):
                    p.tensor.matmul(out=pl[:, 0:NE], lhsT=h[:, kc, :], rhs=wr[:, kc, :], start=(kc == 0), stop=(kc == 7))
                p.scalar.activation(out=sc_all[:, tl, :], in_=pl[:, 0:NE], func=AF.Sigmoid)

            pend = None
            for tl, t in enumerate(tiles):
                cur = rt_s1(tl, t)
                if pend is not None:
                    rt_s2(*pend)
                pend = cur
            rt_s2(*pend)
            G4 = NTL * 4
            T3 = lambda nm: p.sb(nm, [128, NTL, NE], F32)
            g4v = lambda t_: t_[:].rearrange("p t (g j) -> p (t g) j", j=4)
            sel = T3("sel")
            p.vector.tensor_tensor(out=sel[:], in0=sc_all[:], in1=fap(rb[:, 0:1], [[0, NTL], [1, NE]]), op=ALU.add)
            m1 = p.sb("m1", [128, G4], F32)
            m2 = p.sb("m2", [128, G4], F32)
            gs = p.sb("gs", [128, G4], F32)
            gmax = p.sb("gmax", [128, NTL], F32)
            p.vector.tensor_reduce(out=m1[:], in_=g4v(sel), axis=AX.X, op=ALU.max)
            eq = T3("eq")
            p.vector.tensor_tensor(out=g4v(eq), in0=g4v(sel), in1=fap(m1[:, 0:1], [[1, G4], [0, 4]]), op=ALU.is_equal)
            p.vector.scalar_tensor_tensor(out=eq[:], in0=eq[:], scalar=-1e9, in1=sel[:], op0=ALU.mult, op1=ALU.add)
            p.vector.tensor_reduce(out=m2[:], in_=g4v(eq), axis=AX.X, op=ALU.max)
            p.vector.tensor_tensor(out=gs[:], in0=m1[:], in1=m2[:], op=ALU.add)
            p.vector.tensor_reduce(out=gmax[:], in_=gs[:].rearrange("p (t g) -> p t g", g=4), axis=AX.X, op=ALU.max)
            ing = p.sb("ing", [128, G4], F32)
            p.vector.tensor_tensor(out=ing[:].rearrange("p (t g) -> p t g", g=4), in0=gs[:].rearrange("p (t g) -> p t g", g=4),
                                   in1=fap(gmax[:, 0:1], [[1, NTL], [0, 4]]), op=ALU.is_equal)
            p.vector.tensor_tensor(out=g4v(eq), in0=g4v(sel), in1=fap(m2[:, 0:1], [[1, G4], [0, 4]]), op=ALU.is_ge)
            p.vector.tensor_tensor(out=g4v(mask_all), in0=g4v(eq), in1=fap(ing[:, 0:1], [[1, G4], [0, 4]]), op=ALU.mult)
            wts = T3("wts")
            p.vector.tensor_tensor(out=wts[:], in0=mask_all[:], in1=sc_all[:], op=ALU.mult)
            den = p.sb("den", [128, NTL], F32)
            p.vector.tensor_reduce(out=den[:], in_=wts[:], axis=AX.X, op=ALU.add)
            p.vector.reciprocal(out=den[:], in_=den[:])
            p.vector.tensor_tensor(out=comb_all[:], in0=wts[:], in1=fap(den[:, 0:1], [[1, NTL], [0, NE]]), op=ALU.mult)
            ctile = T3("ctile")
            pos = T3("pos")
            mflat = mask_all[:].rearrange("p t e -> p (t e)")
            n0 = min(NTL, 32) * NE
            pcA = ps_all.next()
            p.tensor.matmul(out=pcA[:, 0:n0], lhsT=ones_f[:], rhs=mflat[:, 0:n0], start=True, stop=True)
            p.vector.tensor_copy(out=ctile[:].rearrange("p t e -> p (t e)")[:, 0:n0], in_=pcA[:, 0:n0])
            if NTL > 32:
                pcB = ps_all.next()
                p.tensor.matmul(out=pcB[:, 0:NTL * NE - n0], lhsT=ones_f[:], rhs=mflat[:, n0:NTL * NE], start=True, stop=True)
                p.vector.tensor_copy(out=ctile[:].rearrange("p t e -> p (t e)")[:, n0:NTL * NE], in_=pcB[:, 0:NTL * NE - n0])
            for t0_ in range(0, NTL, 32):
                t1_ = min(NTL, t0_ + 32)
                pp = ps_all.next()
                for tl in range(t0_, t1_):
                    p.tensor.matmul(out=pp[:, (tl - t0_) * NE:(tl - t0_ + 1) * NE], lhsT=su[:], rhs=mask_all[:, tl, :], start=True, stop=True)
                p.vector.tensor_copy(out=pos[:, t0_:t1_, :].rearrange("p t e -> p (t e)"), in_=pp[:, 0:(t1_ - t0_) * NE])
            cst = p.sb("cst", [128, 8, NE], F32)
            cnt = cst[:, 0, :]
            nt_ = cst[:, 1, :]
            p.vector.tensor_reduce(out=cnt, in_=ctile[:].rearrange("p t e -> p e t"), axis=AX.X, op=ALU.add)
            p.vector.tensor_scalar(out=nt_, in0=cnt, scalar1=0.0, scalar2=None, op0=ALU.is_gt)
            for kk in range(1, NST):
                p.vector.scalar_tensor_tensor(out=nt_, in0=cnt, scalar=512.0 * kk, in1=nt_, op0=ALU.is_gt, op1=ALU.add)
            pcn = cst[:, 2, :]
            p.vector.tensor_scalar(out=pcn, in0=nt_, scalar1=512.0, scalar2=None, op0=ALU.mult)
            a_, b_ = 3, 4
            p.vector.tensor_copy(out=cst[:, a_, :], in_=pcn)
            for sh in (1, 2, 4, 8):
                p.vector.tensor_copy(out=cst[:, b_, :], in_=cst[:, a_, :])
                p.vector.tensor_tensor(out=cst[:, b_, sh:NE], in0=cst[:, a_, sh:NE], in1=cst[:, a_, 0:NE - sh], op=ALU.add)
                a_, b_ = b_, a_
            seg_end = cst[:, a_, :]
            seg_start = cst[:, 5, :]
            p.vector.tensor_tensor(out=seg_start, in0=seg_end, in1=pcn, op=ALU.subtract)
            pa_, pb_ = T3("pfa"), T3("pfb")
            p.vector.tensor_copy(out=pa_[:], in_=ctile[:])
            sh = 1
            while sh < NTL:
                p.vector.tensor_copy(out=pb_[:], in_=pa_[:])
                p.vector.tensor_tensor(out=pb_[:, sh:NTL, :], in0=pa_[:, sh:NTL, :], in1=pa_[:, 0:NTL - sh, :], op=ALU.add)
                pa_, pb_ = pb_, pa_
                sh *= 2
            slot = T3("slot")
            p.vector.tensor_tensor(out=slot[:], in0=pa_[:], in1=ctile[:], op=ALU.subtract)
            p.vector.tensor_tensor(out=slot[:], in0=slot[:], in1=pos[:], op=ALU.add)
            p.vector.tensor_tensor(out=slot[:], in0=slot[:], in1=fap(seg_start[:, 0:1], [[0, NTL], [1, NE]]), op=ALU.add)
            mm1 = T3("mm1")
            p.vector.scalar_tensor_tensor(out=mm1[:], in0=slot[:], scalar=1.0, in1=mask_all[:], op0=ALU.add, op1=ALU.mult)
            shi1 = p.sb("shi1", [128, NTL], F32)
            p.vector.tensor_reduce(out=shi1[:], in_=mm1[:], axis=AX.X, op=ALU.max)
            mm2 = T3("mm2")
            p.vector.scalar_tensor_tensor(out=mm2[:], in0=mask_all[:], scalar=-1.0e6, in1=slot[:], op0=ALU.mult, op1=ALU.add)
            p.vector.tensor_scalar(out=mm2[:], in0=mm2[:], scalar1=1.0e6, scalar2=None, op0=ALU.add)
            slo = p.sb("slo", [128, NTL], F32)
            p.vector.tensor_reduce(out=slo[:], in_=mm2[:], axis=AX.X, op=ALU.min)
            p.vector.tensor_copy(out=sl_f[:, :, 0], in_=slo[:])
            p.vector.tensor_scalar(out=sl_f[:, :, 1], in0=shi1[:], scalar1=-1.0, scalar2=None, op0=ALU.add)
            p.vector.tensor_tensor(out=mm1[:], in0=mm1[:], in1=fap(shi1[:, 0:1], [[1, NTL], [0, NE]]), op=ALU.is_equal)
            p.vector.tensor_tensor(out=mm1[:], in0=mm1[:], in1=comb_all[:], op=ALU.mult)
            p.vector.tensor_reduce(out=wl[:, :, 1], in_=mm1[:], axis=AX.X, op=ALU.add)
            p.vector.tensor_tensor(out=mm2[:], in0=mm2[:], in1=fap(slo[:, 0:1], [[1, NTL], [0, NE]]), op=ALU.is_equal)
            p.vector.tensor_tensor(out=mm2[:], in0=mm2[:], in1=comb_all[:], op=ALU.mult)
            p.vector.tensor_reduce(out=wl[:, :, 0], in_=mm2[:], axis=AX.X, op=ALU.add)
            p.vector.tensor_copy(out=sl_i[:], in_=sl_f[:])
            cmp_ = p.sb("cmp_", [128, NST, NE], F32)
            p.vector.tensor_tensor(out=cmp_[:], in0=fap(seg_end[:, 0:1], [[0, NST], [1, NE]]),
                                   in1=fap(thr[:, 0:1], [[1, NST], [0, NE]]), op=ALU.is_le)
            eall = p.sb("eall", [128, NST], F32)
            p.vector.tensor_reduce(out=eall[:], in_=cmp_[:], axis=AX.X, op=ALU.add)
            p.vector.tensor_scalar(out=eall[:], in0=eall[:], scalar1=float(NE - 1), scalar2=None, op0=ALU.min)
            wif = p.sb("wif", [128, NST, 8], F32)
            p.vector.scalar_tensor_tensor(out=wif[:], in0=fap(eall[:, 0:1], [[1, NST], [0, 8]]), scalar=1024.0,
                                          in1=fap(b8[:, 0:1], [[0, NST], [1, 8]]), op0=ALU.mult, op1=ALU.add)
            if l > 0:
                p.vector.tensor_scalar(out=wif[:], in0=wif[:], scalar1=float(l * NE * D), scalar2=None, op0=ALU.add)
            p.vector.tensor_copy(out=widx_i[:], in_=wif[:])
            p.vector.scalar_tensor_tensor(out=wif[:, :, 0:4], in0=fap(eall[:, 0:1], [[1, NST], [0, 4]]), scalar=512.0,
                                          in1=fap(b4[:, 0:1], [[0, NST], [1, 4]]), op0=ALU.mult, op1=ALU.add)
            if l > 0:
                p.vector.tensor_scalar(out=wif[:, :, 0:4], in0=wif[:, :, 0:4], scalar1=float(l * NE * FF), scalar2=None, op0=ALU.add)
            p.vector.tensor_copy(out=didx_i[:], in_=wif[:, :, 0:4])
            for tl in range(NTL):
                h2t = h2p.next()
                p.sync.dma_start(out=h2t[:], in_=H2[tl * 128:(tl + 1) * 128, :])
                for j in range(2):
                    p.gpsimd.indirect_dma_start(out=Xs[:, :], out_offset=IOA(ap=sl_i[:, tl, j:j + 1], axis=0), in_=h2t[:, :],
                                                in_offset=None)
        if os.environ.get("MSTOP", "") == "R":
            k.dbg("d_sl", sl_f[:], [128, NTL, 2])
            k.dbg("d_wl", wl[:], [128, NTL, 2])
            k.dbg("d_comb", comb_all[:], [128, NTL, NE])
            return
        with p.scope():
            wgup = p.pool("wgus", [128, 8, 2 * FF], BF16, bufs=2)
            wdp = p.pool("wds", [128, 4, D], BF16, bufs=2)
            xsp = p.pool("xsp", [128, 4, D], BF16, bufs=2)
            xTp = p.pool("xTp", [128, 8, 512], BF16, bufs=2)
            aTp = p.pool("aTs", [128, 4, 512], BF16, bufs=2)
            sgp = p.pool("sgs", [128, 512], BF16, bufs=2)
            ysp = p.pool("ysp", [128, 4, D], BF16, bufs=2)
            def ex_load(i):
                wgu = wgup.next()
                wd = wdp.next()
                for kc in range(8):
                    p.gpsimd.indirect_dma_start(out=wgu[:, kc, 0:FF], out_offset=None, in_=wgb[:, :],
                                                in_offset=IOA(ap=widx_i[:, i, kc:kc + 1], axis=0))
                    p.gpsimd.indirect_dma_start(out=wgu[:, kc, FF:2 * FF], out_offset=None, in_=wub[:, :],
                                                in_offset=IOA(ap=widx_i[:, i, kc:kc + 1], axis=0))
                for f in range(4):
                    p.gpsimd.indirect_dma_start(out=wd[:, f, :], out_offset=None, in_=wd_tab[:, :],
                                                in_offset=IOA(ap=didx_i[:, i, f:f + 1], axis=0))
                xs_ = xsp.next()
                p.sync.dma_start(out=xs_[:], in_=Xs[i * 512:(i + 1) * 512, :].rearrange("(q p) d -> p q d", p=128))
                xT = xTp.next()
                for q in range(4):
                    for half in range(2):
                        pt = ps_all.next(BF16)
                        for j in range(4):
                            kc = half * 4 + j
                            p.tensor.transpose(out=pt[:, j * 128:(j + 1) * 128], in_=xs_[:, q, kc * 128:(kc + 1) * 128], identity=idb[:])
                        if (q + half) % 2 == 0:
                            p.scalar.copy(out=xT[:, half * 4:(half + 1) * 4, q * 128:(q + 1) * 128],
                                          in_=pt[:, 0:512].rearrange("p (j c) -> p j c", j=4))
                        else:
                            p.vector.tensor_copy(out=xT[:, half * 4:(half + 1) * 4, q * 128:(q + 1) * 128],
                                                 in_=pt[:, 0:512].rearrange("p (j c) -> p j c", j=4))
                return (i, wgu, wd, xT)

            def ex_compute(i, wgu, wd, xT):
                aT = aTp.next()
                for f in range(4):
                    pg = ps_all.next()
                    pu = ps_all.next()
                    for kc in range(8):
                        p.tensor.matmul(out=pg[:, :], lhsT=wgu[:, kc, f * 128:(f + 1) * 128], rhs=xT[:, kc, :], start=(kc == 0), stop=(kc == 7))
                    for kc in range(8):
                        p.tensor.matmul(out=pu[:, :], lhsT=wgu[:, kc, FF + f * 128:FF + (f + 1) * 128], rhs=xT[:, kc, :], start=(kc == 0), stop=(kc == 7))
                    sg = sgp.next()
                    p.scalar.activation(out=sg[:], in_=pg[:, :], func=AF.Silu)
                    p.vector.tensor_tensor(out=aT[:, f, :], in0=sg[:], in1=pu[:, :], op=ALU.mult)
                ys = ysp.next()
                for q in range(4):
                    for half in range(2):
                        pd = ps_all.next()
                        for f in range(4):
                            p.tensor.matmul(out=pd[:, :], lhsT=aT[:, f, q * 128:(q + 1) * 128], rhs=wd[:, f, half * 512:(half + 1) * 512],
                                            start=(f == 0), stop=(f == 3))
                        if (q + half) % 2 == 0:
                            p.scalar.copy(out=ys[:, q, half * 512:(half + 1) * 512], in_=pd[:, :])
                        else:
                            p.vector.tensor_copy(out=ys[:, q, half * 512:(half + 1) * 512], in_=pd[:, :])
                p.sync.dma_start(out=Ys[i * 512:(i + 1) * 512, :].rearrange("(q p) d -> p q d", p=128), in_=ys[:])

            pend = None
            for i in range(NST):
                cur = ex_load(i)
                if pend is not None:
                    ex_compute(*pend)
                pend = cur
            ex_compute(*pend)
        with p.scope():
            ygp = p.pool("ygp", [128, 2, D], BF16, bufs=3)
            fp_ = p.pool("fcomb", [128, D], F32, bufs=2)

            def cb_s1(tl, t):
                yg = ygp.next()
                for j in range(2):
                    p.gpsimd.indirect_dma_start(out=yg[:, j, :], out_offset=None, in_=Ys[:, :],
                                                in_offset=IOA(ap=sl_i[:, tl, j:j + 1], axis=0))
                xt = xpool.next()
                p.sync.dma_start(out=xt[:], in_=tile_ap(src[0], src[1], t))
                return (tl, t, yg, xt)

            def cb_s2(tl, t, yg, xt):
                w_ = 1 if t < 2 else 0
                f_ = fp_.next()
                p.vector.tensor_scalar(out=f_[:], in0=yg[:, 0, :], scalar1=wl[:, tl, 0:1], scalar2=None, op0=ALU.mult)
                p.vector.scalar_tensor_tensor(out=f_[:], in0=yg[:, 1, :], scalar=wl[:, tl, 1:2], in1=f_[:], op0=ALU.mult, op1=ALU.add)
                tm = xspool.next()
                p.vector.tensor_tensor(out=tm[:], in0=f_[:], in1=gabc[:, 2 + w_, :], op=ALU.mult)
                p.vector.tensor_tensor(out=tm[:], in0=tm[:], in1=xt[:], op=ALU.add)
                if final:
                    jk = junk.next()
                    sm = small.next()
                    p.scalar.activation(out=jk[:], in_=tm[:], func=AF.Square, accum_out=sm[:, 0:1])
                    rstd_from_ss(p, sm[:, 2:3], sm[:, 0:1], D, sm[:, 1:2])
                    p.vector.scalar_tensor_tensor(out=tm[:], in0=tm[:], scalar=sm[:, 2:3], in1=fng_bc[:],
                                                  op0=ALU.mult, op1=ALU.mult)
                p.sync.dma_start(out=tile_ap(dst[0], dst[1], t), in_=tm[:])

            pend = None
            for tl, t in enumerate(tiles):
                cur = cb_s1(tl, t)
                if pend is not None:
                    cb_s2(*pend)
                pend = cur
            cb_s2(*pend)

    k.moe_sparse = moe_sparse

    NCH = 68
    cd_w_in = IN("cd_w_in", [D, 3088])
    cv_dw_w = IN("cv_dw_w", [31, 512])
    cv_vec = IN("cv_vec", [12, 128])
    dn_conv_w = IN("dn_conv_w", [5, 1536])
    dn_alog = IN("dn_a_log", [1, 8])
    dn_dtb = IN("dn_dt_bias", [1, 8])
    dn_onorm = IN("dn_o_norm", [1, 128])
    cd_w_out = IN("cd_w_out", [D, D])
    dn_consts = IN("dn_consts", [64, 5, 128])
    qT_d = p.dram("qT_d", [4, 128, NT * 128], BF16, kind=dk)
    kT_d = p.dram("kT_d", [4, 128, NT * 128], BF16, kind=dk)
    vT_d = p.dram("vT_d", [4, 128, NT * 128], BF16, kind=dk)
    convT_d = p.dram("convT_d", [4, 128, S], BF16, kind=dk)
    cv_d = p.dram("cv_d", [4, 128, S], F32, kind=dk)
    sog_d = p.dram("sog_d", [S, 512], BF16, kind=dk)
    o_d = p.dram("o_d", [2, S, 512], F32, kind=dk)
    L1STOP = os.environ.get("L1STOP", "")

    def layer1_mixer(src_lat, src_ctx, dst_lat):
      with p.scope():
        abr = p.sb("abr", [64, 2, 2, NCH, 4], F32)
        gall = p.sb("gall", [64, 2, NCH, 4], F32)
        beta = p.sb("beta", [64, 2, NCH, 4], F32)
        egc = p.sb("egc", [64, 2, NCH, 4], F32)
        egd = p.sb("egd", [64, 2, NCH, 4], F32)
        be = p.sb("be", [64, 2, NCH, 4], F32)
        eglb = p.sb("eglb", [128, 2, NCH, 4], F32)
        dnc = p.sb("dnc", [64, 5, 128], F32)
        p.sync.dma_start(out=dnc[:], in_=dn_consts)
        id64 = idt[0:64, 0:64]
        CO, LO = 2, 2 + 256 + 2 + 2
        segs = [(CO, 0, 256), (LO, 256, 4096)]
        with p.scope():
            hT = p.sb("hT1", [128, 8, NT * 128], BF16)
            for t in range(NT):
                w_ = 1 if t < 2 else 0
                norm_tile(tile_ap(src_lat, src_ctx, t), gmod1, 0, w_,
                          lambda kc, t=t: hT[:, kc, t * 128:(t + 1) * 128])
            cw5 = p.sb("cw5", [128, 12, 5], F32)
            cw31 = p.sb("cw31", [128, 4, 31], F32)
            cvc = p.sb("cvc", [128, 12], F32)
            ones_b = p.sb("ones_b", [128, 128], BF16)
            p.vector.memset(ap=ones_b[:], constant=1.0)
            with p.scope():
                cw5s = p.sb("cw5s", [5, 1536], F32)
                p.sync.dma_start(out=cw5s[:], in_=dn_conv_w)
                for half in range(2):
                    pt = ps_all.next()
                    for q in range(6):
                        cc = half * 6 + q
                        p.tensor.transpose(out=pt[:, q * 5:(q + 1) * 5], in_=cw5s[0:5, cc * 128:(cc + 1) * 128],
                                           identity=idt[0:5, 0:5])
                    p.vector.tensor_copy(out=cw5[:, half * 6:(half + 1) * 6, :],
                                         in_=pt[:, 0:30].rearrange("p (c k) -> p c k", k=5))
                cw31s = p.sb("cw31s", [31, 512], F32)
                p.sync.dma_start(out=cw31s[:], in_=cv_dw_w)
                pt = ps_all.next()
                for cc in range(4):
                    p.tensor.transpose(out=pt[:, cc * 31:(cc + 1) * 31], in_=cw31s[0:31, cc * 128:(cc + 1) * 128],
                                       identity=idt[0:31, 0:31])
                p.vector.tensor_copy(out=cw31[:], in_=pt[:, 0:124].rearrange("p (c k) -> p c k", k=31))
                cvs = p.sb("cvs", [12, 128], F32)
                p.sync.dma_start(out=cvs[:], in_=cv_vec)
                pt = ps_all.next()
                p.tensor.transpose(out=pt[:, 0:12], in_=cvs[0:12, :], identity=idt[0:12, 0:12])
                p.vector.tensor_copy(out=cvc[:], in_=pt[:, 0:12])

            if L1STOP == "F0":
                return
            with p.scope():
                wab = p.sb("wab", [128, 8, 16], BF16)
                p.gpsimd.dma_start(out=wab[:], in_=cd_w_in[:, 2560:2576].rearrange("(kc p) n -> p kc n", p=128))
                for c in range(NCH):
                    pa = ps_all.next()
                    for kc in range(8):
                        p.tensor.matmul(out=pa[0:64, 0:16], lhsT=hT[:, kc, c * 64:(c + 1) * 64], rhs=wab[:, kc, :],
                                        start=(kc == 0), stop=(kc == 7))
                    p.scalar.copy(out=abr[:, :, :, c, :], in_=pa[0:64, 0:16].rearrange("p (k d h) -> p k d h", k=2, d=2))
                if L1STOP == "F1a":
                    return
                dtb = p.sb("dtb", [64, 8], F32)
                nega = p.sb("nega", [64, 8], F32)
                p.sync.dma_start(out=dtb[:], in_=dn_dtb.partition_broadcast(64))
                p.sync.dma_start(out=nega[:], in_=dn_alog.partition_broadcast(64))
                p.scalar.activation(out=nega[:], in_=nega[:], func=AF.Exp)
                p.vector.tensor_scalar(out=nega[:], in0=nega[:], scalar1=-1.0, scalar2=None, op0=ALU.mult)
                bc8 = lambda t_: fap(t_[:, 0:1], [[4, 2], [0, NCH], [1, 4]])
                p.vector.tensor_tensor(out=gall[:], in0=abr[:, 0], in1=bc8(dtb), op=ALU.add)
                p.scalar.activation(out=gall[:], in_=gall[:], func=AF.Exp)
                p.vector.tensor_scalar(out=gall[:], in0=gall[:], scalar1=1.0, scalar2=None, op0=ALU.add)
                p.scalar.activation(out=gall[:], in_=gall[:], func=AF.Ln)
                p.vector.tensor_tensor(out=gall[:], in0=gall[:], in1=bc8(nega), op=ALU.mult)
                p.scalar.activation(out=beta[:], in_=abr[:, 1], func=AF.Sigmoid)
                if L1STOP == "F1b":
                    return
                for d_ in range(2):
                    pg = ps_all.next()
                    p.tensor.matmul(out=pg[0:64, 0:NCH * 4], lhsT=dnc[:, d_, 64:128],
                                    rhs=gall[:, d_].rearrange("p c h -> p (c h)"), start=True, stop=True)
                    pgl = ps_all.next()
                    p.tensor.matmul(out=pgl[:, 0:NCH * 4], lhsT=ones_f[0:64, :],
                                    rhs=gall[:, d_].rearrange("p c h -> p (c h)"), start=True, stop=True)
                    fl = lambda t_: t_[:, d_].rearrange("p c h -> p (c h)")
                    p.scalar.activation(out=fl(egc), in_=pg[0:64, 0:NCH * 4], func=AF.Exp)
                    p.scalar.activation(out=eglb[:, d_].rearrange("p c h -> p (c h)"), in_=pgl[:, 0:NCH * 4], func=AF.Exp)
                    p.vector.tensor_copy(out=fl(egd), in_=pg[0:64, 0:NCH * 4])
                    p.vector.tensor_tensor(out=fl(egd), in0=pgl[0:64, 0:NCH * 4], in1=fl(egd), op=ALU.subtract)
                    p.scalar.activation(out=fl(egd), in_=fl(egd), func=AF.Exp)
                p.vector.tensor_tensor(out=be[:], in0=beta[:], in1=egc[:], op=ALU.mult)
                if L1STOP == "F1c":
                    return
                wog = p.sb("wog", [128, 8, 512], BF16)
                p.gpsimd.dma_start(out=wog[:], in_=cd_w_in[:, 2576:3088].rearrange("(kc p) n -> p kc n", p=128))
                sogp = p.pool("sogp", [128, 512], BF16, bufs=2)
                for t in range(2, NT):
                    po = ps_all.next()
                    for kc in range(8):
                        p.tensor.matmul(out=po[:, :], lhsT=hT[:, kc, t * 128:(t + 1) * 128], rhs=wog[:, kc, :],
                                        start=(kc == 0), stop=(kc == 7))
                    so = sogp.next()
                    p.scalar.activation(out=so[:], in_=po[:, :], func=AF.Silu)
                    p.sync.dma_start(out=sog_d[(t - 2) * 128:(t - 1) * 128, :], in_=so[:])

            if L1STOP == "F1":
                return

            def project_cc(col0, xc, wcc):
                w = wcc.next()
                p.gpsimd.dma_start(out=w[:], in_=cd_w_in[:, col0:col0 + 128].rearrange("(kc p) n -> p kc n", p=128))
                for (t0, ntl) in blocks:
                    N = ntl * 128
                    pp = ps_all.next()
                    for kc in range(8):
                        p.tensor.matmul(out=pp[:, 0:N], lhsT=w[:, kc, :], rhs=hT[:, kc, t0 * 128:t0 * 128 + N],
                                        start=(kc == 0), stop=(kc == 7))
                    off = CO + t0 * 128 if t0 < 2 else LO + (t0 - 2) * 128
                    p.scalar.copy(out=xc[:, off:off + N], in_=pp[:, 0:N])

            def alloc_xc(dt_=F32):
                xc = p.sb("xc", [128, NT * 128 + 8], dt_)
                p.vector.memset(ap=xc[:, 0:2], constant=0.0)
                p.vector.memset(ap=xc[:, 258:262], constant=0.0)
                p.vector.memset(ap=xc[:, LO + 4096:LO + 4098], constant=0.0)
                return xc

            with p.scope():
                wcc = p.pool("wcc", [128, 8, 128], BF16, bufs=2)
                xc = alloc_xc(BF16)
                yc = p.sb("yc", [128, NT * 128], F32)
                ynp = p.pool("ynp", [128, NT * 128], BF16, bufs=2)
                sqp = p.pool("sqp", [128, 512], BF16, bufs=3)
                rsp = p.pool("rsp", [128, 512], F32, bufs=2)
                dg5 = p.sb("dg5", [128, 12, 5, 128], BF16)
                for cc in range(12):
                    for k_ in range(5):
                        p.vector.tensor_scalar(out=dg5[:, cc, k_, :], in0=idb[:], scalar1=cw5[:, cc, k_:k_ + 1], scalar2=None,
                                               op0=ALU.mult)
                for cc in range(12):
                    project_cc(1024 + cc * 128, xc, wcc)
                    yn = ynp.next()
                    pend = None
                    for (t0, ntl) in blocks + [(None, None)]:
                        if t0 is not None if False else False:
                            pass
                        cur = None
                        if (t0, ntl) != (None, None) and t0 is not None:
                            pass
                        if (t0 is not None):
                            N = ntl * 128
                            c0 = t0 * 128
                            off = CO + t0 * 128 if t0 < 2 else LO + (t0 - 2) * 128
                            pc = ps_all.next()
                            for k_ in range(5):
                                p.tensor.matmul(out=pc[:, 0:N], lhsT=dg5[:, cc, k_, :], rhs=xc[:, off + k_ - 2:off + k_ - 2 + N],
                                                start=(k_ == 0), stop=(k_ == 4))
                            if cc >= 8:
                                p.scalar.activation(out=yn[:, c0:c0 + N], in_=pc[:, 0:N], func=AF.Silu)
                            else:
                                p.scalar.activation(out=yc[:, c0:c0 + N], in_=pc[:, 0:N], func=AF.Silu)
                                sq = sqp.next()
                                p.scalar.activation(out=sq[:, 0:N], in_=yc[:, c0:c0 + N], func=AF.Square)
                                cur = (N, c0, sq)
                        if pend is not None:
                            (N, c0, sq) = pend
                            pss = ps_all.next()
                            p.tensor.matmul(out=pss[:, 0:N], lhsT=ones_b[:], rhs=sq[:, 0:N], start=True, stop=True)
                            rs = rsp.next()
                            p.vector.tensor_scalar(out=rs[:, 0:N], in0=pss[:, 0:N], scalar1=EPS, scalar2=None, op0=ALU.add)
                            p.scalar.activation(out=rs[:, 0:N], in_=rs[:, 0:N], func=AF.Sqrt, scale=(128.0 if cc < 4 else 1.0))
                            p.vector.reciprocal(out=rs[:, 0:N], in_=rs[:, 0:N])
                            p.gpsimd.tensor_tensor(out=yn[:, c0:c0 + N], in0=yc[:, c0:c0 + N], in1=rs[:, 0:N], op=ALU.mult)
                        pend = cur
                    dst = qT_d if cc < 4 else (kT_d if cc < 8 else vT_d)
                    p.sync.dma_start(out=dst[cc % 4], in_=yn[:])

            if L1STOP == "F2":
                return
            with p.scope():
                wcc = p.pool("wcc2", [128, 8, 128], BF16, bufs=2)
                xc = alloc_xc(F32)
                xg2 = p.sb("xg2", [128, S + 30], F32)
                xgb = p.sb("xgb", [128, S + 30], BF16)
                cvyp = p.pool("cvyp", [128, 512], F32, bufs=3)
                dgp = p.pool("dg31", [128, 31, 128], BF16, bufs=2)
                p.vector.memset(ap=xgb[:, 0:15], constant=0.0)
                p.vector.memset(ap=xgb[:, 15 + S:30 + S], constant=0.0)
                for cc in range(4):
                    dg = dgp.next()
                    for k_ in range(31):
                        p.vector.tensor_scalar(out=dg[:, k_, :], in0=idb[:], scalar1=cw31[:, cc, k_:k_ + 1], scalar2=None,
                                               op0=ALU.mult)
                    project_cc(cc * 128, xc, wcc)
                    w = wcc.next()
                    p.gpsimd.dma_start(out=w[:], in_=cd_w_in[:, 512 + cc * 128:512 + (cc + 1) * 128].rearrange("(kc p) n -> p kc n", p=128))
                    for (t0, ntl) in blocks[1:]:
                        pp = ps_all.next()
                        for kc in range(8):
                            p.tensor.matmul(out=pp[:, :], lhsT=w[:, kc, :], rhs=hT[:, kc, t0 * 128:t0 * 128 + 512],
                                            start=(kc == 0), stop=(kc == 7))
                        o0 = 15 + (t0 - 2) * 128
                        p.scalar.activation(out=xg2[:, o0:o0 + 512], in_=pp[:, :], func=AF.Sigmoid)
                        eng = p.gpsimd if (t0 // 4) % 2 == 0 else p.vector
                        eng.tensor_tensor(out=xgb[:, o0:o0 + 512], in0=xg2[:, o0:o0 + 512],
                                          in1=xc[:, LO + (t0 - 2) * 128:LO + (t0 - 2) * 128 + 512], op=ALU.mult)
                    for bi in range(8):
                        pc = ps_all.next()
                        for k_ in range(31):
                            p.tensor.matmul(out=pc[:, :], lhsT=dg[:, k_, :], rhs=xgb[:, bi * 512 + k_:bi * 512 + k_ + 512],
                                            start=(k_ == 0), stop=(k_ == 30))
                        cv = cvyp.next()
                        p.scalar.activation(out=cv[:], in_=pc[:, :], func=AF.Identity, bias=cvc[:, cc:cc + 1], scale=1.0)
                        p.sync.dma_start(out=cv_d[cc, :, bi * 512:(bi + 1) * 512], in_=cv[:])
        if L1STOP == "F3":
            return
        with p.scope():
            cvc = p.sb("cvc2", [128, 12], F32)
            cvs = p.sb("cvs2", [12, 128], F32)
            p.sync.dma_start(out=cvs[:], in_=cv_vec)
            pt = ps_all.next()
            p.tensor.transpose(out=pt[:, 0:12], in_=cvs[0:12, :], identity=idt[0:12, 0:12])
            p.vector.tensor_copy(out=cvc[:], in_=pt[:, 0:12])
            cvo = p.pool("cvo", [128, 4, 512], BF16, bufs=2)
            cvi = p.pool("cvi", [128, 4, 512], F32, bufs=2)
            lnp = p.pool("lnp", [128, 512], F32, bufs=4)
            for bi in range(8):
                cvy = cvi.next()
                p.sync.dma_start(out=cvy[:], in_=cv_d[:, :, bi * 512:(bi + 1) * 512].rearrange("j c t -> c j t"))
                pmu = ps_all.next()
                for cc in range(4):
                    p.tensor.matmul(out=pmu[:, :], lhsT=ones_f[:], rhs=cvy[:, cc, :], start=(cc == 0), stop=(cc == 3))
                nmean = lnp.next()
                p.vector.tensor_scalar(out=nmean[:], in0=pmu[:, :], scalar1=-1.0 / 512, scalar2=None, op0=ALU.mult)
                pvar = ps_all.next()
                for cc in range(4):
                    p.gpsimd.tensor_tensor(out=cvy[:, cc, :], in0=cvy[:, cc, :], in1=nmean[:], op=ALU.add)
                    sq = lnp.next()
                    p.scalar.activation(out=sq[:], in_=cvy[:, cc, :], func=AF.Square)
                    p.tensor.matmul(out=pvar[:, :], lhsT=ones_f[:], rhs=sq[:], start=(cc == 0), stop=(cc == 3))
                rs = lnp.next()
                p.vector.tensor_scalar(out=rs[:], in0=pvar[:, :], scalar1=1.0 / 512, scalar2=EPS, op0=ALU.mult, op1=ALU.add)
                p.scalar.activation(out=rs[:], in_=rs[:], func=AF.Sqrt)
                p.vector.reciprocal(out=rs[:], in_=rs[:])
                co = cvo.next()
                for cc in range(4):
                    p.vector.tensor_tensor(out=cvy[:, cc, :], in0=cvy[:, cc, :], in1=rs[:], op=ALU.mult)
                    p.scalar.activation(out=co[:, cc, :], in_=cvy[:, cc, :], func=AF.Silu,
                                        bias=cvc[:, 8 + cc:9 + cc], scale=cvc[:, 4 + cc:5 + cc])
                p.sync.dma_start(out=convT_d[:, :, bi * 512:(bi + 1) * 512].rearrange("j c t -> c j t"), in_=co[:])
        if L1STOP == "F":
            k.dbg("d_gall", gall[:], [64, 2, NCH, 4])
            k.dbg("d_beta", beta[:], [64, 2, NCH, 4])
            k.dbg("d_egc", egc[:], [64, 2, NCH, 4])
            k.dbg("d_egd", egd[:], [64, 2, NCH, 4])
            k.dbg("d_eglb", eglb[:], [128, 2, NCH, 4])
            return

        with p.scope():
            ps_a = PsumPool(p, [0, 1, 2, 3, 4, 5])
            ps_q = PsumPool(p, [6, 7])
            GS = 8
            kqv = p.pool("kqv", [128, 3, 4, 512], BF16, bufs=2)
            kdp = p.pool("kdp", [64, GS, 4, 128], BF16, bufs=2)
            qkp = p.pool("qkp", [64, GS, 4, 64], BF16, bufs=2)
            up = p.pool("up", [64, GS, 4, 128], BF16, bufs=2)
            wTp = p.pool("wTp", [128, GS, 4, 64], BF16, bufs=2)
            bvp = p.pool("bvp", [64, 2, 4, 128], BF16, bufs=4)
            guup = p.pool("guup", [64, 4, 128], F32, bufs=3)
            ddp = p.pool("ddp", [64, 4, 128], F32, bufs=3)
            mat = p.pool("mat", [64, 6, 4, 64], BF16, bufs=4)
            S32 = p.sb("S32", [128, 4, 128], F32)
            Sb = p.sb("Sb", [128, 4, 128], BF16)
            vnp_ = p.pool("vnp_", [64, 4, 128], BF16, bufs=3)
            otp_ = p.pool("otp_", [64, 512], F32, bufs=3)
            oqs = p.pool("oqs", [64, 512], F32, bufs=3)
            v4 = lambda ap_, st, w: fap(ap_, [[st, 4], [1, w]])
            for d_ in range(2):
                p.vector.memset(ap=S32[:], constant=0.0)
                p.vector.memset(ap=Sb[:], constant=0.0)
                if d_ == 0:
                    groups = [list(range(0, 4))] + [list(range(4 + 8 * g, 12 + 8 * g)) for g in range(8)]
                else:
                    groups = [list(range(3, -1, -1))] + [list(range(11 + 8 * g, 3 + 8 * g, -1)) for g in range(7, -1, -1)]
                UU = dnc[:, d_, :]
                PN = dnc[:, 2, :]
                NM = dnc[:, 3 + d_, :]

                def precompute(c, lo, kq, kd, qk, u_, wT):
                    ci = c - lo
                    ts_ = slice(ci * 64, (ci + 1) * 64)
                    sc4 = lambda t_, w: fap(t_[:, d_, c, 0:1], [[1, 4], [0, w]])
                    m = mat.next()
                    bv = bvp.next()
                    ptk = ps_a.next(BF16)
                    for h in range(4):
                        p.tensor.transpose(out=ptk[0:64, h * 256:h * 256 + 128], in_=kq[:, 0, h, ts_], identity=idb[:])
                        p.tensor.transpose(out=ptk[0:64, h * 256 + 128:h * 256 + 256], in_=kq[:, 2, h, ts_], identity=idb[:])
                    pk = v4(ptk[0:64, 0:1], 256, 128)
                    pv = v4(ptk[0:64, 128:129], 256, 128)
                    p.vector.tensor_tensor(out=kd[:, ci, :, :], in0=pk, in1=sc4(egd, 128), op=ALU.mult)
                    p.vector.tensor_tensor(out=bv[:, 0, :, :], in0=pk, in1=sc4(be, 128), op=ALU.mult)
                    p.vector.tensor_tensor(out=bv[:, 1, :, :], in0=pv, in1=sc4(beta, 128), op=ALU.mult)
                    yield
                    pkg = ps_a.next()
                    for h in range(4):
                        p.tensor.matmul(out=pkg[0:64, h * 128:h * 128 + 64], lhsT=kq[:, 0, h, ts_], rhs=kq[:, 0, h, ts_], start=True, stop=True)
                        p.tensor.matmul(out=pkg[0:64, h * 128 + 64:h * 128 + 128], lhsT=kq[:, 0, h, ts_], rhs=kq[:, 1, h, ts_], start=True, stop=True)
                    guu = guup.next()
                    p.gpsimd.tensor_tensor(out=guu[:], in0=fap(UU[:, 0:1], [[0, 4], [1, 128]]), in1=sc4(gall, 128), op=ALU.mult)
                    yield
                    pdd = ps_a.next()
                    for h in range(4):
                        o_ = pdd[0:64, h * 128:(h + 1) * 128]
                        p.tensor.matmul(out=o_, lhsT=guu[:, h, 64:128], rhs=PN, start=True, stop=False)
                        p.tensor.matmul(out=o_, lhsT=ones_f[0:64, 0:64], rhs=guu[:, h, :], start=False, stop=False)
                        p.tensor.matmul(out=o_, lhsT=id64, rhs=NM, start=False, stop=True)
                    dd = ddp.next()
                    p.scalar.activation(out=dd[:].rearrange("p h w -> p (h w)"), in_=pdd[0:64, 0:512], func=AF.Exp)
                    yield
                    p.vector.tensor_tensor(out=m[:, 0, :, :], in0=v4(pkg[0:64, 0:1], 128, 64), in1=dd[:, :, 0:64], op=ALU.mult)
                    p.vector.tensor_tensor(out=qk[:, ci, :, :], in0=v4(pkg[0:64, 64:65], 128, 64), in1=dd[:, :, 64:128], op=ALU.mult)
                    p.gpsimd.tensor_tensor(out=m[:, 0, :, :], in0=m[:, 0, :, :], in1=sc4(beta, 64), op=ALU.mult)
                    yield
                    pb = ps_a.next(BF16)
                    for h in range(4):
                        p.tensor.transpose(out=pb[0:64, h * 64:(h + 1) * 64], in_=m[:, 0, h, :], identity=idb[0:64, 0:64])
                    p.scalar.copy(out=m[:, 1, :, :], in_=pb[0:64, 0:256].rearrange("p (h w) -> p h w", h=4))
                    p.gpsimd.tensor_tensor(out=m[:, 3, :, :], in0=fap(idb[0:64, 0:1], [[0, 4], [1, 64]]), in1=m[:, 1, :, :], op=ALU.subtract)
                    yield
                    Pc, Qc, R = 1, 0, 3
                    for lev in range(1, 6):
                        nP = 4 if Pc == 1 else 1
                        nQ = 2 if Qc in (0, 5) else 5
                        pp_ = ps_a.next()
                        for h in range(4):
                            if lev < 5:
                                p.tensor.matmul(out=pp_[0:64, h * 128:h * 128 + 64], lhsT=m[:, Qc, h, :], rhs=m[:, Pc, h, :], start=True, stop=True)
                            p.tensor.matmul(out=pp_[0:64, h * 128 + 64:h * 128 + 128], lhsT=m[:, Pc, h, :], rhs=m[:, Qc, h, :], start=True, stop=True)
                        if lev < 5:
                            p.scalar.copy(out=m[:, nP, :, :], in_=v4(pp_[0:64, 0:1], 128, 64))
                        p.vector.tensor_copy(out=m[:, nQ, :, :], in_=v4(pp_[0:64, 64:65], 128, 64))
                        yield
                        pr = ps_a.next()
                        for h in range(4):
                            p.tensor.matmul(out=pr[0:64, h * 64:(h + 1) * 64], lhsT=m[:, nQ, h, :], rhs=m[:, R, h, :], start=True, stop=True)
                        p.vector.tensor_tensor(out=m[:, R, :, :], in0=m[:, R, :, :],
                                               in1=pr[0:64, 0:256].rearrange("p (h w) -> p h w", h=4), op=ALU.add)
                        Pc, Qc = nP, nQ
                        yield
                    pu_ = ps_a.next()
                    pw_ = ps_a.next()
                    for h in range(4):
                        p.tensor.matmul(out=pu_[0:64, h * 128:(h + 1) * 128], lhsT=m[:, R, h, :], rhs=bv[:, 1, h, :], start=True, stop=True)
                    for h in range(4):
                        p.tensor.matmul(out=pw_[:, h * 64:(h + 1) * 64], lhsT=bv[:, 0, h, :], rhs=m[:, R, h, :], start=True, stop=True)
                    p.scalar.copy(out=u_[:, ci, :, :], in_=pu_[0:64, 0:512].rearrange("p (h w) -> p h w", h=4))
                    p.scalar.copy(out=wT[:, ci, :, :], in_=pw_[:, 0:256].rearrange("p (h w) -> p h w", h=4))
                    yield

                def scan_gen(grp, lo, kq, kd, qk, u_, wT):
                    for c in grp:
                        ci = c - lo
                        ts_ = slice(ci * 64, (ci + 1) * 64)
                        islat = c >= 4
                        sc4 = lambda t_, w, c=c: fap(t_[:, d_, c, 0:1], [[1, 4], [0, w]])
                        p1a = ps_q.next()
                        for h in range(4):
                            p.tensor.matmul(out=p1a[0:64, h * 128:(h + 1) * 128], lhsT=wT[:, ci, h, :], rhs=Sb[:, h, :], start=True, stop=True)
                        if islat:
                            p1b = ps_q.next()
                            for h in range(4):
                                p.tensor.matmul(out=p1b[0:64, h * 128:(h + 1) * 128], lhsT=kq[:, 1, h, ts_], rhs=Sb[:, h, :], start=True, stop=True)
                        p.gpsimd.tensor_tensor(out=S32[:], in0=S32[:], in1=sc4(eglb, 128), op=ALU.mult)
                        vn = vnp_.next()
                        p.vector.tensor_tensor(out=vn[:], in0=u_[:, ci, :, :], in1=p1a[0:64, 0:512].rearrange("p (h w) -> p h w", h=4),
                                               op=ALU.subtract)
                        if islat:
                            oq = oqs.next()
                            p.vector.tensor_tensor(out=oq[:].rearrange("p (h w) -> p h w", h=4),
                                                   in0=p1b[0:64, 0:512].rearrange("p (h w) -> p h w", h=4), in1=sc4(egc, 128), op=ALU.mult)
                        yield
                        p2a = ps_q.next()
                        for h in range(4):
                            p.tensor.matmul(out=p2a[:, h * 128:(h + 1) * 128], lhsT=kd[:, ci, h, :], rhs=vn[:, h, :], start=True, stop=True)
                        p.vector.tensor_tensor(out=S32[:], in0=S32[:], in1=p2a[:, :].rearrange("p (h w) -> p h w", h=4), op=ALU.add)
                        p.scalar.copy(out=Sb[:], in_=S32[:])
                        if islat:
                            p2b = ps_q.next()
                            for h in range(4):
                                p.tensor.matmul(out=p2b[0:64, h * 128:(h + 1) * 128], lhsT=qk[:, ci, h, :], rhs=vn[:, h, :], start=True, stop=True)
                            ot = otp_.next()
                            p.vector.tensor_tensor(out=ot[:], in0=oq[:], in1=p2b[0:64, 0:512], op=ALU.add)
                            p.sync.dma_start(out=o_d[d_, (c - 4) * 64:(c - 3) * 64, :], in_=ot[:])
                        yield

                prev_scan = None
                for grp in groups:
                    lo = min(grp)
                    ntok = len(grp) * 64
                    kq = kqv.next()
                    for si, src in enumerate((kT_d, qT_d, vT_d)):
                        p.sync.dma_start(out=kq[:, si, :, 0:ntok],
                                         in_=src[:, :, lo * 64:lo * 64 + ntok].rearrange("h c t -> c h t"))
                    kd = kdp.next()
                    qk = qkp.next()
                    u_ = up.next()
                    wT = wTp.next()
                    for pi in range(0, len(grp), 2):
                        gens = [precompute(c, lo, kq, kd, qk, u_, wT) for c in grp[pi:pi + 2]]
                        alive = list(gens)
                        rnd = 0
                        while alive:
                            nxt = []
                            for g_ in alive:
                                try:
                                    next(g_)
                                    nxt.append(g_)
                                except StopIteration:
                                    pass
                            alive = nxt
                            rnd += 1
                            if prev_scan is not None and rnd % 3 == 0:
                                try:
                                    next(prev_scan)
                                except StopIteration:
                                    prev_scan = None
                    if prev_scan is not None:
                        for _ in prev_scan:
                            pass
                    prev_scan = scan_gen(grp, lo, kq, kd, qk, u_, wT)
                for _ in prev_scan:
                    pass
        if L1STOP == "D":
            return
        with p.scope():
            wo = p.sb("wo1", [128, 8, D], BF16)
            p.gpsimd.dma_start(out=wo[:], in_=cd_w_out.rearrange("(c p) n -> p c n", p=128))
            gon = p.sb("gon", [128, 128], F32)
            p.sync.dma_start(out=gon[:], in_=dn_onorm.partition_broadcast(128))
            o0p = p.pool("o0p", [128, 512], F32, bufs=2)
            o1p = p.pool("o1p", [128, 512], F32, bufs=2)
            sgl = p.pool("sgl", [128, 512], BF16, bufs=2)
            ybp = p.pool("ybp", [128, 512], BF16, bufs=3)
            yTp = p.pool("yTp", [128, 8, 128], BF16, bufs=3)
            tmpp = p.pool("tmp1", [128, D], F32, bufs=2)
            def fl_s1(t):
                r0 = (t - 2) * 128
                o0 = o0p.next()
                o1 = o1p.next()
                sg = sgl.next()
                p.sync.dma_start(out=o0[:], in_=o_d[0, r0:r0 + 128, :])
                p.sync.dma_start(out=o1[:], in_=o_d[1, r0:r0 + 128, :])
                p.sync.dma_start(out=sg[:], in_=sog_d[r0:r0 + 128, :])
                yT = yTp.next()
                p.sync.dma_start(out=yT[:, 0:4, :], in_=convT_d[:, :, r0:r0 + 128].rearrange("j c t -> c j t"))
                xt = xpool.next()
                p.sync.dma_start(out=xt[:], in_=src_lat[r0:r0 + 128, :])
                p.gpsimd.tensor_tensor(out=o0[:], in0=o0[:], in1=o1[:], op=ALU.add)
                jk = junk.next()
                sm = small.next()
                p.scalar.activation(out=jk[:, 0:512], in_=o0[:], func=AF.Square)
                p.vector.tensor_reduce(out=sm[:, 0:4], in_=jk[:, 0:512].rearrange("p (h d) -> p h d", d=128), axis=AX.X, op=ALU.add)
                sm2 = small.next()
                rstd_from_ss(p, sm2[:, 0:4], sm[:, 0:4], 128, sm[:, 4:8])
                p.vector.tensor_tensor(out=o0[:].rearrange("p (h d) -> p h d", d=128), in0=o0[:].rearrange("p (h d) -> p h d", d=128),
                                       in1=fap(sm2[:, 0:1], [[1, 4], [0, 128]]), op=ALU.mult)
                p.gpsimd.tensor_tensor(out=o0[:].rearrange("p (h d) -> p h d", d=128), in0=o0[:].rearrange("p (h d) -> p h d", d=128),
                                       in1=fap(gon[:, 0:1], [[0, 4], [1, 128]]), op=ALU.mult)
                yb = ybp.next()
                p.vector.tensor_tensor(out=yb[:], in0=o0[:], in1=sg[:], op=ALU.mult)
                return (t, yb, yT, xt)

            def fl_s2(t, yb, yT, xt):
                r0 = (t - 2) * 128
                pty = ps_all.next(BF16)
                for j in range(4):
                    p.tensor.transpose(out=pty[:, j * 128:(j + 1) * 128], in_=yb[:, j * 128:(j + 1) * 128], identity=idb[:])
                p.scalar.copy(out=yT[:, 4:8, :], in_=pty[:, 0:512].rearrange("p (j q) -> p j q", j=4))
                tm = tmpp.next()
                for half in range(2):
                    pm = ps_all.next()
                    for c_ in range(8):
                        p.tensor.matmul(out=pm[:, :], lhsT=yT[:, c_, :], rhs=wo[:, c_, half * 512:(half + 1) * 512],
                                        start=(c_ == 0), stop=(c_ == 7))
                    p.vector.tensor_tensor(out=tm[:, half * 512:(half + 1) * 512], in0=pm[:, :],
                                           in1=gabc[:, 0, half * 512:(half + 1) * 512], op=ALU.mult)
                p.gpsimd.tensor_tensor(out=tm[:], in0=tm[:], in1=xt[:], op=ALU.add)
                p.sync.dma_start(out=dst_lat[r0:r0 + 128, :], in_=tm[:])

            pend = None
            for t in range(2, NT):
                cur = fl_s1(t)
                if pend is not None:
                    fl_s2(*pend)
                pend = cur
            fl_s2(*pend)

    k.layer1_mixer = layer1_mixer

    def dbg(name, ap, shape, dt=F32):
        o = p.dram(name, shape, dt, kind="ExternalOutput")
        p.sync.dma_start(out=o, in_=ap)
    k.dbg = dbg
    k.sb = dict(modcol=modcol, gabc=gabc, gmod1=gmod1, gmod2=gmod2, colT=colT)
    k.mod_stage = mod_stage
    k.layer0_mixer = layer0_mixer
    k.moe = moe
    k.p = p
    k.nc = nc
    k.drams = dict(x_in=x_in, ctx_in=ctx_in, xl1=xl1, xc1=xc1, xl2=xl2, xc2=xc2, xl3=xl3, out=out)
    return k


def dn_consts():
    i = np.arange(64)
    out = np.zeros((64, 5, 128), np.float32)
    U0 = (i[:, None] <= i[None, :]).astype(np.float32)
    U1 = (i[:, None] >= i[None, :]).astype(np.float32)
    out[:, 0, :64] = -U0; out[:, 0, 64:] = U0
    out[:, 1, :64] = -U1; out[:, 1, 64:] = U1
    out[:, 2, :64] = 1.0; out[:, 2, 64:] = -1.0
    NEG = -30000.0
    out[:, 3, :64] = np.where(i[:, None] > i[None, :], 0.0, NEG)
    out[:, 3, 64:] = np.where(i[None, :] >= i[:, None], 0.0, NEG)
    out[:, 4, :64] = np.where(i[:, None] < i[None, :], 0.0, NEG)
    out[:, 4, 64:] = np.where(i[None, :] <= i[:, None], 0.0, NEG)
    return out

def host_inputs(inputs, b):
    f = lambda a: np.ascontiguousarray(np.asarray(a, dtype=np.float32))
    rows = S // 64
    row = np.repeat(np.arange(rows), 64); col = np.tile(np.arange(64), rows)
    freqs = (10000.0 ** (-np.arange(16, dtype=np.float32) / 16)).astype(np.float32)
    ang = np.stack([row, col], -1).astype(np.float32)[..., None] * freqs
    m = {
        "x": f(inputs["x"][b]), "ctx": f(inputs["ctx"][b]), "c": f(inputs["c"][b]).reshape(8, 128),
        "c_ctx": f(inputs["c_ctx"]).reshape(8, 128), "mod_w": f(inputs["mod_w"]),
        "mod_b": f(inputs["mod_b"]).reshape(2, 48, 128), "norm1_g": f(inputs["norm1_g"]).reshape(2, 8, 128),
        "norm2_g": f(inputs["norm2_g"]).reshape(2, 8, 128), "final_norm_g": f(inputs["final_norm_g"]).reshape(1, D),
        "ab_w_in": f(inputs["ab_w_in"][0]), "ab_q_norm": f(inputs["ab_q_norm"]), "ab_k_norm": f(inputs["ab_k_norm"]),
        "gm_ln_g": f(inputs["gm_ln_g"]), "gm_ln_b": f(inputs["gm_ln_b"]), "gm_w_s": f(inputs["gm_w_s"][0]),
        "gm_b_s": f(inputs["gm_b_s"][0]).reshape(1, 512), "ab_w_out": f(inputs["ab_w_out"][0]),
        "router_w": f(inputs["router_w"]), "router_b": f(inputs["router_b"]).reshape(1, NE),
        "moe_w_gate": f(inputs["moe_w_gate"]), "moe_w_up": f(inputs["moe_w_up"]), "moe_w_down": f(inputs["moe_w_down"]),
        "ident": np.eye(128, dtype=np.float32),
        "rope_cos": np.cos(ang).reshape(S, 32).astype(np.float32), "rope_sin": np.sin(ang).reshape(S, 32).astype(np.float32),
        "cd_w_in": f(inputs["cd_w_in"][0]), "cv_dw_w": f(inputs["cv_dw_w"][0]),
        "cv_vec": np.concatenate([f(inputs["cv_dw_b"][0]).reshape(4, 128), f(inputs["cv_ln_g"][0]).reshape(4, 128), f(inputs["cv_ln_b"][0]).reshape(4, 128)], 0),
        "dn_conv_w": f(inputs["dn_conv_w"][0]), "dn_a_log": f(inputs["dn_a_log"][0]).reshape(1, 8),
        "dn_dt_bias": f(inputs["dn_dt_bias"][0]).reshape(1, 8), "dn_o_norm": f(inputs["dn_o_norm"]).reshape(1, 128),
        "cd_w_out": f(inputs["cd_w_out"][0]), "dn_consts": dn_consts(),
        "su_mat": np.triu(np.ones((128, 128), np.float32), 1),
        "thr512": (512.0 * np.arange(40, dtype=np.float32)).reshape(1, 40),
        "base8": (np.arange(8, dtype=np.float32)[None, :] * 128 + np.arange(128, dtype=np.float32)[:, None]),
        "base4": (np.arange(4, dtype=np.float32)[None, :] * 128 + np.arange(128, dtype=np.float32)[:, None]),
        "zeros_bf": np.zeros((512, D), ml_dtypes.bfloat16),
    }
    return m


def build_full():
    k = build(debug=False)
    d = k.drams
    k.precast_iter = k.precast_weights()
    k.mod_stage(0)
    k.layer0_mixer()
    k.moe_sparse(0, (d["xl1"], d["xc1"]), (d["xl2"], d["xc2"]), list(range(NT)))
    k.mod_stage(1)
    k.layer1_mixer(d["xl2"], d["xc2"], d["xl3"])
    k.moe_sparse(1, (d["xl3"], d["xc2"]), (d["out"], d["xc2"]), list(range(2, NT)), final=True)
    k.p.emit()
    return k.nc


def kernel(**inputs):
    nb = inputs["x"].shape[0]
    nc = build_full()
    shared = host_inputs(inputs, 0)
    in_maps = []
    for b in range(nb):
        m = dict(shared)
        m["x"] = np.ascontiguousarray(np.asarray(inputs["x"][b], dtype=np.float32))
        m["ctx"] = np.ascontiguousarray(np.asarray(inputs["ctx"][b], dtype=np.float32))
        m["c"] = np.ascontiguousarray(np.asarray(inputs["c"][b], dtype=np.float32)).reshape(8, 128)
        in_maps.append(m)
    res = run_bass_kernel_spmd(nc, in_maps, core_ids=list(range(nb)))
    return np.stack([np.asarray(r["out"], dtype=np.float32) for r in res.results], axis=0)
```

```python
import os
import contextlib
import numpy as np
import ml_dtypes
import concourse.bass as bass
import concourse.mybir as mybir
from concourse.bass_utils import run_bass_kernel_spmd


F32 = mybir.dt.float32
BF16 = mybir.dt.bfloat16
AF = mybir.ActivationFunctionType
ALU = mybir.AluOpType
AX = mybir.AxisListType

ENGS = ["sync", "scalar", "vector", "gpsimd", "tensor"]


def _box(ap):
    t = ap.tensor
    name = ap.name
    dsz = mybir.dt.size(ap.dtype)
    pairs = list(ap.ap)
    off = int(ap.offset)
    space = str(ap.space)
    if space in ("SB", "PSUM"):
        pstep, pcnt = pairs[0]
        rest = pairs[1:]
        if pstep == 0:
            p0, f0 = 0, off
            pcnt_eff = 1
        else:
            p0, f0 = off // pstep, off % pstep
            pcnt_eff = pcnt
        ext = 0
        for st, cn in rest:
            ext += abs(st) * (cn - 1)
        if space == "PSUM":
            return (name, 0, 128, 0, 1 << 20)
        return (name, p0, p0 + pcnt_eff, f0 * dsz, (f0 + ext + 1) * dsz)
    else:
        ext = 0
        for st, cn in pairs:
            ext += abs(st) * (cn - 1)
        return (name, 0, 1, off * dsz, (off + ext + 1) * dsz)


def _ovl(a, b):
    return a[1] < b[2] and b[1] < a[2] and a[3] < b[4] and b[3] < a[4]


def _covers(a, b):
    return a[1] <= b[1] and a[2] >= b[2] and a[3] <= b[3] and a[4] >= b[4]


class _EngProxy:
    def __init__(self, prog, name):
        self._p = prog
        self._n = name

    def __getattr__(self, meth):
        def call(**kw):
            reads, writes = [], []
            for k, v in kw.items():
                if v is not None and k in ("out_offset", "in_offset") and hasattr(v, "ap"):
                    reads.append(v.ap)
                    continue
                if v is None or not hasattr(v, "ap") or not hasattr(v, "tensor"):
                    continue
                if k in ("out", "accum_out", "ap"):
                    writes.append(v)
                else:
                    reads.append(v)
            if meth == "matmul" and kw.get("start") is False:
                pass
            self._p.add(self._n, meth, kw, reads, writes)
        return call


class Prog:
    def __init__(self, nc, n_dma_sems=12, epoch=20000):
        self.nc = nc
        self.ops = []
        self.hist = {}
        self.epoch = epoch
        self.n_dma_sems = n_dma_sems
        self.sync = _EngProxy(self, "sync")
        self.scalar = _EngProxy(self, "scalar")
        self.vector = _EngProxy(self, "vector")
        self.gpsimd = _EngProxy(self, "gpsimd")
        self.tensor = _EngProxy(self, "tensor")
        self._psum_banks = None
        self._psum_i = 0
        self._uid = 0
        self._stacks = []
        self._bar_from = 0
        self._bar_tails = set()
        self._pending = {}

    def sb(self, name, shape, dtype):
        self._uid += 1
        nm = f"{name}_{self._uid}"
        if self._stacks:
            return self._stacks[-1].enter_context(self.nc.sbuf_tensor(nm, list(shape), dtype))
        return self.nc.alloc_sbuf_tensor(nm, list(shape), dtype)

    def pool(self, name, shape, dtype, bufs=2):
        ts = [self.sb(f"{name}{i}", shape, dtype) for i in range(bufs)]
        return _Pool(ts)

    def scope(self):
        return _Scope(self)

    def barrier(self):
        tails = set()
        last = {}
        for i in range(self._bar_from, len(self.ops)):
            op = self.ops[i]
            if op["dma"]:
                tails.add(i)
            else:
                last[op["eng"]] = i
        tails |= set(last.values())
        tails |= self._bar_tails
        self._bar_tails = set(last.values())
        self._bar_from = len(self.ops)
        self._pending = {e: set(tails) | self._pending.get(e, set()) for e in ENGS}

    def psum_init(self):
        self._psum_banks = [self.nc.alloc_psum_tensor(f"psb{i}", [128, 512], F32) for i in range(8)]

    def psum(self, dtype=F32):
        t = self._psum_banks[self._psum_i % 8]
        self._psum_i += 1
        a = t[:]
        if dtype != F32:
            a = a.bitcast(dtype)
        return a

    def dram(self, name, shape, dtype, kind="Internal"):
        return self.nc.dram_tensor(name, list(shape), dtype, kind=kind).ap()

    def add(self, eng, meth, kw, reads, writes):
        idx = len(self.ops)
        deps = set()
        if eng in self._pending:
            deps |= self._pending.pop(eng)
        rb = [_box(a) for a in reads]
        wb = [_box(a) for a in writes]
        wb = wb + [b for b in rb if b[4] == (1 << 20)]
        for b in rb:
            for (ob, oi, kind) in self.hist.get(b[0], ()):
                if kind == "w" and _ovl(b, ob):
                    deps.add(oi)
        for b in wb:
            for (ob, oi, kind) in self.hist.get(b[0], ()):
                if _ovl(b, ob):
                    deps.add(oi)
        for b in wb:
            lst = self.hist.setdefault(b[0], [])
            lst[:] = [e for e in lst if not _covers(b, e[0])]
            lst.append((b, idx, "w"))
        for b in rb:
            lst = self.hist.setdefault(b[0], [])
            lst[:] = [e for e in lst if not (e[2] == "r" and e[0] == b and (e[1] == idx or self.ops[e[1]]["eng"] == eng))]
            lst.append((b, idx, "r"))
        self.ops.append(dict(eng=eng, meth=meth, kw=kw, deps=deps, dma=(meth in ("dma_start", "indirect_dma_start"))))

    def emit(self):
        nc = self.nc
        ops = self.ops
        eng_count = {e: 0 for e in ENGS}
        eng_sems = {e: [] for e in ENGS}
        dma_sems = {e: [] for e in ENGS}
        dma_cnt = {}
        dma_rr = {e: 0 for e in ENGS}
        dma_last = {}
        for i, op in enumerate(ops):
            e = op["eng"]
            if op["dma"]:
                if not dma_sems[e]:
                    n = self.n_dma_sems if e == "sync" else 6
                    dma_sems[e] = [nc.alloc_semaphore(f"dq_{e}_{k}") for k in range(n)]
                k = dma_rr[e] % len(dma_sems[e])
                dma_rr[e] += 1
                sem = dma_sems[e][k]
                key = (e, k)
                prev = dma_last.get(key)
                if prev is not None:
                    op["deps"].add(prev)
                dma_last[key] = i
                c = dma_cnt.get(key, 0) + 1
                dma_cnt[key] = c
                op["tok"] = (sem, 16 * c, key)
                op["inc"] = 16
            else:
                c = eng_count[e]
                ep = c // self.epoch
                if ep >= len(eng_sems[e]):
                    eng_sems[e].append(nc.alloc_semaphore(f"es_{e}_{ep}"))
                sem = eng_sems[e][ep]
                eng_count[e] = c + 1
                op["tok"] = (sem, (c % self.epoch) + 1, (e, "c", ep))
                op["inc"] = 1
        self.final_dma = [(dma_sems[e][k], 16 * c) for (e, k), c in dma_cnt.items()]
        per_eng = {e: [i for i, op in enumerate(ops) if op["eng"] == e] for e in ENGS}
        self.n_waits = 0

        def run(engname, eobj):
            seen = {}
            for i in per_eng[engname]:
                op = ops[i]
                need = {}
                for d in op["deps"]:
                    dop = ops[d]
                    if dop["eng"] == engname and not dop["dma"] and engname == "tensor":
                        continue
                    sem, val, key = dop["tok"]
                    if seen.get(key, 0) >= val:
                        continue
                    if key not in need or need[key][1] < val:
                        need[key] = (sem, val)
                for key, (sem, val) in need.items():
                    eobj.wait_ge(sem, val)
                    seen[key] = val
                    self.n_waits += 1
                try:
                    ins = getattr(eobj, op["meth"])(**op["kw"])
                except Exception:
                    print("FAILED OP", engname, op["meth"], {k_: (getattr(v_, "shape", None), getattr(v_, "name", None)) for k_, v_ in op["kw"].items()})
                    raise
                sem, val, key = op["tok"]
                ins.then_inc(sem, op["inc"])
            if engname == "sync":
                for sem, val in self.final_dma:
                    eobj.wait_ge(sem, val)

        with nc.Block() as block:
            @block.sync
            def _(e):
                run("sync", e)

            @block.scalar
            def _(e):
                run("scalar", e)

            @block.vector
            def _(e):
                run("vector", e)

            @block.gpsimd
            def _(e):
                run("gpsimd", e)

            @block.tensor
            def _(e):
                run("tensor", e)


class _Pool:
    def __init__(self, ts):
        self.ts = ts
        self.i = 0

    def next(self):
        t = self.ts[self.i % len(self.ts)]
        self.i += 1
        return t


class _Scope:
    def __init__(self, p):
        self.p = p

    def __enter__(self):
        st = contextlib.ExitStack()
        st.__enter__()
        self.p._stacks.append(st)
        return self

    def __exit__(self, *a):
        st = self.p._stacks.pop()
        st.__exit__(None, None, None)
        self.p.barrier()
        return False


D = 1024
S = 4096
CL = 256
NT = 34
EPS = 1e-6
NE = 16
FF = 512


def fap(ap, dims):
    return bass.AP(ap.tensor, ap.offset, [list(ap.ap[0])] + [list(d) for d in dims])


class PsumPool:
    def __init__(self, p, banks):
        self.p = p
        self.banks = banks
        self.i = 0

    def next(self, dtype=F32):
        t = self.p._psum_banks[self.banks[self.i % len(self.banks)]]
        self.i += 1
        a = t[:]
        if dtype != F32:
            a = a.bitcast(dtype)
        return a


class K:
    pass


def rstd_from_ss(p, out, ss, n, tmp):
    p.vector.tensor_scalar(out=tmp, in0=ss, scalar1=1.0 / n, scalar2=EPS, op0=ALU.mult, op1=ALU.add)
    p.scalar.activation(out=tmp, in_=tmp, func=AF.Sqrt)
    p.vector.reciprocal(out=out, in_=tmp)


def build(debug=False, stop_after=None):
    nc = bass.Bass("TRN2", target_bir_lowering=False)
    p = Prog(nc)
    p.psum_init()
    k = K()
    IN = lambda name, shape, dt=F32: p.dram(name, shape, dt, kind="ExternalInput")
    x_in = IN("x", [S, D])
    ctx_in = IN("ctx", [CL, D])
    c_in = IN("c", [8, 128])
    cc_in = IN("c_ctx", [8, 128])
    mod_w = IN("mod_w", [2, D, 6 * D])
    mod_b = IN("mod_b", [2, 48, 128])
    n1g = IN("norm1_g", [2, 8, 128])
    n2g = IN("norm2_g", [2, 8, 128])
    fng = IN("final_norm_g", [1, D])
    ab_w_in = IN("ab_w_in", [D, 1792])
    ab_qn = IN("ab_q_norm", [1, 64])
    ab_kn = IN("ab_k_norm", [1, 64])
    gm_lng = IN("gm_ln_g", [1, 512])
    gm_lnb = IN("gm_ln_b", [1, 512])
    gm_ws = IN("gm_w_s", [4, 128, 128])
    gm_bs = IN("gm_b_s", [1, 512])
    ab_w_out = IN("ab_w_out", [D, D])
    router_w = IN("router_w", [D, NE])
    router_b = IN("router_b", [1, NE])
    w_gate = IN("moe_w_gate", [2, NE, D, FF])
    w_up = IN("moe_w_up", [2, NE, D, FF])
    w_down = IN("moe_w_down", [2, NE, FF, D])
    ident_in = IN("ident", [128, 128])
    cos_in = IN("rope_cos", [S, 32])
    sin_in = IN("rope_sin", [S, 32])
    out = p.dram("out", [S, D], F32, kind="ExternalOutput")

    dk = "ExternalOutput" if debug else "Internal"
    xl1 = p.dram("xl1", [S, D], F32, kind=dk)
    xc1 = p.dram("xc1", [CL, D], F32, kind=dk)
    xl2 = p.dram("xl2", [S, D], F32, kind=dk)
    xc2 = p.dram("xc2", [CL, D], F32, kind=dk)
    xl3 = p.dram("xl3", [S, D], F32, kind=dk)

    def tile_ap(lat, ctx, t):
        if t < 2:
            return ctx[t * 128:(t + 1) * 128, :]
        return lat[(t - 2) * 128:(t - 1) * 128, :]

    idt = p.sb("idt", [128, 128], F32)
    idb = p.sb("idb", [128, 128], BF16)
    ones_f = p.sb("ones_f", [128, 128], F32)
    p.sync.dma_start(out=idt[:], in_=ident_in)
    p.vector.tensor_copy(out=idb[:], in_=idt[:])
    p.vector.memset(ap=ones_f[:], constant=1.0)

    ps_all = PsumPool(p, list(range(8)))

    colT = p.sb("colT", [128, 96], F32)
    cs = p.sb("cs", [128, 8, 2], F32)
    modcol = p.sb("modcol", [128, 48, 2], F32)
    gmod1 = p.sb("gmod1", [128, 8, 2], F32)
    gmod2 = p.sb("gmod2", [128, 8, 2], F32)
    gabc = p.sb("gabc", [128, 4, D], F32)
    fng_bc = p.sb("fng_bc", [128, D], F32)
    p.sync.dma_start(out=fng_bc[:], in_=fng.partition_broadcast(128))
    small = p.pool("small", [128, 16], F32, bufs=8)
    junk = p.pool("junk", [128, D], BF16, bufs=2)

    def mod_stage(l):
      with p.scope():
        stg = p.sb(f"stg{l}", [96, 128], F32)
        p.sync.dma_start(out=stg[0:48, :], in_=mod_b[l])
        p.sync.dma_start(out=stg[48:56, :], in_=c_in)
        p.sync.dma_start(out=stg[56:64, :], in_=cc_in)
        p.sync.dma_start(out=stg[64:72, :], in_=n1g[l])
        p.sync.dma_start(out=stg[72:80, :], in_=n2g[l])
        pt = ps_all.next()
        p.tensor.transpose(out=pt[:, 0:80], in_=stg[0:80, :], identity=idt[0:80, 0:80])
        p.vector.tensor_copy(out=colT[:, 0:80], in_=pt[:, 0:80])
        p.scalar.activation(out=cs[:, :, 0], in_=colT[:, 48:56], func=AF.Silu)
        p.scalar.activation(out=cs[:, :, 1], in_=colT[:, 56:64], func=AF.Silu)
        wm = p.pool(f"wm{l}", [128, 8, 512], F32, bufs=2)
        pm = ps_all.next()
        for pn in range(12):
            w = wm.next()
            src = mod_w[l, :, pn * 512:(pn + 1) * 512].rearrange("(kc p) n -> p kc n", p=128)
            p.sync.dma_start(out=w[:, 0:4, :], in_=src[:, 0:4, :])
            p.sync.dma_start(out=w[:, 4:8, :], in_=src[:, 4:8, :])
            for jj in range(4):
                j = pn * 4 + jj
                for kc in range(8):
                    p.tensor.matmul(out=pm[:, 2 * j:2 * j + 2], lhsT=w[:, kc, jj * 128:(jj + 1) * 128], rhs=cs[:, kc, :],
                                    start=(kc == 0), stop=(kc == 7))
        p.vector.tensor_tensor(out=modcol[:], in0=fap(pm[:, 0:1], [[2, 48], [1, 2]]),
                               in1=fap(colT[:, 0:1], [[1, 48], [0, 2]]), op=ALU.add)
        p.vector.scalar_tensor_tensor(out=gmod1[:], in0=modcol[:, 8:16, :], scalar=1.0,
                                      in1=fap(colT[:, 64:65], [[1, 8], [0, 2]]), op0=ALU.add, op1=ALU.mult)
        p.vector.scalar_tensor_tensor(out=gmod2[:], in0=modcol[:, 32:40, :], scalar=1.0,
                                      in1=fap(colT[:, 72:73], [[1, 8], [0, 2]]), op0=ALU.add, op1=ALU.mult)
        dg = p.pool(f"dg{l}", [128, 128], F32, bufs=2)
        for vi, v in enumerate((2, 5)):
            for w_ in range(2):
                for half in range(2):
                    pb = ps_all.next()
                    for q in range(4):
                        kc = half * 4 + q
                        d_ = dg.next()
                        p.vector.tensor_scalar(out=d_[:], in0=idt[:], scalar1=modcol[:, v * 8 + kc, w_:w_ + 1],
                                               scalar2=None, op0=ALU.mult)
                        p.tensor.matmul(out=pb[:, q * 128:(q + 1) * 128], lhsT=ones_f[:], rhs=d_[:],
                                        start=True, stop=True)
                    p.scalar.copy(out=gabc[:, vi * 2 + w_, half * 512:(half + 1) * 512], in_=pb[:, :])

    xpool = p.pool("xpool", [128, D], F32, bufs=3)
    xspool = p.pool("xspool", [128, D], F32, bufs=2)

    def norm_tile(src_ap, gmod, shift_v, w_, out_fn):
        NSTOP = int(os.environ.get("NSTOP", "99"))
        xt = xpool.next()
        p.sync.dma_start(out=xt[:], in_=src_ap)
        jk = junk.next()
        sm = small.next()
        if NSTOP < 1:
            return xt
        p.scalar.activation(out=jk[:], in_=xt[:], func=AF.Square, accum_out=sm[:, 0:1])
        if NSTOP < 2:
            return xt
        rstd_from_ss(p, sm[:, 2:3], sm[:, 0:1], D, sm[:, 1:2])
        if NSTOP < 3:
            return xt
        xs = xspool.next()
        p.vector.tensor_scalar(out=xs[:], in0=xt[:], scalar1=sm[:, 2:3], scalar2=None, op0=ALU.mult)
        if NSTOP < 4:
            return xt
        for half in range(2):
            pt = ps_all.next()
            for q in range(4):
                kc = half * 4 + q
                p.tensor.transpose(out=pt[:, q * 128:(q + 1) * 128], in_=xs[:, kc * 128:(kc + 1) * 128], identity=idt[:])
            if NSTOP < 5:
                continue
            for q in range(4):
                kc = half * 4 + q
                AV = os.environ.get("AV", "0")
                if AV == "0":
                    p.scalar.activation(out=out_fn(kc), in_=pt[:, q * 128:(q + 1) * 128], func=AF.Identity,
                                        bias=modcol[:, shift_v * 8 + kc, w_:w_ + 1], scale=gmod[:, kc, w_:w_ + 1])
                elif AV == "1":
                    p.scalar.activation(out=out_fn(kc), in_=pt[:, q * 128:(q + 1) * 128], func=AF.Identity,
                                        scale=gmod[:, kc, w_:w_ + 1])
                elif AV == "2":
                    p.scalar.activation(out=out_fn(kc), in_=pt[:, q * 128:(q + 1) * 128], func=AF.Identity,
                                        bias=modcol[:, shift_v * 8 + kc, w_:w_ + 1])
                elif AV == "3":
                    p.scalar.activation(out=out_fn(kc), in_=pt[:, q * 128:(q + 1) * 128], func=AF.Copy)
                elif AV == "4":
                    p.vector.tensor_scalar(out=out_fn(kc), in0=pt[:, q * 128:(q + 1) * 128], scalar1=gmod[:, kc, w_:w_ + 1],
                                           scalar2=modcol[:, shift_v * 8 + kc, w_:w_ + 1], op0=ALU.mult, op1=ALU.add)
        k.last_xs = xs
        return xt

    blocks = [(0, 2)] + [(2 + 4 * i, 4) for i in range(8)]
    slT_d = p.dram("slT_d", [4, 128, NT * 128], BF16, kind=dk)
    oT_d = p.dram("oT_d", [4, 128, NT * 128], BF16, kind=dk)
    L0STOP = os.environ.get("L0STOP", "")

    def layer0_mixer():
      with p.scope():
        qT = p.sb("qT", [128, 4, NT * 128], BF16)
        kT = p.sb("kT", [128, NT * 128], BF16)
        va0 = p.sb("va0", [128, NT, 128], BF16)
        va1 = p.sb("va1", [128, NT, 128], BF16)
        p.gpsimd.memset(ap=va0[:, :, 64:128], constant=1.0)
        p.gpsimd.memset(ap=va1[:, :, 0:64], constant=1.0)
        with p.scope():
            win = p.sb("win", [128, 8, 1792], BF16)
            for kc in range(8):
                for w_ in range(2):
                    p.gpsimd.dma_start(out=fap(win[:, kc, w_ * 64:w_ * 64 + 1], [[128, 4], [1, 64]]),
                                       in_=ab_w_in[kc * 128:(kc + 1) * 128, w_ * 256:(w_ + 1) * 256].rearrange("p (j d) -> p j d", j=4))
                p.gpsimd.dma_start(out=win[:, kc, 512:1792], in_=ab_w_in[kc * 128:(kc + 1) * 128, 512:1792])
            gq = p.sb("gq", [128, 64], F32)
            gk = p.sb("gk", [128, 64], F32)
            lng = p.sb("lng", [128, 512], F32)
            lnb = p.sb("lnb", [128, 512], F32)
            bsb = p.sb("bsb", [128, 512], F32)
            p.sync.dma_start(out=gq[:], in_=ab_qn.partition_broadcast(128))
            p.sync.dma_start(out=gk[:], in_=ab_kn.partition_broadcast(128))
            p.sync.dma_start(out=lng[:], in_=gm_lng.partition_broadcast(128))
            p.sync.dma_start(out=lnb[:], in_=gm_lnb.partition_broadcast(128))
            p.sync.dma_start(out=bsb[:], in_=gm_bs.partition_broadcast(128))
            wsT = p.sb("wsT", [128, 4, 128], BF16)
            wsf = p.sb("wsf", [128, 4, 128], F32)
            p.sync.dma_start(out=wsf[:], in_=gm_ws.rearrange("g p q -> p g q"))
            pw = ps_all.next()
            for g in range(4):
                p.tensor.transpose(out=pw[:, g * 128:(g + 1) * 128], in_=wsf[:, g, :], identity=idt[:])
            p.vector.tensor_copy(out=wsT[:], in_=pw[:, :].rearrange("p (g q) -> p g q", g=4))

            hTp = p.pool("hTp", [128, 8, 512], BF16, bufs=2)
            guTp = p.pool("guTp", [128, 4, 512], BF16, bufs=3)
            sqq = p.pool("sqq", [128, 640], F32, bufs=1)
            qnp = p.pool("qnp", [128, 640], F32, bufs=1)
            qrp = p.pool("qrp", [128, 640], BF16, bufs=2)
            rt = p.pool("rt", [128, 4, 320], F32, bufs=1)
            csp = p.pool("csp", [128, 64], F32, bufs=2)
            gvp = p.pool("gvp", [128, 512], F32, bufs=2)
            vnp = p.pool("vnp", [128, 512], F32, bufs=1)
            vnbp = p.pool("vnbp", [128, 512], BF16, bufs=2)
            s1p = p.pool("s1p", [128, 512], F32, bufs=1)
            slp = p.pool("slp", [128, 4, 128], BF16, bufs=2)

            if L0STOP == "S":
                k.dbg("d_win", win[:], [128, 8, 1792], BF16)
                k.dbg("d_wsT", wsT[:], [128, 4, 128], BF16)
                return
            BLIM = int(os.environ.get("BLIM", "99"))
            SUB = int(os.environ.get("SUB", "99"))
            def sb_s1(t0, ntl):
                N = ntl * 128
                hT = hTp.next()
                for ti in range(ntl):
                    t = t0 + ti
                    w_ = 1 if t < 2 else 0
                    norm_tile(tile_ap(x_in, ctx_in, t), gmod1, 0, w_,
                              lambda kc, hT=hT, ti=ti: hT[:, kc, ti * 128:(ti + 1) * 128])
                guT = guTp.next()
                for g in range(4):
                    pu = ps_all.next()
                    for kc in range(8):
                        p.tensor.matmul(out=pu[:, 0:N], lhsT=win[:, kc, 768 + g * 128:768 + (g + 1) * 128],
                                        rhs=hT[:, kc, 0:N], start=(kc == 0), stop=(kc == 7))
                    p.scalar.activation(out=guT[:, g, 0:N], in_=pu[:, 0:N], func=AF.Gelu)
                return (t0, ntl, hT, guT)

            def sb_A(t0, ntl, hT, guT, ti):
                t = t0 + ti
                tok = t * 128
                lat = t >= 2
                pq = ps_all.next()
                pkv = ps_all.next()
                pvg = ps_all.next()
                for (pp, c0, c1) in ((pq, 0, 512), (pkv, 512, 768), (pvg, 1280, 1792)):
                    for kc in range(8):
                        p.tensor.matmul(out=pp[:, 0:c1 - c0], lhsT=hT[:, kc, ti * 128:(ti + 1) * 128],
                                        rhs=win[:, kc, c0:c1], start=(kc == 0), stop=(kc == 7))
                sq = sqq.next()
                sm = small.next()
                p.scalar.activation(out=sq[:, 0:512], in_=pq[:, 0:512], func=AF.Square)
                p.scalar.activation(out=sq[:, 512:640], in_=pkv[:, 0:128], func=AF.Square)
                p.vector.tensor_reduce(out=sm[:, 0:10], in_=sq[:].rearrange("p (h d) -> p h d", d=64),
                                       axis=AX.X, op=ALU.add)
                sm2 = small.next()
                rstd_from_ss(p, sm2[:, 0:10], sm[:, 0:10], 64, sm[:, 0:10])
                qn = qnp.next()
                p.vector.tensor_tensor(out=qn[:, 0:512].rearrange("p (h d) -> p h d", d=64),
                                       in0=pq[:, 0:512].rearrange("p (h d) -> p h d", d=64),
                                       in1=fap(sm2[:, 0:1], [[1, 8], [0, 64]]), op=ALU.mult)
                p.vector.tensor_tensor(out=qn[:, 512:640].rearrange("p (h d) -> p h d", d=64),
                                       in0=pkv[:, 0:128].rearrange("p (h d) -> p h d", d=64),
                                       in1=fap(sm2[:, 8:9], [[1, 2], [0, 64]]), op=ALU.mult)
                p.gpsimd.tensor_tensor(out=qn[:, 0:512].rearrange("p (h d) -> p h d", d=64),
                                       in0=qn[:, 0:512].rearrange("p (h d) -> p h d", d=64),
                                       in1=fap(gq[:, 0:1], [[0, 8], [1, 64]]), op=ALU.mult)
                p.gpsimd.tensor_tensor(out=qn[:, 512:640].rearrange("p (h d) -> p h d", d=64),
                                       in0=qn[:, 512:640].rearrange("p (h d) -> p h d", d=64),
                                       in1=fap(gk[:, 0:1], [[0, 2], [1, 64]]), op=ALU.mult)
                qr = qrp.next()
                if lat:
                    cst = csp.next()
                    p.sync.dma_start(out=cst[:, 0:32], in_=cos_in[(t - 2) * 128:(t - 1) * 128, :])
                    p.sync.dma_start(out=cst[:, 32:64], in_=sin_in[(t - 2) * 128:(t - 1) * 128, :])
                    dims = [[64, 10], [32, 2], [1, 16]]
                    x1 = fap(qn[:, 0:1], dims)
                    x2 = fap(qn[:, 16:17], dims)
                    o1 = fap(qr[:, 0:1], dims)
                    o2 = fap(qr[:, 16:17], dims)
                    cb = fap(cst[:, 0:1], [[0, 10], [16, 2], [1, 16]])
                    sb_ = fap(cst[:, 32:33], [[0, 10], [16, 2], [1, 16]])
                    r = rt.next()
                    tv = lambda i: fap(r[:, i, 0:1], [[32, 10], [16, 2], [1, 16]])
                    p.vector.tensor_tensor(out=tv(0), in0=x1, in1=cb, op=ALU.mult)
                    p.gpsimd.tensor_tensor(out=tv(1), in0=x2, in1=sb_, op=ALU.mult)
                    p.vector.tensor_tensor(out=tv(2), in0=x1, in1=sb_, op=ALU.mult)
                    p.gpsimd.tensor_tensor(out=tv(3), in0=x2, in1=cb, op=ALU.mult)
                    p.vector.tensor_tensor(out=o1, in0=tv(0), in1=tv(1), op=ALU.subtract)
                    p.gpsimd.tensor_tensor(out=o2, in0=tv(3), in1=tv(2), op=ALU.add)
                else:
                    p.vector.tensor_copy(out=qr[:], in_=qn[:])
                k.precast_step(3)
                p.scalar.copy(out=va0[:, t, 0:64], in_=pkv[:, 128:192])
                p.scalar.copy(out=va1[:, t, 64:128], in_=pkv[:, 192:256])
                gv = gvp.next()
                sm3 = small.next()
                p.scalar.activation(out=gv[:], in_=pvg[:, :], func=AF.Gelu, accum_out=sm3[:, 0:1])
                p.vector.tensor_scalar(out=sm3[:, 1:2], in0=sm3[:, 0:1], scalar1=-1.0 / 512, scalar2=None, op0=ALU.mult)
                jk = junk.next()
                p.scalar.activation(out=jk[:, 0:512], in_=gv[:], func=AF.Square, bias=sm3[:, 1:2], scale=1.0,
                                    accum_out=sm3[:, 2:3])
                rstd_from_ss(p, sm3[:, 4:5], sm3[:, 2:3], 512, sm3[:, 3:4])
                vn = vnp.next()
                p.vector.tensor_scalar(out=vn[:], in0=gv[:], scalar1=sm3[:, 1:2], scalar2=sm3[:, 4:5],
                                       op0=ALU.add, op1=ALU.mult)
                p.gpsimd.tensor_tensor(out=vn[:], in0=vn[:], in1=lng[:], op=ALU.mult)
                vnb = vnbp.next()
                p.gpsimd.tensor_tensor(out=vnb[:], in0=vn[:], in1=lnb[:], op=ALU.add)
                return (t, tok, ti, qr, vnb, guT)

            def sb_B(t, tok, ti, qr, vnb, guT):
                ptq = ps_all.next(BF16)
                for j in range(4):
                    p.tensor.transpose(out=ptq[:, j * 128:(j + 1) * 128],
                                       in_=qr[:, j * 128:(j + 1) * 128], identity=idb[:])
                p.tensor.transpose(out=ptq[:, 512:640], in_=qr[:, 512:640], identity=idb[:])
                p.vector.tensor_copy(out=qT[:, :, tok:tok + 128],
                                     in_=ptq[:, 0:512].rearrange("p (j q) -> p j q", j=4))
                p.vector.tensor_copy(out=kT[:, tok:tok + 128], in_=ptq[:, 512:640])
                psp = ps_all.next()
                for g in range(4):
                    p.tensor.matmul(out=psp[:, g * 128:(g + 1) * 128], lhsT=vnb[:, g * 128:(g + 1) * 128],
                                    rhs=wsT[:, g, :], start=True, stop=True)
                s1 = s1p.next()
                p.vector.tensor_tensor(out=s1[:], in0=psp[:, :], in1=bsb[:], op=ALU.add)
                sl = slp.next()
                p.gpsimd.tensor_tensor(out=sl[:], in0=s1[:].rearrange("p (g q) -> p g q", g=4),
                                       in1=guT[:, :, ti * 128:(ti + 1) * 128], op=ALU.mult)
                p.sync.dma_start(out=slT_d[:, :, tok:tok + 128].rearrange("g c t -> c g t"), in_=sl[:])


            pendA = None
            pend = None
            for (t0, ntl) in blocks:
                cur = sb_s1(t0, ntl)
                if pend is not None:
                    (pt0, pntl, phT, pguT) = pend
                    for ti in range(pntl):
                        a = sb_A(pt0, pntl, phT, pguT, ti)
                        if pendA is not None:
                            sb_B(*pendA)
                        pendA = a
                pend = cur
            (pt0, pntl, phT, pguT) = pend
            for ti in range(pntl):
                a = sb_A(pt0, pntl, phT, pguT, ti)
                if pendA is not None:
                    sb_B(*pendA)
                pendA = a
            sb_B(*pendA)

        if L0STOP == "B":
            k.dbg("d_qT", qT[:], [128, 4, NT * 128], BF16)
            k.dbg("d_kT", kT[:], [128, NT * 128], BF16)
            k.dbg("d_va0", va0[:], [128, NT, 128], BF16)
            k.dbg("d_va1", va1[:], [128, NT, 128], BF16)
            return
        with p.scope():
            ps_s = PsumPool(p, [0, 1, 2, 3, 4, 5])
            ps_acc = PsumPool(p, [6, 7])
            ppool = p.pool("ppool", [128, 512], BF16, bufs=6)
            rdp = p.pool("rdp", [128, 512], F32, bufs=2)
            otp = p.pool("otp", [128, 512], BF16, bufs=2)
            vas = (va0, va1)
            qblocks = [(0, 256, [0, 1])] + [(256 + 512 * i, 512, list(range(NT))) for i in range(8)]
            items = []
            for j in range(4):
                for (q0, N, kts) in qblocks:
                    for w_ in range(2):
                        for ki, kt in enumerate(kts):
                            items.append((j, q0, N, w_, kt, ki == 0, ki == len(kts) - 1))
            LA = 3
            pTs = {}
            state = {}
            qpads = []
            for w_ in range(2):
                qp = p.pool(f"qpad{w_}", [128, 512], BF16, bufs=2)
                for t_ in qp.ts:
                    p.gpsimd.memset(ap=t_[:], constant=0.0)
                qpads.append(qp)
            for idx in range(len(items) + LA):
                if idx < len(items):
                    (j, q0, N, w_, kt, first, last) = items[idx]
                    b0 = 64 * w_
                    if first:
                        state["qp"] = qpads[w_].next()
                        p.gpsimd.tensor_copy(out=state["qp"][b0:b0 + 64, 0:N], in_=qT[b0:b0 + 64, j, q0:q0 + N])
                    ps = ps_s.next()
                    p.tensor.matmul(out=ps[:, 0:N], lhsT=kT[:, kt * 128:(kt + 1) * 128],
                                    rhs=state["qp"][:, 0:N], start=True, stop=True)
                    pT = ppool.next()
                    p.scalar.activation(out=pT[:, 0:N], in_=ps[:, 0:N], func=AF.Exp, scale=0.125)
                    pTs[idx] = pT
                if idx >= LA:
                    (j, q0, N, w_, kt, first, last) = items[idx - LA]
                    pT = pTs.pop(idx - LA)
                    if first:
                        state["acc"] = ps_acc.next()
                        if w_ == 0:
                            state["ot"] = otp.next()
                    acc = state["acc"]
                    ot = state["ot"]
                    p.tensor.matmul(out=acc[:, 0:N], lhsT=vas[w_][:, kt, :], rhs=pT[:, 0:N], start=first, stop=last)
                    if last:
                        ob, db = (0, 64) if w_ == 0 else (64, 0)
                        rd = rdp.next()
                        p.vector.reciprocal(out=rd[ob:ob + 64, 0:N], in_=acc[db:db + 64, 0:N])
                        p.vector.tensor_tensor(out=ot[ob:ob + 64, 0:N], in0=acc[ob:ob + 64, 0:N],
                                               in1=rd[ob:ob + 64, 0:N], op=ALU.mult)
                        if w_ == 1:
                            p.sync.dma_start(out=oT_d[j, :, q0:q0 + N], in_=ot[:, 0:N])

      if L0STOP in ("B", "C"):
          return
      with p.scope():
        wo = p.sb("wo", [128, 8, D], BF16)
        for c_ in range(8):
            if c_ < 4:
                for w_ in range(2):
                    r0 = (c_ + 4 * w_) * 64
                    p.gpsimd.dma_start(out=wo[64 * w_:64 * w_ + 64, c_, :], in_=ab_w_out[r0:r0 + 64, :])
            else:
                r0 = 512 + (c_ - 4) * 128
                p.gpsimd.dma_start(out=wo[:, c_, :], in_=ab_w_out[r0:r0 + 128, :])
        tmpp = p.pool("tmpp", [128, D], F32, bufs=2)
        ylp = p.pool("ylp", [128, 8, 512], BF16, bufs=2)
        def sd_s1(t0, ntl):
            N = ntl * 128
            yl = ylp.next()
            p.sync.dma_start(out=yl[:, 0:4, 0:N], in_=oT_d[:, :, t0 * 128:t0 * 128 + N].rearrange("j c t -> c j t"))
            p.sync.dma_start(out=yl[:, 4:8, 0:N], in_=slT_d[:, :, t0 * 128:t0 * 128 + N].rearrange("j c t -> c j t"))
            xts = []
            for ti in range(ntl):
                xt = xp4.next()
                p.sync.dma_start(out=xt[:], in_=tile_ap(x_in, ctx_in, t0 + ti))
                xts.append(xt)
            return (t0, ntl, yl, xts)

        def sd_s2(t0, ntl, yl, xts):
            for ti in range(ntl):
                t = t0 + ti
                w_ = 1 if t < 2 else 0
                xt = xts[ti]
                tm = tmpp.next()
                for half in range(2):
                    pm = ps_all.next()
                    for c_ in range(8):
                        p.tensor.matmul(out=pm[:, :], lhsT=yl[:, c_, ti * 128:(ti + 1) * 128],
                                        rhs=wo[:, c_, half * 512:(half + 1) * 512], start=(c_ == 0), stop=(c_ == 7))
                    p.vector.tensor_tensor(out=tm[:, half * 512:(half + 1) * 512], in0=pm[:, :],
                                           in1=gabc[:, w_, half * 512:(half + 1) * 512], op=ALU.mult)
                p.gpsimd.tensor_tensor(out=tm[:], in0=tm[:], in1=xt[:], op=ALU.add)
                p.sync.dma_start(out=tile_ap(xl1, xc1, t), in_=tm[:])

        xp4 = p.pool("xp4", [128, D], F32, bufs=8)
        pend = None
        for (t0, ntl) in blocks:
            cur = sd_s1(t0, ntl)
            if pend is not None:
                sd_s2(*pend)
            pend = cur
        sd_s2(*pend)

    def moe(l, src, dst, tiles, final=False):
      with p.scope():
        wr = p.sb(f"wr{l}", [128, 8, NE], F32)
        p.sync.dma_start(out=wr[:], in_=router_w.rearrange("(kc p) e -> p kc e", p=128))
        rb = p.sb(f"rb{l}", [128, NE], F32)
        p.sync.dma_start(out=rb[:], in_=router_b.partition_broadcast(128))
        sbs = []
        cur = []
        for t in tiles:
            cur.append(t)
            if len(cur) == 12 or (len(cur) == 10 and cur[0] == 0):
                sbs.append(cur)
                cur = []
        if cur:
            sbs.append(cur)
        h2T = p.sb(f"h2T{l}", [128, 8, 12 * 128], BF16)
        acc = p.sb(f"macc{l}", [128, 12, D], F32)
        comb = p.sb(f"comb{l}", [128, 12, NE], F32)
        hf = p.pool(f"hf{l}", [128, 8, 128], F32, bufs=2)
        wgp = p.pool(f"wgp{l}", [128, 8, FF], BF16, bufs=2)
        wup = p.pool(f"wup{l}", [128, 8, FF], BF16, bufs=2)
        wdp = p.pool(f"wdp{l}", [128, 4, D], BF16, bufs=2)
        aTp = p.pool(f"aTp{l}", [128, 4, 512], BF16, bufs=2)
        sgp = p.pool(f"sgp{l}", [128, 512], BF16, bufs=3)
        rsm = p.pool(f"rsm{l}", [128, 8, 16], F32, bufs=2)
        for sbt in sbs:
            nloc = len(sbt)
            for tl, t in enumerate(sbt):
                w_ = 1 if t < 2 else 0
                h = hf.next()
                norm_tile(tile_ap(src[0], src[1], t), gmod2, 3, w_, lambda kc, h=h: h[:, kc, :])
                p.vector.tensor_copy(out=h2T[:, :, tl * 128:(tl + 1) * 128], in_=h[:])
                pl = ps_all.next()
                for kc in range(8):
                    p.tensor.matmul(out=pl[:, 0:NE], lhsT=h[:, kc, :], rhs=wr[:, kc, :], start=(kc == 0), stop=(kc == 7))
                r = rsm.next()
                sc = r[:, 0, :]
                sel = r[:, 1, :]
                sel4 = fap(r[:, 1, 0:1], [[4, 4], [1, 4]])
                p.scalar.activation(out=sc, in_=pl[:, 0:NE], func=AF.Sigmoid)
                p.vector.tensor_tensor(out=sel, in0=sc, in1=rb[:], op=ALU.add)
                m1 = r[:, 2, 0:4]
                m2 = r[:, 2, 4:8]
                gs = r[:, 2, 8:12]
                gmax = r[:, 2, 12:13]
                p.vector.tensor_reduce(out=m1, in_=sel4, axis=AX.X, op=ALU.max)
                eq = fap(r[:, 3, 0:1], [[4, 4], [1, 4]])
                p.vector.tensor_tensor(out=eq, in0=sel4, in1=fap(r[:, 2, 0:1], [[1, 4], [0, 4]]), op=ALU.is_equal)
                s2 = fap(r[:, 4, 0:1], [[4, 4], [1, 4]])
                p.vector.scalar_tensor_tensor(out=s2, in0=eq, scalar=-1e9, in1=sel4, op0=ALU.mult, op1=ALU.add)
                p.vector.tensor_reduce(out=m2, in_=s2, axis=AX.X, op=ALU.max)
                p.vector.tensor_tensor(out=gs, in0=m1, in1=m2, op=ALU.add)
                p.vector.tensor_reduce(out=gmax, in_=gs, axis=AX.X, op=ALU.max)
                ing = r[:, 5, 0:4]
                p.vector.tensor_scalar(out=ing, in0=gs, scalar1=gmax, scalar2=None, op0=ALU.is_equal)
                ge = fap(r[:, 6, 0:1], [[4, 4], [1, 4]])
                p.vector.tensor_tensor(out=ge, in0=sel4, in1=fap(r[:, 2, 4:5], [[1, 4], [0, 4]]), op=ALU.is_ge)
                p.vector.tensor_tensor(out=ge, in0=ge, in1=fap(r[:, 5, 0:1], [[1, 4], [0, 4]]), op=ALU.mult)
                wts = r[:, 7, :]
                p.vector.tensor_tensor(out=wts, in0=r[:, 6, :], in1=sc, op=ALU.mult)
                den = r[:, 5, 4:5]
                p.vector.tensor_reduce(out=den, in_=wts, axis=AX.X, op=ALU.add)
                rden = r[:, 5, 5:6]
                p.vector.reciprocal(out=rden, in_=den)
                p.vector.tensor_scalar(out=comb[:, tl, :], in0=wts, scalar1=rden, scalar2=None, op0=ALU.mult)
            lblocks = []
            i = 0
            while i < nloc:
                n = 2 if sbt[i] == 0 else min(4, nloc - i)
                lblocks.append((i, n))
                i += n
            for e in range(NE):
                wg = wgp.next()
                wu = wup.next()
                wd = wdp.next()
                p.gpsimd.dma_start(out=wg[:], in_=w_gate[l, e].rearrange("(kc p) f -> p kc f", p=128))
                p.gpsimd.dma_start(out=wu[:], in_=w_up[l, e].rearrange("(kc p) f -> p kc f", p=128))
                p.gpsimd.dma_start(out=wd[:], in_=w_down[l, e].rearrange("(f p) n -> p f n", p=128))
                for (i0, n) in lblocks:
                    N = n * 128
                    c0 = i0 * 128
                    aT = aTp.next()
                    for f in range(4):
                        pg = ps_all.next()
                        pu = ps_all.next()
                        for kc in range(8):
                            p.tensor.matmul(out=pg[:, 0:N], lhsT=wg[:, kc, f * 128:(f + 1) * 128],
                                            rhs=h2T[:, kc, c0:c0 + N], start=(kc == 0), stop=(kc == 7))
                        for kc in range(8):
                            p.tensor.matmul(out=pu[:, 0:N], lhsT=wu[:, kc, f * 128:(f + 1) * 128],
                                            rhs=h2T[:, kc, c0:c0 + N], start=(kc == 0), stop=(kc == 7))
                        sg = sgp.next()
                        p.scalar.activation(out=sg[:, 0:N], in_=pg[:, 0:N], func=AF.Silu)
                        p.vector.tensor_tensor(out=aT[:, f, 0:N], in0=sg[:, 0:N], in1=pu[:, 0:N], op=ALU.mult)
                    for ti in range(n):
                        tl = i0 + ti
                        for half in range(2):
                            pd = ps_all.next()
                            for f in range(4):
                                p.tensor.matmul(out=pd[:, :], lhsT=aT[:, f, ti * 128:(ti + 1) * 128],
                                                rhs=wd[:, f, half * 512:(half + 1) * 512], start=(f == 0), stop=(f == 3))
                            a_ = acc[:, tl, half * 512:(half + 1) * 512]
                            if e == 0:
                                p.vector.tensor_scalar(out=a_, in0=pd[:, :], scalar1=comb[:, tl, e:e + 1], scalar2=None,
                                                       op0=ALU.mult)
                            else:
                                p.vector.scalar_tensor_tensor(out=a_, in0=pd[:, :], scalar=comb[:, tl, e:e + 1], in1=a_,
                                                              op0=ALU.mult, op1=ALU.add)
            for tl, t in enumerate(sbt):
                w_ = 1 if t < 2 else 0
                xt = xpool.next()
                p.sync.dma_start(out=xt[:], in_=tile_ap(src[0], src[1], t))
                tm = xspool.next()
                p.vector.tensor_tensor(out=tm[:], in0=acc[:, tl, :], in1=gabc[:, 2 + w_, :], op=ALU.mult)
                p.vector.tensor_tensor(out=tm[:], in0=tm[:], in1=xt[:], op=ALU.add)
                if final:
                    jk = junk.next()
                    sm = small.next()
                    p.scalar.activation(out=jk[:], in_=tm[:], func=AF.Square, accum_out=sm[:, 0:1])
                    rstd_from_ss(p, sm[:, 2:3], sm[:, 0:1], D, sm[:, 1:2])
                    p.vector.scalar_tensor_tensor(out=tm[:], in0=tm[:], scalar=sm[:, 2:3], in1=fng_bc[:],
                                                  op0=ALU.mult, op1=ALU.mult)
                p.sync.dma_start(out=tile_ap(dst[0], dst[1], t), in_=tm[:])


    I32 = mybir.dt.int32
    su_in = IN("su_mat", [128, 128])
    thr_in = IN("thr512", [1, 40])
    base8_in = IN("base8", [128, 8])
    base4_in = IN("base4", [128, 4])
    zeros_in = IN("zeros_bf", [512, D], BF16)

    wgb = p.dram("wgb", [2 * NE * D, FF], BF16)
    wub = p.dram("wub", [2 * NE * D, FF], BF16)
    wdb = p.dram("wdb", [2 * NE * FF, D], BF16)

    def precast_weights():
        wg_f = w_gate.rearrange("l e k f -> (l e k) f")
        wu_f = w_up.rearrange("l e k f -> (l e k) f")
        wd_f = w_down.rearrange("l e k f -> (l e k) f")
        for l in range(2):
            for e in range(NE):
                r0 = (l * NE + e) * D
                p.gpsimd.dma_start(out=wgb[r0:r0 + D, :], in_=wg_f[r0:r0 + D, :])
                yield
                p.gpsimd.dma_start(out=wub[r0:r0 + D, :], in_=wu_f[r0:r0 + D, :])
                yield
                r1 = (l * NE + e) * FF
                p.gpsimd.dma_start(out=wdb[r1:r1 + FF, :], in_=wd_f[r1:r1 + FF, :])
                yield
    k.precast_weights = precast_weights
    k.precast_iter = None

    def precast_step(n):
        if k.precast_iter is None:
            return
        for _ in range(n):
            try:
                next(k.precast_iter)
            except StopIteration:
                k.precast_iter = None
                return
    k.precast_step = precast_step

    def moe_sparse(l, src, dst, tiles, final=False):
      NTL = len(tiles)
      ntok = NTL * 128
      NST = (2 * ntok) // 512 + 16
      NSLOT = NST * 512
      Xs = p.dram(f"Xs{l}", [NSLOT, D], BF16)
      Ys = p.dram(f"Ys{l}", [NSLOT, D], BF16)
      H2 = p.dram(f"H2{l}", [ntok, D], BF16)
      wd_tab = wdb
      IOA = bass.IndirectOffsetOnAxis
      with p.scope():
        comb_all = p.sb("comb_all", [128, NTL, NE], F32)
        mask_all = p.sb("mask_all", [128, NTL, NE], F32)
        sc_all = p.sb("sc_all", [128, NTL, NE], F32)
        sl_f = p.sb("sl_f", [128, NTL, 2], F32)
        sl_i = p.sb("sl_i", [128, NTL, 2], I32)
        wl = p.sb("wl", [128, NTL, 2], F32)
        widx_i = p.sb("widx_i", [128, NST, 8], I32)
        didx_i = p.sb("didx_i", [128, NST, 4], I32)
        for i in range(NST):
            p.sync.dma_start(out=Xs[i * 512:(i + 1) * 512, :], in_=zeros_in)
        with p.scope():
            wr = p.sb("wr", [128, 8, NE], F32)
            p.sync.dma_start(out=wr[:], in_=router_w.rearrange("(kc p) e -> p kc e", p=128))
            rb = p.sb("rb", [128, NE], F32)
            p.sync.dma_start(out=rb[:], in_=router_b.partition_broadcast(128))
            su = p.sb("su", [128, 128], F32)
            p.sync.dma_start(out=su[:], in_=su_in)
            thr = p.sb("thr", [128, 40], F32)
            p.sync.dma_start(out=thr[:], in_=thr_in.partition_broadcast(128))
            b8 = p.sb("b8", [128, 8], F32)
            b4 = p.sb("b4", [128, 4], F32)
            p.sync.dma_start(out=b8[:], in_=base8_in)
            p.sync.dma_start(out=b4[:], in_=base4_in)
            mbc = p.sb("mbc", [128, 4, D], F32)
            dg = p.pool("dgm", [128, 128], F32, bufs=2)
            for vi in range(2):
                for w_ in range(2):
                    for half in range(2):
                        pb = ps_all.next()
                        for q in range(4):
                            kc = half * 4 + q
                            d_ = dg.next()
                            col = gmod2[:, kc, w_:w_ + 1] if vi == 0 else modcol[:, 24 + kc, w_:w_ + 1]
                            p.vector.tensor_scalar(out=d_[:], in0=idt[:], scalar1=col, scalar2=None, op0=ALU.mult)
                            p.tensor.matmul(out=pb[:, q * 128:(q + 1) * 128], lhsT=ones_f[:], rhs=d_[:], start=True, stop=True)
                        p.scalar.copy(out=mbc[:, vi * 2 + w_, half * 512:(half + 1) * 512], in_=pb[:, :])
            hf = p.pool("hf", [128, 8, 128], F32, bufs=2)
            rsm = p.pool("rsm", [128, 8, 16], F32, bufs=2)
            h2p = p.pool("h2p", [128, D], BF16, bufs=2)
            h2f = p.pool("h2f", [128, D], F32, bufs=1)
            xs3 = p.pool("xs3", [128, D], F32, bufs=3)

            def rt_s1(tl, t):
                w_ = 1 if t < 2 else 0
                xt = xpool.next()
                p.sync.dma_start(out=xt[:], in_=tile_ap(src[0], src[1], t))
                jk = junk.next()
                sm = small.next()
                p.scalar.activation(out=jk[:], in_=xt[:], func=AF.Square, accum_out=sm[:, 0:1])
                rstd_from_ss(p, sm[:, 2:3], sm[:, 0:1], D, sm[:, 1:2])
                xs = xs3.next()
                p.vector.tensor_scalar(out=xs[:], in0=xt[:], scalar1=sm[:, 2:3], scalar2=None, op0=ALU.mult)
                hf32 = h2f.next()
                p.vector.tensor_tensor(out=hf32[:], in0=xs[:], in1=mbc[:, w_, :], op=ALU.mult)
                h2t = h2p.next()
                p.gpsimd.tensor_tensor(out=h2t[:], in0=hf32[:], in1=mbc[:, 2 + w_, :], op=ALU.add)
                p.sync.dma_start(out=H2[tl * 128:(tl + 1) * 128, :], in_=h2t[:])
                return (tl, xs, w_)

            def rt_s2(tl, xs, w_):
                h = hf.next()
                for half in range(2):
                    pt = ps_all.next()
                    for q in range(4):
                        kc = half * 4 + q
                        p.tensor.transpose(out=pt[:, q * 128:(q + 1) * 128], in_=xs[:, kc * 128:(kc + 1) * 128], identity=idt[:])
                    for q in range(4):
                        kc = half * 4 + q
                        p.scalar.activation(out=h[:, kc, :], in_=pt[:, q * 128:(q + 1) * 128], func=AF.Identity,
                                            bias=modcol[:, 24 + kc, w_:w_ + 1], scale=gmod2[:, kc, w_:w_ + 1])
                pl = ps_all.next()
                for kc in range(8):
                    p.tensor.matmul(out=pl[:, 0:NE], lhsT=h[:, kc, :], rhs=wr[:, kc, :], start=(kc == 0), stop=(kc == 7))
                p.scalar.activation(out=sc_all[:, tl, :], in_=pl[:, 0:NE], func=AF.Sigmoid)

            pend = None
            for tl, t in enumerate(tiles):
                cur = rt_s1(tl, t)
                if pend is not None:
                    rt_s2(*pend)
                pend = cur
            rt_s2(*pend)
            G4 = NTL * 4
            T3 = lambda nm: p.sb(nm, [128, NTL, NE], F32)
            g4v = lambda t_: t_[:].rearrange("p t (g j) -> p (t g) j", j=4)
            sel = T3("sel")
            p.vector.tensor_tensor(out=sel[:], in0=sc_all[:], in1=fap(rb[:, 0:1], [[0, NTL], [1, NE]]), op=ALU.add)
            m1 = p.sb("m1", [128, G4], F32)
            m2 = p.sb("m2", [128, G4], F32)
            gs = p.sb("gs", [128, G4], F32)
            gmax = p.sb("gmax", [128, NTL], F32)
            p.vector.tensor_reduce(out=m1[:], in_=g4v(sel), axis=AX.X, op=ALU.max)
            eq = T3("eq")
            p.vector.tensor_tensor(out=g4v(eq), in0=g4v(sel), in1=fap(m1[:, 0:1], [[1, G4], [0, 4]]), op=ALU.is_equal)
            p.vector.scalar_tensor_tensor(out=eq[:], in0=eq[:], scalar=-1e9, in1=sel[:], op0=ALU.mult, op1=ALU.add)
            p.vector.tensor_reduce(out=m2[:], in_=g4v(eq), axis=AX.X, op=ALU.max)
            p.vector.tensor_tensor(out=gs[:], in0=m1[:], in1=m2[:], op=ALU.add)
            p.vector.tensor_reduce(out=gmax[:], in_=gs[:].rearrange("p (t g) -> p t g", g=4), axis=AX.X, op=ALU.max)
            ing = p.sb("ing", [128, G4], F32)
            p.vector.tensor_tensor(out=ing[:].rearrange("p (t g) -> p t g", g=4), in0=gs[:].rearrange("p (t g) -> p t g", g=4),
                                   in1=fap(gmax[:, 0:1], [[1, NTL], [0, 4]]), op=ALU.is_equal)
            p.vector.tensor_tensor(out=g4v(eq), in0=g4v(sel), in1=fap(m2[:, 0:1], [[1, G4], [0, 4]]), op=ALU.is_ge)
            p.vector.tensor_tensor(out=g4v(mask_all), in0=g4v(eq), in1=fap(ing[:, 0:1], [[1, G4], [0, 4]]), op=ALU.mult)
            wts = T3("wts")
            p.vector.tensor_tensor(out=wts[:], in0=mask_all[:], in1=sc_all[:], op=ALU.mult)
            den = p.sb("den", [128, NTL], F32)
            p.vector.tensor_reduce(out=den[:], in_=wts[:], axis=AX.X, op=ALU.add)
            p.vector.reciprocal(out=den[:], in_=den[:])
            p.vector.tensor_tensor(out=comb_all[:], in0=wts[:], in1=fap(den[:, 0:1], [[1, NTL], [0, NE]]), op=ALU.mult)
            ctile = T3("ctile")
            pos = T3("pos")
            mflat = mask_all[:].rearrange("p t e -> p (t e)")
            n0 = min(NTL, 32) * NE
            pcA = ps_all.next()
            p.tensor.matmul(out=pcA[:, 0:n0], lhsT=ones_f[:], rhs=mflat[:, 0:n0], start=True, stop=True)
            p.vector.tensor_copy(out=ctile[:].rearrange("p t e -> p (t e)")[:, 0:n0], in_=pcA[:, 0:n0])
            if NTL > 32:
                pcB = ps_all.next()
                p.tensor.matmul(out=pcB[:, 0:NTL * NE - n0], lhsT=ones_f[:], rhs=mflat[:, n0:NTL * NE], start=True, stop=True)
                p.vector.tensor_copy(out=ctile[:].rearrange("p t e -> p (t e)")[:, n0:NTL * NE], in_=pcB[:, 0:NTL * NE - n0])
            for t0_ in range(0, NTL, 32):
                t1_ = min(NTL, t0_ + 32)
                pp = ps_all.next()
                for tl in range(t0_, t1_):
                    p.tensor.matmul(out=pp[:, (tl - t0_) * NE:(tl - t0_ + 1) * NE], lhsT=su[:], rhs=mask_all[:, tl, :], start=True, stop=True)
                p.vector.tensor_copy(out=pos[:, t0_:t1_, :].rearrange("p t e -> p (t e)"), in_=pp[:, 0:(t1_ - t0_) * NE])
            cst = p.sb("cst", [128, 8, NE], F32)
            cnt = cst[:, 0, :]
            nt_ = cst[:, 1, :]
            p.vector.tensor_reduce(out=cnt, in_=ctile[:].rearrange("p t e -> p e t"), axis=AX.X, op=ALU.add)
            p.vector.tensor_scalar(out=nt_, in0=cnt, scalar1=0.0, scalar2=None, op0=ALU.is_gt)
            for kk in range(1, NST):
                p.vector.scalar_tensor_tensor(out=nt_, in0=cnt, scalar=512.0 * kk, in1=nt_, op0=ALU.is_gt, op1=ALU.add)
            pcn = cst[:, 2, :]
            p.vector.tensor_scalar(out=pcn, in0=nt_, scalar1=512.0, scalar2=None, op0=ALU.mult)
            a_, b_ = 3, 4
            p.vector.tensor_copy(out=cst[:, a_, :], in_=pcn)
            for sh in (1, 2, 4, 8):
                p.vector.tensor_copy(out=cst[:, b_, :], in_=cst[:, a_, :])
                p.vector.tensor_tensor(out=cst[:, b_, sh:NE], in0=cst[:, a_, sh:NE], in1=cst[:, a_, 0:NE - sh], op=ALU.add)
                a_, b_ = b_, a_
            seg_end = cst[:, a_, :]
            seg_start = cst[:, 5, :]
            p.vector.tensor_tensor(out=seg_start, in0=seg_end, in1=pcn, op=ALU.subtract)
            pa_, pb_ = T3("pfa"), T3("pfb")
            p.vector.tensor_copy(out=pa_[:], in_=ctile[:])
            sh = 1
            while sh < NTL:
                p.vector.tensor_copy(out=pb_[:], in_=pa_[:])
                p.vector.tensor_tensor(out=pb_[:, sh:NTL, :], in0=pa_[:, sh:NTL, :], in1=pa_[:, 0:NTL - sh, :], op=ALU.add)
                pa_, pb_ = pb_, pa_
                sh *= 2
            slot = T3("slot")
            p.vector.tensor_tensor(out=slot[:], in0=pa_[:], in1=ctile[:], op=ALU.subtract)
            p.vector.tensor_tensor(out=slot[:], in0=slot[:], in1=pos[:], op=ALU.add)
            p.vector.tensor_tensor(out=slot[:], in0=slot[:], in1=fap(seg_start[:, 0:1], [[0, NTL], [1, NE]]), op=ALU.add)
            mm1 = T3("mm1")
            p.vector.scalar_tensor_tensor(out=mm1[:], in0=slot[:], scalar=1.0, in1=mask_all[:], op0=ALU.add, op1=ALU.mult)
            shi1 = p.sb("shi1", [128, NTL], F32)
            p.vector.tensor_reduce(out=shi1[:], in_=mm1[:], axis=AX.X, op=ALU.max)
            mm2 = T3("mm2")
            p.vector.scalar_tensor_tensor(out=mm2[:], in0=mask_all[:], scalar=-1.0e6, in1=slot[:], op0=ALU.mult, op1=ALU.add)
            p.vector.tensor_scalar(out=mm2[:], in0=mm2[:], scalar1=1.0e6, scalar2=None, op0=ALU.add)
            slo = p.sb("slo", [128, NTL], F32)
            p.vector.tensor_reduce(out=slo[:], in_=mm2[:], axis=AX.X, op=ALU.min)
            p.vector.tensor_copy(out=sl_f[:, :, 0], in_=slo[:])
            p.vector.tensor_scalar(out=sl_f[:, :, 1], in0=shi1[:], scalar1=-1.0, scalar2=None, op0=ALU.add)
            p.vector.tensor_tensor(out=mm1[:], in0=mm1[:], in1=fap(shi1[:, 0:1], [[1, NTL], [0, NE]]), op=ALU.is_equal)
            p.vector.tensor_tensor(out=mm1[:], in0=mm1[:], in1=comb_all[:], op=ALU.mult)
            p.vector.tensor_reduce(out=wl[:, :, 1], in_=mm1[:], axis=AX.X, op=ALU.add)
            p.vector.tensor_tensor(out=mm2[:], in0=mm2[:], in1=fap(slo[:, 0:1], [[1, NTL], [0, NE]]), op=ALU.is_equal)
            p.vector.tensor_tensor(out=mm2[:], in0=mm2[:], in1=comb_all[:], op=ALU.mult)
            p.vector.tensor_reduce(out=wl[:, :, 0], in_=mm2[:], axis=AX.X, op=ALU.add)
            p.vector.tensor_copy(out=sl_i[:], in_=sl_f[:])
            cmp_ = p.sb("cmp_", [128, NST, NE], F32)
            p.vector.tensor_tensor(out=cmp_[:], in0=fap(seg_end[:, 0:1], [[0, NST], [1, NE]]),
                                   in1=fap(thr[:, 0:1], [[1, NST], [0, NE]]), op=ALU.is_le)
            eall = p.sb("eall", [128, NST], F32)
            p.vector.tensor_reduce(out=eall[:], in_=cmp_[:], axis=AX.X, op=ALU.add)
            p.vector.tensor_scalar(out=eall[:], in0=eall[:], scalar1=float(NE - 1), scalar2=None, op0=ALU.min)
            wif = p.sb("wif", [128, NST, 8], F32)
            p.vector.scalar_tensor_tensor(out=wif[:], in0=fap(eall[:, 0:1], [[1, NST], [0, 8]]), scalar=1024.0,
                                          in1=fap(b8[:, 0:1], [[0, NST], [1, 8]]), op0=ALU.mult, op1=ALU.add)
            if l > 0:
                p.vector.tensor_scalar(out=wif[:], in0=wif[:], scalar1=float(l * NE * D), scalar2=None, op0=ALU.add)
            p.vector.tensor_copy(out=widx_i[:], in_=wif[:])
            p.vector.scalar_tensor_tensor(out=wif[:, :, 0:4], in0=fap(eall[:, 0:1], [[1, NST], [0, 4]]), scalar=512.0,
                                          in1=fap(b4[:, 0:1], [[0, NST], [1, 4]]), op0=ALU.mult, op1=ALU.add)
            if l > 0:
                p.vector.tensor_scalar(out=wif[:, :, 0:4], in0=wif[:, :, 0:4], scalar1=float(l * NE * FF), scalar2=None, op0=ALU.add)
            p.vector.tensor_copy(out=didx_i[:], in_=wif[:, :, 0:4])
            for tl in range(NTL):
                h2t = h2p.next()
                p.sync.dma_start(out=h2t[:], in_=H2[tl * 128:(tl + 1) * 128, :])
                for j in range(2):
                    p.gpsimd.indirect_dma_start(out=Xs[:, :], out_offset=IOA(ap=sl_i[:, tl, j:j + 1], axis=0), in_=h2t[:, :],
                                                in_offset=None)
        if os.environ.get("MSTOP", "") == "R":
            k.dbg("d_sl", sl_f[:], [128, NTL, 2])
            k.dbg("d_wl", wl[:], [128, NTL, 2])
            k.dbg("d_comb", comb_all[:], [128, NTL, NE])
            return
        with p.scope():
            wgup = p.pool("wgus", [128, 8, 2 * FF], BF16, bufs=2)
            wdp = p.pool("wds", [128, 4, D], BF16, bufs=2)
            xsp = p.pool("xsp", [128, 4, D], BF16, bufs=2)
            xTp = p.pool("xTp", [128, 8, 512], BF16, bufs=2)
            aTp = p.pool("aTs", [128, 4, 512], BF16, bufs=2)
            sgp = p.pool("sgs", [128, 512], BF16, bufs=2)
            ysp = p.pool("ysp", [128, 4, D], BF16, bufs=2)
            def ex_load(i):
                wgu = wgup.next()
                wd = wdp.next()
                for kc in range(8):
                    p.gpsimd.indirect_dma_start(out=wgu[:, kc, 0:FF], out_offset=None, in_=wgb[:, :],
                                                in_offset=IOA(ap=widx_i[:, i, kc:kc + 1], axis=0))
                    p.gpsimd.indirect_dma_start(out=wgu[:, kc, FF:2 * FF], out_offset=None, in_=wub[:, :],
                                                in_offset=IOA(ap=widx_i[:, i, kc:kc + 1], axis=0))
                for f in range(4):
                    p.gpsimd.indirect_dma_start(out=wd[:, f, :], out_offset=None, in_=wd_tab[:, :],
                                                in_offset=IOA(ap=didx_i[:, i, f:f + 1], axis=0))
                xs_ = xsp.next()
                p.sync.dma_start(out=xs_[:], in_=Xs[i * 512:(i + 1) * 512, :].rearrange("(q p) d -> p q d", p=128))
                xT = xTp.next()
                for q in range(4):
                    for half in range(2):
                        pt = ps_all.next(BF16)
                        for j in range(4):
                            kc = half * 4 + j
                            p.tensor.transpose(out=pt[:, j * 128:(j + 1) * 128], in_=xs_[:, q, kc * 128:(kc + 1) * 128], identity=idb[:])
                        if (q + half) % 2 == 0:
                            p.scalar.copy(out=xT[:, half * 4:(half + 1) * 4, q * 128:(q + 1) * 128],
                                          in_=pt[:, 0:512].rearrange("p (j c) -> p j c", j=4))
                        else:
                            p.vector.tensor_copy(out=xT[:, half * 4:(half + 1) * 4, q * 128:(q + 1) * 128],
                                                 in_=pt[:, 0:512].rearrange("p (j c) -> p j c", j=4))
                return (i, wgu, wd, xT)

            def ex_compute(i, wgu, wd, xT):
                aT = aTp.next()
                for f in range(4):
                    pg = ps_all.next()
                    pu = ps_all.next()
                    for kc in range(8):
                        p.tensor.matmul(out=pg[:, :], lhsT=wgu[:, kc, f * 128:(f + 1) * 128], rhs=xT[:, kc, :], start=(kc == 0), stop=(kc == 7))
                    for kc in range(8):
                        p.tensor.matmul(out=pu[:, :], lhsT=wgu[:, kc, FF + f * 128:FF + (f + 1) * 128], rhs=xT[:, kc, :], start=(kc == 0), stop=(kc == 7))
                    sg = sgp.next()
                    p.scalar.activation(out=sg[:], in_=pg[:, :], func=AF.Silu)
                    p.vector.tensor_tensor(out=aT[:, f, :], in0=sg[:], in1=pu[:, :], op=ALU.mult)
                ys = ysp.next()
                for q in range(4):
                    for half in range(2):
                        pd = ps_all.next()
                        for f in range(4):
                            p.tensor.matmul(out=pd[:, :], lhsT=aT[:, f, q * 128:(q + 1) * 128], rhs=wd[:, f, half * 512:(half + 1) * 512],
                                            start=(f == 0), stop=(f == 3))
                        if (q + half) % 2 == 0:
                            p.scalar.copy(out=ys[:, q, half * 512:(half + 1) * 512], in_=pd[:, :])
                        else:
                            p.vector.tensor_copy(out=ys[:, q, half * 512:(half + 1) * 512], in_=pd[:, :])
                p.sync.dma_start(out=Ys[i * 512:(i + 1) * 512, :].rearrange("(q p) d -> p q d", p=128), in_=ys[:])

            pend = None
            for i in range(NST):
                cur = ex_load(i)
                if pend is not None:
                    ex_compute(*pend)
                pend = cur
            ex_compute(*pend)
        with p.scope():
            ygp = p.pool("ygp", [128, 2, D], BF16, bufs=3)
            fp_ = p.pool("fcomb", [128, D], F32, bufs=2)

            def cb_s1(tl, t):
                yg = ygp.next()
                for j in range(2):
                    p.gpsimd.indirect_dma_start(out=yg[:, j, :], out_offset=None, in_=Ys[:, :],
                                                in_offset=IOA(ap=sl_i[:, tl, j:j + 1], axis=0))
                xt = xpool.next()
                p.sync.dma_start(out=xt[:], in_=tile_ap(src[0], src[1], t))
                return (tl, t, yg, xt)

            def cb_s2(tl, t, yg, xt):
                w_ = 1 if t < 2 else 0
                f_ = fp_.next()
                p.vector.tensor_scalar(out=f_[:], in0=yg[:, 0, :], scalar1=wl[:, tl, 0:1], scalar2=None, op0=ALU.mult)
                p.vector.scalar_tensor_tensor(out=f_[:], in0=yg[:, 1, :], scalar=wl[:, tl, 1:2], in1=f_[:], op0=ALU.mult, op1=ALU.add)
                tm = xspool.next()
                p.vector.tensor_tensor(out=tm[:], in0=f_[:], in1=gabc[:, 2 + w_, :], op=ALU.mult)
                p.vector.tensor_tensor(out=tm[:], in0=tm[:], in1=xt[:], op=ALU.add)
                if final:
                    jk = junk.next()
                    sm = small.next()
                    p.scalar.activation(out=jk[:], in_=tm[:], func=AF.Square, accum_out=sm[:, 0:1])
                    rstd_from_ss(p, sm[:, 2:3], sm[:, 0:1], D, sm[:, 1:2])
                    p.vector.scalar_tensor_tensor(out=tm[:], in0=tm[:], scalar=sm[:, 2:3], in1=fng_bc[:],
                                                  op0=ALU.mult, op1=ALU.mult)
                p.sync.dma_start(out=tile_ap(dst[0], dst[1], t), in_=tm[:])

            pend = None
            for tl, t in enumerate(tiles):
                cur = cb_s1(tl, t)
                if pend is not None:
                    cb_s2(*pend)
                pend = cur
            cb_s2(*pend)

    k.moe_sparse = moe_sparse

    NCH = 68
    cd_w_in = IN("cd_w_in", [D, 3088])
    cv_dw_w = IN("cv_dw_w", [31, 512])
    cv_vec = IN("cv_vec", [12, 128])
    dn_conv_w = IN("dn_conv_w", [5, 1536])
    dn_alog = IN("dn_a_log", [1, 8])
    dn_dtb = IN("dn_dt_bias", [1, 8])
    dn_onorm = IN("dn_o_norm", [1, 128])
    cd_w_out = IN("cd_w_out", [D, D])
    dn_consts = IN("dn_consts", [64, 5, 128])
    qT_d = p.dram("qT_d", [4, 128, NT * 128], BF16, kind=dk)
    kT_d = p.dram("kT_d", [4, 128, NT * 128], BF16, kind=dk)
    vT_d = p.dram("vT_d", [4, 128, NT * 128], BF16, kind=dk)
    convT_d = p.dram("convT_d", [4, 128, S], BF16, kind=dk)
    cv_d = p.dram("cv_d", [4, 128, S], F32, kind=dk)
    sog_d = p.dram("sog_d", [S, 512], BF16, kind=dk)
    o_d = p.dram("o_d", [2, S, 512], F32, kind=dk)
    L1STOP = os.environ.get("L1STOP", "")

    def layer1_mixer(src_lat, src_ctx, dst_lat):
      with p.scope():
        abr = p.sb("abr", [64, 2, 2, NCH, 4], F32)
        gall = p.sb("gall", [64, 2, NCH, 4], F32)
        beta = p.sb("beta", [64, 2, NCH, 4], F32)
        egc = p.sb("egc", [64, 2, NCH, 4], F32)
        egd = p.sb("egd", [64, 2, NCH, 4], F32)
        be = p.sb("be", [64, 2, NCH, 4], F32)
        eglb = p.sb("eglb", [128, 2, NCH, 4], F32)
        dnc = p.sb("dnc", [64, 5, 128], F32)
        p.sync.dma_start(out=dnc[:], in_=dn_consts)
        id64 = idt[0:64, 0:64]
        CO, LO = 2, 2 + 256 + 2 + 2
        segs = [(CO, 0, 256), (LO, 256, 4096)]
        with p.scope():
            hT = p.sb("hT1", [128, 8, NT * 128], BF16)
            for t in range(NT):
                w_ = 1 if t < 2 else 0
                norm_tile(tile_ap(src_lat, src_ctx, t), gmod1, 0, w_,
                          lambda kc, t=t: hT[:, kc, t * 128:(t + 1) * 128])
            cw5 = p.sb("cw5", [128, 12, 5], F32)
            cw31 = p.sb("cw31", [128, 4, 31], F32)
            cvc = p.sb("cvc", [128, 12], F32)
            ones_b = p.sb("ones_b", [128, 128], BF16)
            p.vector.memset(ap=ones_b[:], constant=1.0)
            with p.scope():
                cw5s = p.sb("cw5s", [5, 1536], F32)
                p.sync.dma_start(out=cw5s[:], in_=dn_conv_w)
                for half in range(2):
                    pt = ps_all.next()
                    for q in range(6):
                        cc = half * 6 + q
                        p.tensor.transpose(out=pt[:, q * 5:(q + 1) * 5], in_=cw5s[0:5, cc * 128:(cc + 1) * 128],
                                           identity=idt[0:5, 0:5])
                    p.vector.tensor_copy(out=cw5[:, half * 6:(half + 1) * 6, :],
                                         in_=pt[:, 0:30].rearrange("p (c k) -> p c k", k=5))
                cw31s = p.sb("cw31s", [31, 512], F32)
                p.sync.dma_start(out=cw31s[:], in_=cv_dw_w)
                pt = ps_all.next()
                for cc in range(4):
                    p.tensor.transpose(out=pt[:, cc * 31:(cc + 1) * 31], in_=cw31s[0:31, cc * 128:(cc + 1) * 128],
                                       identity=idt[0:31, 0:31])
                p.vector.tensor_copy(out=cw31[:], in_=pt[:, 0:124].rearrange("p (c k) -> p c k", k=31))
                cvs = p.sb("cvs", [12, 128], F32)
                p.sync.dma_start(out=cvs[:], in_=cv_vec)
                pt = ps_all.next()
                p.tensor.transpose(out=pt[:, 0:12], in_=cvs[0:12, :], identity=idt[0:12, 0:12])
                p.vector.tensor_copy(out=cvc[:], in_=pt[:, 0:12])

            if L1STOP == "F0":
                return
            with p.scope():
                wab = p.sb("wab", [128, 8, 16], BF16)
                p.gpsimd.dma_start(out=wab[:], in_=cd_w_in[:, 2560:2576].rearrange("(kc p) n -> p kc n", p=128))
                for c in range(NCH):
                    pa = ps_all.next()
                    for kc in range(8):
                        p.tensor.matmul(out=pa[0:64, 0:16], lhsT=hT[:, kc, c * 64:(c + 1) * 64], rhs=wab[:, kc, :],
                                        start=(kc == 0), stop=(kc == 7))
                    p.scalar.copy(out=abr[:, :, :, c, :], in_=pa[0:64, 0:16].rearrange("p (k d h) -> p k d h", k=2, d=2))
                if L1STOP == "F1a":
                    return
                dtb = p.sb("dtb", [64, 8], F32)
                nega = p.sb("nega", [64, 8], F32)
                p.sync.dma_start(out=dtb[:], in_=dn_dtb.partition_broadcast(64))
                p.sync.dma_start(out=nega[:], in_=dn_alog.partition_broadcast(64))
                p.scalar.activation(out=nega[:], in_=nega[:], func=AF.Exp)
                p.vector.tensor_scalar(out=nega[:], in0=nega[:], scalar1=-1.0, scalar2=None, op0=ALU.mult)
                bc8 = lambda t_: fap(t_[:, 0:1], [[4, 2], [0, NCH], [1, 4]])
                p.vector.tensor_tensor(out=gall[:], in0=abr[:, 0], in1=bc8(dtb), op=ALU.add)
                p.scalar.activation(out=gall[:], in_=gall[:], func=AF.Exp)
                p.vector.tensor_scalar(out=gall[:], in0=gall[:], scalar1=1.0, scalar2=None, op0=ALU.add)
                p.scalar.activation(out=gall[:], in_=gall[:], func=AF.Ln)
                p.vector.tensor_tensor(out=gall[:], in0=gall[:], in1=bc8(nega), op=ALU.mult)
                p.scalar.activation(out=beta[:], in_=abr[:, 1], func=AF.Sigmoid)
                if L1STOP == "F1b":
                    return
                for d_ in range(2):
                    pg = ps_all.next()
                    p.tensor.matmul(out=pg[0:64, 0:NCH * 4], lhsT=dnc[:, d_, 64:128],
                                    rhs=gall[:, d_].rearrange("p c h -> p (c h)"), start=True, stop=True)
                    pgl = ps_all.next()
                    p.tensor.matmul(out=pgl[:, 0:NCH * 4], lhsT=ones_f[0:64, :],
                                    rhs=gall[:, d_].rearrange("p c h -> p (c h)"), start=True, stop=True)
                    fl = lambda t_: t_[:, d_].rearrange("p c h -> p (c h)")
                    p.scalar.activation(out=fl(egc), in_=pg[0:64, 0:NCH * 4], func=AF.Exp)
                    p.scalar.activation(out=eglb[:, d_].rearrange("p c h -> p (c h)"), in_=pgl[:, 0:NCH * 4], func=AF.Exp)
                    p.vector.tensor_copy(out=fl(egd), in_=pg[0:64, 0:NCH * 4])
                    p.vector.tensor_tensor(out=fl(egd), in0=pgl[0:64, 0:NCH * 4], in1=fl(egd), op=ALU.subtract)
                    p.scalar.activation(out=fl(egd), in_=fl(egd), func=AF.Exp)
                p.vector.tensor_tensor(out=be[:], in0=beta[:], in1=egc[:], op=ALU.mult)
                if L1STOP == "F1c":
                    return
                wog = p.sb("wog", [128, 8, 512], BF16)
                p.gpsimd.dma_start(out=wog[:], in_=cd_w_in[:, 2576:3088].rearrange("(kc p) n -> p kc n", p=128))
                sogp = p.pool("sogp", [128, 512], BF16, bufs=2)
                for t in range(2, NT):
                    po = ps_all.next()
                    for kc in range(8):
                        p.tensor.matmul(out=po[:, :], lhsT=hT[:, kc, t * 128:(t + 1) * 128], rhs=wog[:, kc, :],
                                        start=(kc == 0), stop=(kc == 7))
                    so = sogp.next()
                    p.scalar.activation(out=so[:], in_=po[:, :], func=AF.Silu)
                    p.sync.dma_start(out=sog_d[(t - 2) * 128:(t - 1) * 128, :], in_=so[:])

            if L1STOP == "F1":
                return

            def project_cc(col0, xc, wcc):
                w = wcc.next()
                p.gpsimd.dma_start(out=w[:], in_=cd_w_in[:, col0:col0 + 128].rearrange("(kc p) n -> p kc n", p=128))
                for (t0, ntl) in blocks:
                    N = ntl * 128
                    pp = ps_all.next()
                    for kc in range(8):
                        p.tensor.matmul(out=pp[:, 0:N], lhsT=w[:, kc, :], rhs=hT[:, kc, t0 * 128:t0 * 128 + N],
                                        start=(kc == 0), stop=(kc == 7))
                    off = CO + t0 * 128 if t0 < 2 else LO + (t0 - 2) * 128
                    p.scalar.copy(out=xc[:, off:off + N], in_=pp[:, 0:N])

            def alloc_xc(dt_=F32):
                xc = p.sb("xc", [128, NT * 128 + 8], dt_)
                p.vector.memset(ap=xc[:, 0:2], constant=0.0)
                p.vector.memset(ap=xc[:, 258:262], constant=0.0)
                p.vector.memset(ap=xc[:, LO + 4096:LO + 4098], constant=0.0)
                return xc

            with p.scope():
                wcc = p.pool("wcc", [128, 8, 128], BF16, bufs=2)
                xc = alloc_xc(BF16)
                yc = p.sb("yc", [128, NT * 128], F32)
                ynp = p.pool("ynp", [128, NT * 128], BF16, bufs=2)
                sqp = p.pool("sqp", [128, 512], BF16, bufs=3)
                rsp = p.pool("rsp", [128, 512], F32, bufs=2)
                dg5 = p.sb("dg5", [128, 12, 5, 128], BF16)
                for cc in range(12):
                    for k_ in range(5):
                        p.vector.tensor_scalar(out=dg5[:, cc, k_, :], in0=idb[:], scalar1=cw5[:, cc, k_:k_ + 1], scalar2=None,
                                               op0=ALU.mult)
                for cc in range(12):
                    project_cc(1024 + cc * 128, xc, wcc)
                    yn = ynp.next()
                    pend = None
                    for (t0, ntl) in blocks + [(None, None)]:
                        if t0 is not None if False else False:
                            pass
                        cur = None
                        if (t0, ntl) != (None, None) and t0 is not None:
                            pass
                        if (t0 is not None):
                            N = ntl * 128
                            c0 = t0 * 128
                            off = CO + t0 * 128 if t0 < 2 else LO + (t0 - 2) * 128
                            pc = ps_all.next()
                            for k_ in range(5):
                                p.tensor.matmul(out=pc[:, 0:N], lhsT=dg5[:, cc, k_, :], rhs=xc[:, off + k_ - 2:off + k_ - 2 + N],
                                                start=(k_ == 0), stop=(k_ == 4))
                            if cc >= 8:
                                p.scalar.activation(out=yn[:, c0:c0 + N], in_=pc[:, 0:N], func=AF.Silu)
                            else:
                                p.scalar.activation(out=yc[:, c0:c0 + N], in_=pc[:, 0:N], func=AF.Silu)
                                sq = sqp.next()
                                p.scalar.activation(out=sq[:, 0:N], in_=yc[:, c0:c0 + N], func=AF.Square)
                                cur = (N, c0, sq)
                        if pend is not None:
                            (N, c0, sq) = pend
                            pss = ps_all.next()
                            p.tensor.matmul(out=pss[:, 0:N], lhsT=ones_b[:], rhs=sq[:, 0:N], start=True, stop=True)
                            rs = rsp.next()
                            p.vector.tensor_scalar(out=rs[:, 0:N], in0=pss[:, 0:N], scalar1=EPS, scalar2=None, op0=ALU.add)
                            p.scalar.activation(out=rs[:, 0:N], in_=rs[:, 0:N], func=AF.Sqrt, scale=(128.0 if cc < 4 else 1.0))
                            p.vector.reciprocal(out=rs[:, 0:N], in_=rs[:, 0:N])
                            p.gpsimd.tensor_tensor(out=yn[:, c0:c0 + N], in0=yc[:, c0:c0 + N], in1=rs[:, 0:N], op=ALU.mult)
                        pend = cur
                    dst = qT_d if cc < 4 else (kT_d if cc < 8 else vT_d)
                    p.sync.dma_start(out=dst[cc % 4], in_=yn[:])

            if L1STOP == "F2":
                return
            with p.scope():
                wcc = p.pool("wcc2", [128, 8, 128], BF16, bufs=2)
                xc = alloc_xc(F32)
                xg2 = p.sb("xg2", [128, S + 30], F32)
                xgb = p.sb("xgb", [128, S + 30], BF16)
                cvyp = p.pool("cvyp", [128, 512], F32, bufs=3)
                dgp = p.pool("dg31", [128, 31, 128], BF16, bufs=2)
                p.vector.memset(ap=xgb[:, 0:15], constant=0.0)
                p.vector.memset(ap=xgb[:, 15 + S:30 + S], constant=0.0)
                for cc in range(4):
                    dg = dgp.next()
                    for k_ in range(31):
                        p.vector.tensor_scalar(out=dg[:, k_, :], in0=idb[:], scalar1=cw31[:, cc, k_:k_ + 1], scalar2=None,
                                               op0=ALU.mult)
                    project_cc(cc * 128, xc, wcc)
                    w = wcc.next()
                    p.gpsimd.dma_start(out=w[:], in_=cd_w_in[:, 512 + cc * 128:512 + (cc + 1) * 128].rearrange("(kc p) n -> p kc n", p=128))
                    for (t0, ntl) in blocks[1:]:
                        pp = ps_all.next()
                        for kc in range(8):
                            p.tensor.matmul(out=pp[:, :], lhsT=w[:, kc, :], rhs=hT[:, kc, t0 * 128:t0 * 128 + 512],
                                            start=(kc == 0), stop=(kc == 7))
                        o0 = 15 + (t0 - 2) * 128
                        p.scalar.activation(out=xg2[:, o0:o0 + 512], in_=pp[:, :], func=AF.Sigmoid)
                        eng = p.gpsimd if (t0 // 4) % 2 == 0 else p.vector
                        eng.tensor_tensor(out=xgb[:, o0:o0 + 512], in0=xg2[:, o0:o0 + 512],
                                          in1=xc[:, LO + (t0 - 2) * 128:LO + (t0 - 2) * 128 + 512], op=ALU.mult)
                    for bi in range(8):
                        pc = ps_all.next()
                        for k_ in range(31):
                            p.tensor.matmul(out=pc[:, :], lhsT=dg[:, k_, :], rhs=xgb[:, bi * 512 + k_:bi * 512 + k_ + 512],
                                            start=(k_ == 0), stop=(k_ == 30))
                        cv = cvyp.next()
                        p.scalar.activation(out=cv[:], in_=pc[:, :], func=AF.Identity, bias=cvc[:, cc:cc + 1], scale=1.0)
                        p.sync.dma_start(out=cv_d[cc, :, bi * 512:(bi + 1) * 512], in_=cv[:])
        if L1STOP == "F3":
            return
        with p.scope():
            cvc = p.sb("cvc2", [128, 12], F32)
            cvs = p.sb("cvs2", [12, 128], F32)
            p.sync.dma_start(out=cvs[:], in_=cv_vec)
            pt = ps_all.next()
            p.tensor.transpose(out=pt[:, 0:12], in_=cvs[0:12, :], identity=idt[0:12, 0:12])
            p.vector.tensor_copy(out=cvc[:], in_=pt[:, 0:12])
            cvo = p.pool("cvo", [128, 4, 512], BF16, bufs=2)
            cvi = p.pool("cvi", [128, 4, 512], F32, bufs=2)
            lnp = p.pool("lnp", [128, 512], F32, bufs=4)
            for bi in range(8):
                cvy = cvi.next()
                p.sync.dma_start(out=cvy[:], in_=cv_d[:, :, bi * 512:(bi + 1) * 512].rearrange("j c t -> c j t"))
                pmu = ps_all.next()
                for cc in range(4):
                    p.tensor.matmul(out=pmu[:, :], lhsT=ones_f[:], rhs=cvy[:, cc, :], start=(cc == 0), stop=(cc == 3))
                nmean = lnp.next()
                p.vector.tensor_scalar(out=nmean[:], in0=pmu[:, :], scalar1=-1.0 / 512, scalar2=None, op0=ALU.mult)
                pvar = ps_all.next()
                for cc in range(4):
                    p.gpsimd.tensor_tensor(out=cvy[:, cc, :], in0=cvy[:, cc, :], in1=nmean[:], op=ALU.add)
                    sq = lnp.next()
                    p.scalar.activation(out=sq[:], in_=cvy[:, cc, :], func=AF.Square)
                    p.tensor.matmul(out=pvar[:, :], lhsT=ones_f[:], rhs=sq[:], start=(cc == 0), stop=(cc == 3))
                rs = lnp.next()
                p.vector.tensor_scalar(out=rs[:], in0=pvar[:, :], scalar1=1.0 / 512, scalar2=EPS, op0=ALU.mult, op1=ALU.add)
                p.scalar.activation(out=rs[:], in_=rs[:], func=AF.Sqrt)
                p.vector.reciprocal(out=rs[:], in_=rs[:])
                co = cvo.next()
                for cc in range(4):
                    p.vector.tensor_tensor(out=cvy[:, cc, :], in0=cvy[:, cc, :], in1=rs[:], op=ALU.mult)
                    p.scalar.activation(out=co[:, cc, :], in_=cvy[:, cc, :], func=AF.Silu,
                                        bias=cvc[:, 8 + cc:9 + cc], scale=cvc[:, 4 + cc:5 + cc])
                p.sync.dma_start(out=convT_d[:, :, bi * 512:(bi + 1) * 512].rearrange("j c t -> c j t"), in_=co[:])
        if L1STOP == "F":
            k.dbg("d_gall", gall[:], [64, 2, NCH, 4])
            k.dbg("d_beta", beta[:], [64, 2, NCH, 4])
            k.dbg("d_egc", egc[:], [64, 2, NCH, 4])
            k.dbg("d_egd", egd[:], [64, 2, NCH, 4])
            k.dbg("d_eglb", eglb[:], [128, 2, NCH, 4])
            return

        with p.scope():
            ps_a = PsumPool(p, [0, 1, 2, 3, 4, 5])
            ps_q = PsumPool(p, [6, 7])
            GS = 8
            kqv = p.pool("kqv", [128, 3, 4, 512], BF16, bufs=2)
            kdp = p.pool("kdp", [64, GS, 4, 128], BF16, bufs=2)
            qkp = p.pool("qkp", [64, GS, 4, 64], BF16, bufs=2)
            up = p.pool("up", [64, GS, 4, 128], BF16, bufs=2)
            wTp = p.pool("wTp", [128, GS, 4, 64], BF16, bufs=2)
            bvp = p.pool("bvp", [64, 2, 4, 128], BF16, bufs=4)
            guup = p.pool("guup", [64, 4, 128], F32, bufs=3)
            ddp = p.pool("ddp", [64, 4, 128], F32, bufs=3)
            mat = p.pool("mat", [64, 6, 4, 64], BF16, bufs=4)
            S32 = p.sb("S32", [128, 4, 128], F32)
            Sb = p.sb("Sb", [128, 4, 128], BF16)
            vnp_ = p.pool("vnp_", [64, 4, 128], BF16, bufs=3)
            otp_ = p.pool("otp_", [64, 512], F32, bufs=3)
            oqs = p.pool("oqs", [64, 512], F32, bufs=3)
            v4 = lambda ap_, st, w: fap(ap_, [[st, 4], [1, w]])
            for d_ in range(2):
                p.vector.memset(ap=S32[:], constant=0.0)
                p.vector.memset(ap=Sb[:], constant=0.0)
                if d_ == 0:
                    groups = [list(range(0, 4))] + [list(range(4 + 8 * g, 12 + 8 * g)) for g in range(8)]
                else:
                    groups = [list(range(3, -1, -1))] + [list(range(11 + 8 * g, 3 + 8 * g, -1)) for g in range(7, -1, -1)]
                UU = dnc[:, d_, :]
                PN = dnc[:, 2, :]
                NM = dnc[:, 3 + d_, :]

                def precompute(c, lo, kq, kd, qk, u_, wT):
                    ci = c - lo
                    ts_ = slice(ci * 64, (ci + 1) * 64)
                    sc4 = lambda t_, w: fap(t_[:, d_, c, 0:1], [[1, 4], [0, w]])
                    m = mat.next()
                    bv = bvp.next()
                    ptk = ps_a.next(BF16)
                    for h in range(4):
                        p.tensor.transpose(out=ptk[0:64, h * 256:h * 256 + 128], in_=kq[:, 0, h, ts_], identity=idb[:])
                        p.tensor.transpose(out=ptk[0:64, h * 256 + 128:h * 256 + 256], in_=kq[:, 2, h, ts_], identity=idb[:])
                    pk = v4(ptk[0:64, 0:1], 256, 128)
                    pv = v4(ptk[0:64, 128:129], 256, 128)
                    p.vector.tensor_tensor(out=kd[:, ci, :, :], in0=pk, in1=sc4(egd, 128), op=ALU.mult)
                    p.vector.tensor_tensor(out=bv[:, 0, :, :], in0=pk, in1=sc4(be, 128), op=ALU.mult)
                    p.vector.tensor_tensor(out=bv[:, 1, :, :], in0=pv, in1=sc4(beta, 128), op=ALU.mult)
                    yield
                    pkg = ps_a.next()
                    for h in range(4):
                        p.tensor.matmul(out=pkg[0:64, h * 128:h * 128 + 64], lhsT=kq[:, 0, h, ts_], rhs=kq[:, 0, h, ts_], start=True, stop=True)
                        p.tensor.matmul(out=pkg[0:64, h * 128 + 64:h * 128 + 128], lhsT=kq[:, 0, h, ts_], rhs=kq[:, 1, h, ts_], start=True, stop=True)
                    guu = guup.next()
                    p.gpsimd.tensor_tensor(out=guu[:], in0=fap(UU[:, 0:1], [[0, 4], [1, 128]]), in1=sc4(gall, 128), op=ALU.mult)
                    yield
                    pdd = ps_a.next()
                    for h in range(4):
                        o_ = pdd[0:64, h * 128:(h + 1) * 128]
                        p.tensor.matmul(out=o_, lhsT=guu[:, h, 64:128], rhs=PN, start=True, stop=False)
                        p.tensor.matmul(out=o_, lhsT=ones_f[0:64, 0:64], rhs=guu[:, h, :], start=False, stop=False)
                        p.tensor.matmul(out=o_, lhsT=id64, rhs=NM, start=False, stop=True)
                    dd = ddp.next()
                    p.scalar.activation(out=dd[:].rearrange("p h w -> p (h w)"), in_=pdd[0:64, 0:512], func=AF.Exp)
                    yield
                    p.vector.tensor_tensor(out=m[:, 0, :, :], in0=v4(pkg[0:64, 0:1], 128, 64), in1=dd[:, :, 0:64], op=ALU.mult)
                    p.vector.tensor_tensor(out=qk[:, ci, :, :], in0=v4(pkg[0:64, 64:65], 128, 64), in1=dd[:, :, 64:128], op=ALU.mult)
                    p.gpsimd.tensor_tensor(out=m[:, 0, :, :], in0=m[:, 0, :, :], in1=sc4(beta, 64), op=ALU.mult)
                    yield
                    pb = ps_a.next(BF16)
                    for h in range(4):
                        p.tensor.transpose(out=pb[0:64, h * 64:(h + 1) * 64], in_=m[:, 0, h, :], identity=idb[0:64, 0:64])
                    p.scalar.copy(out=m[:, 1, :, :], in_=pb[0:64, 0:256].rearrange("p (h w) -> p h w", h=4))
                    p.gpsimd.tensor_tensor(out=m[:, 3, :, :], in0=fap(idb[0:64, 0:1], [[0, 4], [1, 64]]), in1=m[:, 1, :, :], op=ALU.subtract)
                    yield
                    Pc, Qc, R = 1, 0, 3
                    for lev in range(1, 6):
                        nP = 4 if Pc == 1 else 1
                        nQ = 2 if Qc in (0, 5) else 5
                        pp_ = ps_a.next()
                        for h in range(4):
                            if lev < 5:
                                p.tensor.matmul(out=pp_[0:64, h * 128:h * 128 + 64], lhsT=m[:, Qc, h, :], rhs=m[:, Pc, h, :], start=True, stop=True)
                            p.tensor.matmul(out=pp_[0:64, h * 128 + 64:h * 128 + 128], lhsT=m[:, Pc, h, :], rhs=m[:, Qc, h, :], start=True, stop=True)
                        if lev < 5:
                            p.scalar.copy(out=m[:, nP, :, :], in_=v4(pp_[0:64, 0:1], 128, 64))
                        p.vector.tensor_copy(out=m[:, nQ, :, :], in_=v4(pp_[0:64, 64:65], 128, 64))
                        yield
                        pr = ps_a.next()
                        for h in range(4):
                            p.tensor.matmul(out=pr[0:64, h * 64:(h + 1) * 64], lhsT=m[:, nQ, h, :], rhs=m[:, R, h, :], start=True, stop=True)
                        p.vector.tensor_tensor(out=m[:, R, :, :], in0=m[:, R, :, :],
                                               in1=pr[0:64, 0:256].rearrange("p (h w) -> p h w", h=4), op=ALU.add)
                        Pc, Qc = nP, nQ
                        yield
                    pu_ = ps_a.next()
                    pw_ = ps_a.next()
                    for h in range(4):
                        p.tensor.matmul(out=pu_[0:64, h * 128:(h + 1) * 128], lhsT=m[:, R, h, :], rhs=bv[:, 1, h, :], start=True, stop=True)
                    for h in range(4):
                        p.tensor.matmul(out=pw_[:, h * 64:(h + 1) * 64], lhsT=bv[:, 0, h, :], rhs=m[:, R, h, :], start=True, stop=True)
                    p.scalar.copy(out=u_[:, ci, :, :], in_=pu_[0:64, 0:512].rearrange("p (h w) -> p h w", h=4))
                    p.scalar.copy(out=wT[:, ci, :, :], in_=pw_[:, 0:256].rearrange("p (h w) -> p h w", h=4))
                    yield

                def scan_gen(grp, lo, kq, kd, qk, u_, wT):
                    for c in grp:
                        ci = c - lo
                        ts_ = slice(ci * 64, (ci + 1) * 64)
                        islat = c >= 4
                        sc4 = lambda t_, w, c=c: fap(t_[:, d_, c, 0:1], [[1, 4], [0, w]])
                        p1a = ps_q.next()
                        for h in range(4):
                            p.tensor.matmul(out=p1a[0:64, h * 128:(h + 1) * 128], lhsT=wT[:, ci, h, :], rhs=Sb[:, h, :], start=True, stop=True)
                        if islat:
                            p1b = ps_q.next()
                            for h in range(4):
                                p.tensor.matmul(out=p1b[0:64, h * 128:(h + 1) * 128], lhsT=kq[:, 1, h, ts_], rhs=Sb[:, h, :], start=True, stop=True)
                        p.gpsimd.tensor_tensor(out=S32[:], in0=S32[:], in1=sc4(eglb, 128), op=ALU.mult)
                        vn = vnp_.next()
                        p.vector.tensor_tensor(out=vn[:], in0=u_[:, ci, :, :], in1=p1a[0:64, 0:512].rearrange("p (h w) -> p h w", h=4),
                                               op=ALU.subtract)
                        if islat:
                            oq = oqs.next()
                            p.vector.tensor_tensor(out=oq[:].rearrange("p (h w) -> p h w", h=4),
                                                   in0=p1b[0:64, 0:512].rearrange("p (h w) -> p h w", h=4), in1=sc4(egc, 128), op=ALU.mult)
                        yield
                        p2a = ps_q.next()
                        for h in range(4):
                            p.tensor.matmul(out=p2a[:, h * 128:(h + 1) * 128], lhsT=kd[:, ci, h, :], rhs=vn[:, h, :], start=True, stop=True)
                        p.vector.tensor_tensor(out=S32[:], in0=S32[:], in1=p2a[:, :].rearrange("p (h w) -> p h w", h=4), op=ALU.add)
                        p.scalar.copy(out=Sb[:], in_=S32[:])
                        if islat:
                            p2b = ps_q.next()
                            for h in range(4):
                                p.tensor.matmul(out=p2b[0:64, h * 128:(h + 1) * 128], lhsT=qk[:, ci, h, :], rhs=vn[:, h, :], start=True, stop=True)
                            ot = otp_.next()
                            p.vector.tensor_tensor(out=ot[:], in0=oq[:], in1=p2b[0:64, 0:512], op=ALU.add)
                            p.sync.dma_start(out=o_d[d_, (c - 4) * 64:(c - 3) * 64, :], in_=ot[:])
                        yield

                prev_scan = None
                for grp in groups:
                    lo = min(grp)
                    ntok = len(grp) * 64
                    kq = kqv.next()
                    for si, src in enumerate((kT_d, qT_d, vT_d)):
                        p.sync.dma_start(out=kq[:, si, :, 0:ntok],
                                         in_=src[:, :, lo * 64:lo * 64 + ntok].rearrange("h c t -> c h t"))
                    kd = kdp.next()
                    qk = qkp.next()
                    u_ = up.next()
                    wT = wTp.next()
                    for pi in range(0, len(grp), 2):
                        gens = [precompute(c, lo, kq, kd, qk, u_, wT) for c in grp[pi:pi + 2]]
                        alive = list(gens)
                        rnd = 0
                        while alive:
                            nxt = []
                            for g_ in alive:
                                try:
                                    next(g_)
                                    nxt.append(g_)
                                except StopIteration:
                                    pass
                            alive = nxt
                            rnd += 1
                            if prev_scan is not None and rnd % 3 == 0:
                                try:
                                    next(prev_scan)
                                except StopIteration:
                                    prev_scan = None
                    if prev_scan is not None:
                        for _ in prev_scan:
                            pass
                    prev_scan = scan_gen(grp, lo, kq, kd, qk, u_, wT)
                for _ in prev_scan:
                    pass
        if L1STOP == "D":
            return
        with p.scope():
            wo = p.sb("wo1", [128, 8, D], BF16)
            p.gpsimd.dma_start(out=wo[:], in_=cd_w_out.rearrange("(c p) n -> p c n", p=128))
            gon = p.sb("gon", [128, 128], F32)
            p.sync.dma_start(out=gon[:], in_=dn_onorm.partition_broadcast(128))
            o0p = p.pool("o0p", [128, 512], F32, bufs=2)
            o1p = p.pool("o1p", [128, 512], F32, bufs=2)
            sgl = p.pool("sgl", [128, 512], BF16, bufs=2)
            ybp = p.pool("ybp", [128, 512], BF16, bufs=3)
            yTp = p.pool("yTp", [128, 8, 128], BF16, bufs=3)
            tmpp = p.pool("tmp1", [128, D], F32, bufs=2)
            def fl_s1(t):
                r0 = (t - 2) * 128
                o0 = o0p.next()
                o1 = o1p.next()
                sg = sgl.next()
                p.sync.dma_start(out=o0[:], in_=o_d[0, r0:r0 + 128, :])
                p.sync.dma_start(out=o1[:], in_=o_d[1, r0:r0 + 128, :])
                p.sync.dma_start(out=sg[:], in_=sog_d[r0:r0 + 128, :])
                yT = yTp.next()
                p.sync.dma_start(out=yT[:, 0:4, :], in_=convT_d[:, :, r0:r0 + 128].rearrange("j c t -> c j t"))
                xt = xpool.next()
                p.sync.dma_start(out=xt[:], in_=src_lat[r0:r0 + 128, :])
                p.gpsimd.tensor_tensor(out=o0[:], in0=o0[:], in1=o1[:], op=ALU.add)
                jk = junk.next()
                sm = small.next()
                p.scalar.activation(out=jk[:, 0:512], in_=o0[:], func=AF.Square)
                p.vector.tensor_reduce(out=sm[:, 0:4], in_=jk[:, 0:512].rearrange("p (h d) -> p h d", d=128), axis=AX.X, op=ALU.add)
                sm2 = small.next()
                rstd_from_ss(p, sm2[:, 0:4], sm[:, 0:4], 128, sm[:, 4:8])
                p.vector.tensor_tensor(out=o0[:].rearrange("p (h d) -> p h d", d=128), in0=o0[:].rearrange("p (h d) -> p h d", d=128),
                                       in1=fap(sm2[:, 0:1], [[1, 4], [0, 128]]), op=ALU.mult)
                p.gpsimd.tensor_tensor(out=o0[:].rearrange("p (h d) -> p h d", d=128), in0=o0[:].rearrange("p (h d) -> p h d", d=128),
                                       in1=fap(gon[:, 0:1], [[0, 4], [1, 128]]), op=ALU.mult)
                yb = ybp.next()
                p.vector.tensor_tensor(out=yb[:], in0=o0[:], in1=sg[:], op=ALU.mult)
                return (t, yb, yT, xt)

            def fl_s2(t, yb, yT, xt):
                r0 = (t - 2) * 128
                pty = ps_all.next(BF16)
                for j in range(4):
                    p.tensor.transpose(out=pty[:, j * 128:(j + 1) * 128], in_=yb[:, j * 128:(j + 1) * 128], identity=idb[:])
                p.scalar.copy(out=yT[:, 4:8, :], in_=pty[:, 0:512].rearrange("p (j q) -> p j q", j=4))
                tm = tmpp.next()
                for half in range(2):
                    pm = ps_all.next()
                    for c_ in range(8):
                        p.tensor.matmul(out=pm[:, :], lhsT=yT[:, c_, :], rhs=wo[:, c_, half * 512:(half + 1) * 512],
                                        start=(c_ == 0), stop=(c_ == 7))
                    p.vector.tensor_tensor(out=tm[:, half * 512:(half + 1) * 512], in0=pm[:, :],
                                           in1=gabc[:, 0, half * 512:(half + 1) * 512], op=ALU.mult)
                p.gpsimd.tensor_tensor(out=tm[:], in0=tm[:], in1=xt[:], op=ALU.add)
                p.sync.dma_start(out=dst_lat[r0:r0 + 128, :], in_=tm[:])

            pend = None
            for t in range(2, NT):
                cur = fl_s1(t)
                if pend is not None:
                    fl_s2(*pend)
                pend = cur
            fl_s2(*pend)

    k.layer1_mixer = layer1_mixer

    def dbg(name, ap, shape, dt=F32):
        o = p.dram(name, shape, dt, kind="ExternalOutput")
        p.sync.dma_start(out=o, in_=ap)
    k.dbg = dbg
    k.sb = dict(modcol=modcol, gabc=gabc, gmod1=gmod1, gmod2=gmod2, colT=colT)
    k.mod_stage = mod_stage
    k.layer0_mixer = layer0_mixer
    k.moe = moe
    k.p = p
    k.nc = nc
    k.drams = dict(x_in=x_in, ctx_in=ctx_in, xl1=xl1, xc1=xc1, xl2=xl2, xc2=xc2, xl3=xl3, out=out)
    return k


def dn_consts():
    i = np.arange(64)
    out = np.zeros((64, 5, 128), np.float32)
    U0 = (i[:, None] <= i[None, :]).astype(np.float32)
    U1 = (i[:, None] >= i[None, :]).astype(np.float32)
    out[:, 0, :64] = -U0; out[:, 0, 64:] = U0
    out[:, 1, :64] = -U1; out[:, 1, 64:] = U1
    out[:, 2, :64] = 1.0; out[:, 2, 64:] = -1.0
    NEG = -30000.0
    out[:, 3, :64] = np.where(i[:, None] > i[None, :], 0.0, NEG)
    out[:, 3, 64:] = np.where(i[None, :] >= i[:, None], 0.0, NEG)
    out[:, 4, :64] = np.where(i[:, None] < i[None, :], 0.0, NEG)
    out[:, 4, 64:] = np.where(i[None, :] <= i[:, None], 0.0, NEG)
    return out

def host_inputs(inputs, b):
    f = lambda a: np.ascontiguousarray(np.asarray(a, dtype=np.float32))
    rows = S // 64
    row = np.repeat(np.arange(rows), 64); col = np.tile(np.arange(64), rows)
    freqs = (10000.0 ** (-np.arange(16, dtype=np.float32) / 16)).astype(np.float32)
    ang = np.stack([row, col], -1).astype(np.float32)[..., None] * freqs
    m = {
        "x": f(inputs["x"][b]), "ctx": f(inputs["ctx"][b]), "c": f(inputs["c"][b]).reshape(8, 128),
        "c_ctx": f(inputs["c_ctx"]).reshape(8, 128), "mod_w": f(inputs["mod_w"]),
        "mod_b": f(inputs["mod_b"]).reshape(2, 48, 128), "norm1_g": f(inputs["norm1_g"]).reshape(2, 8, 128),
        "norm2_g": f(inputs["norm2_g"]).reshape(2, 8, 128), "final_norm_g": f(inputs["final_norm_g"]).reshape(1, D),
        "ab_w_in": f(inputs["ab_w_in"][0]), "ab_q_norm": f(inputs["ab_q_norm"]), "ab_k_norm": f(inputs["ab_k_norm"]),
        "gm_ln_g": f(inputs["gm_ln_g"]), "gm_ln_b": f(inputs["gm_ln_b"]), "gm_w_s": f(inputs["gm_w_s"][0]),
        "gm_b_s": f(inputs["gm_b_s"][0]).reshape(1, 512), "ab_w_out": f(inputs["ab_w_out"][0]),
        "router_w": f(inputs["router_w"]), "router_b": f(inputs["router_b"]).reshape(1, NE),
        "moe_w_gate": f(inputs["moe_w_gate"]), "moe_w_up": f(inputs["moe_w_up"]), "moe_w_down": f(inputs["moe_w_down"]),
        "ident": np.eye(128, dtype=np.float32),
        "rope_cos": np.cos(ang).reshape(S, 32).astype(np.float32), "rope_sin": np.sin(ang).reshape(S, 32).astype(np.float32),
        "cd_w_in": f(inputs["cd_w_in"][0]), "cv_dw_w": f(inputs["cv_dw_w"][0]),
        "cv_vec": np.concatenate([f(inputs["cv_dw_b"][0]).reshape(4, 128), f(inputs["cv_ln_g"][0]).reshape(4, 128), f(inputs["cv_ln_b"][0]).reshape(4, 128)], 0),
        "dn_conv_w": f(inputs["dn_conv_w"][0]), "dn_a_log": f(inputs["dn_a_log"][0]).reshape(1, 8),
        "dn_dt_bias": f(inputs["dn_dt_bias"][0]).reshape(1, 8), "dn_o_norm": f(inputs["dn_o_norm"]).reshape(1, 128),
        "cd_w_out": f(inputs["cd_w_out"][0]), "dn_consts": dn_consts(),
        "su_mat": np.triu(np.ones((128, 128), np.float32), 1),
        "thr512": (512.0 * np.arange(40, dtype=np.float32)).reshape(1, 40),
        "base8": (np.arange(8, dtype=np.float32)[None, :] * 128 + np.arange(128, dtype=np.float32)[:, None]),
        "base4": (np.arange(4, dtype=np.float32)[None, :] * 128 + np.arange(128, dtype=np.float32)[:, None]),
        "zeros_bf": np.zeros((512, D), ml_dtypes.bfloat16),
    }
    return m


def build_full():
    k = build(debug=False)
    d = k.drams
    k.precast_iter = k.precast_weights()
    k.mod_stage(0)
    k.layer0_mixer()
    k.moe_sparse(0, (d["xl1"], d["xc1"]), (d["xl2"], d["xc2"]), list(range(NT)))
    k.mod_stage(1)
    k.layer1_mixer(d["xl2"], d["xc2"], d["xl3"])
    k.moe_sparse(1, (d["xl3"], d["xc2"]), (d["out"], d["xc2"]), list(range(2, NT)), final=True)
    k.p.emit()
    return k.nc


def kernel(**inputs):
    nb = inputs["x"].shape[0]
    nc = build_full()
    shared = host_inputs(inputs, 0)
    in_maps = []
    for b in range(nb):
        m = dict(shared)
        m["x"] = np.ascontiguousarray(np.asarray(inputs["x"][b], dtype=np.float32))
        m["ctx"] = np.ascontiguousarray(np.asarray(inputs["ctx"][b], dtype=np.float32))
        m["c"] = np.ascontiguousarray(np.asarray(inputs["c"][b], dtype=np.float32)).reshape(8, 128)
        in_maps.append(m)
    res = run_bass_kernel_spmd(nc, in_maps, core_ids=list(range(nb)))
    return np.stack([np.asarray(r["out"], dtype=np.float32) for r in res.results], axis=0)
```
